# Optimizing a Trainium2 kernel written in Bass

```python
import jax
import jax.numpy as jnp
from jax import lax
import numpy as np

D_MODEL = 1024
BATCH = 4
SEQ = 4096
DEPTH = 4

GRID_W = 64
CTX_LEN = 256

NA_HEAD_DIM = 64
NA_WIDTH = D_MODEL // 2
NA_HEADS = NA_WIDTH // NA_HEAD_DIM
NA_KH = 8
NA_KW = 16
HG_KEY_DIM = 128
HG_WIDTH = D_MODEL // 2
HG_HEADS = HG_WIDTH // HG_KEY_DIM
HG_CHUNK = 16
EVEN_SPLITS = (NA_WIDTH, NA_WIDTH, NA_WIDTH, HG_WIDTH, HG_WIDTH, HG_WIDTH, HG_WIDTH, HG_WIDTH)
EVEN_CUTS = tuple(sum(EVEN_SPLITS[:i + 1]) for i in range(len(EVEN_SPLITS) - 1))
EVEN_IN = sum(EVEN_SPLITS)
EVEN_MIX = NA_WIDTH + HG_WIDTH
RET_QK_DIM = 256
RET_HEADS = D_MODEL // RET_QK_DIM
RET_V_DIM = 2 * RET_QK_DIM
RET_QK_W = RET_HEADS * RET_QK_DIM
RET_V_W = RET_HEADS * RET_V_DIM
RET_CHUNK = 128
ODD_CUTS = (RET_QK_W, 2 * RET_QK_W, 2 * RET_QK_W + RET_V_W)
ODD_IN = 2 * RET_QK_W + 2 * RET_V_W
FFN_DIM = 2816
N_EXPERTS = 8
TOP_K = 2
EXPERT_DIM = 3584
ROPE_BASE = 10000.0
LN_EPS = 1e-5
NORM_EPS = 1e-6
N_EVEN = (DEPTH + 1) // 2
N_ODD = DEPTH // 2
ALPHA = (2 * DEPTH) ** 0.25
BETA = (8 * DEPTH) ** -0.25
F32 = jnp.float32

kernel_name = 'hybrid_natten_hgrn2_retention_moe_dit'


def layer_norm(x, g, b):
    xf = x.astype(F32)
    mu = jnp.mean(xf, -1, keepdims=True)
    var = jnp.mean(jnp.square(xf - mu), -1, keepdims=True)
    return ((xf - mu) * lax.rsqrt(var + LN_EPS) * g + b).astype(x.dtype)


def group_norm(x):
    xf = x.astype(F32)
    mu = jnp.mean(xf, -1, keepdims=True)
    var = jnp.mean(jnp.square(xf - mu), -1, keepdims=True)
    return ((xf - mu) * lax.rsqrt(var + LN_EPS)).astype(x.dtype)


def rms_norm(x, g):
    xf = x.astype(F32)
    return (xf * lax.rsqrt(jnp.mean(jnp.square(xf), -1, keepdims=True) + NORM_EPS) * g).astype(x.dtype)


def modulate(x, shift, scale):
    return x * (1.0 + scale) + shift


def heads(x, n):
    b, t, w = x.shape
    return x.reshape(b, t, n, w // n).transpose(0, 2, 1, 3)


def merge(x):
    b, n, t, d = x.shape
    return x.transpose(0, 2, 1, 3).reshape(b, t, n * d)


def rope_1d(x, pos):
    half = x.shape[-1] // 2
    inv = ROPE_BASE ** (-jnp.arange(half, dtype=F32) / half)
    ang = pos.astype(F32)[:, None] * inv
    cos, sin = jnp.cos(ang), jnp.sin(ang)
    x1, x2 = x[..., :half], x[..., half:]
    return jnp.concatenate([x1 * cos - x2 * sin, x2 * cos + x1 * sin], -1).astype(x.dtype)


def rope_2d(x, rows, cols):
    h = x.shape[-1] // 2
    return jnp.concatenate([rope_1d(x[..., :h], rows), rope_1d(x[..., h:], cols)], -1)


def swiglu(h, w1, w3, w2):
    return (jax.nn.silu(h @ w1) * (h @ w3)) @ w2


def moe_swiglu(h, rw, rb, w1, w3, w2):
    logits = (h @ rw + rb).astype(F32)
    top_val, top_idx = lax.top_k(logits, TOP_K)
    gate = jax.nn.softmax(top_val, axis=-1)
    combine = jnp.sum(jax.nn.one_hot(top_idx, N_EXPERTS, dtype=F32) * gate[..., None], axis=-2)
    out = jnp.zeros_like(h)
    for e in range(N_EXPERTS):
        out = out + combine[..., e:e + 1].astype(h.dtype) * swiglu(h, w1[e], w3[e], w2[e])
    return out


def full_attention(q, k, v):
    s = jnp.einsum('bhqd,bhkd->bhqk', q, k).astype(F32) * q.shape[-1] ** -0.5
    p = jax.nn.softmax(s, axis=-1)
    return jnp.einsum('bhqk,bhkd->bhqd', p, v).astype(q.dtype)


def neighbourhood_attention(q, k, v, kc, vc, rpb):
    b, h, n, dh = q.shape
    rows = n // GRID_W
    kh = min(NA_KH, rows)
    grid = lambda a: a.reshape(b, h, rows, GRID_W, dh)
    qg, kg, vg = grid(q) * dh ** -0.5, grid(k), grid(v)
    r = jnp.arange(rows)
    band = jnp.clip(r - kh // 2, 0, rows - kh)[:, None] + jnp.arange(kh)[None, :]
    k_band = kg[:, :, band]
    v_band = vg[:, :, band]
    col = jnp.arange(GRID_W)
    c_start = jnp.clip(col - NA_KW // 2, 0, GRID_W - NA_KW)
    col_in = (col[None, :] >= c_start[:, None]) & (col[None, :] < c_start[:, None] + NA_KW)
    dr = band - r[:, None] + NA_KH - 1
    dc = jnp.clip(col[None, :] - col[:, None], 1 - NA_KW, NA_KW - 1) + NA_KW - 1
    bias = rpb[:, dr[:, None, :, None], dc[None, :, None, :]]
    s_band = jnp.einsum('bhrcd,bhrkmd->bhrckm', qg, k_band).astype(F32) + bias.astype(F32)
    s_band = jnp.where(col_in[:, None, :], s_band, -jnp.inf)
    s_ctx = jnp.einsum('bhrcd,bhjd->bhrcj', qg, kc).astype(F32)
    nb = kh * GRID_W
    p = jax.nn.softmax(jnp.concatenate([s_band.reshape(b, h, rows, GRID_W, nb), s_ctx], -1), axis=-1)
    o = (jnp.einsum('bhrckm,bhrkmd->bhrcd', p[..., :nb].reshape(b, h, rows, GRID_W, kh, GRID_W), v_band)
         + jnp.einsum('bhrcj,bhjd->bhrcd', p[..., nb:], vc))
    return o.reshape(b, h, n, dh).astype(q.dtype)


def chunk_recurrence(q, k, v, log_f, s0, chunk, per_dim):
    b, h, t, dk = q.shape
    n = t // chunk
    blk = lambda a: a.reshape(b, h, n, chunk, a.shape[-1])
    qb, kb, vb = blk(q), blk(k), blk(v)
    cum = jnp.cumsum(blk(log_f.astype(F32)), axis=-2)
    cum_last = cum[..., -1:, :]
    lower = jnp.tril(jnp.ones((chunk, chunk), bool))
    diff = cum[..., :, None, :] - cum[..., None, :, :]
    if per_dim:
        dec = jnp.exp(jnp.where(lower[..., None], diff, -jnp.inf))
        att = jnp.einsum('bhntd,bhnsd,bhntsd->bhnts', qb, kb, dec)
    else:
        dec = jnp.exp(jnp.where(lower, diff[..., 0], -jnp.inf))
        att = jnp.einsum('bhntd,bhnsd->bhnts', qb, kb) * dec
    o_intra = jnp.einsum('bhnts,bhnsv->bhntv', att, vb)
    q_in = jnp.moveaxis(qb * jnp.exp(cum), 2, 0)
    u = jnp.einsum('bhncd,bhncv->nbhdv', kb * jnp.exp(cum_last - cum), vb)
    g = jnp.moveaxis(jnp.exp(cum_last[..., 0, :]), 2, 0)

    def step(s, inp):
        qi, gi, ui = inp
        return gi[..., None] * s + ui, jnp.einsum('bhcd,bhdv->bhcv', qi, s)

    s_final, o_inter = lax.scan(step, s0.astype(F32), (q_in, g, u))
    o = o_intra + jnp.moveaxis(o_inter, 0, 2)
    return o.reshape(b, h, t, v.shape[-1]).astype(v.dtype), s_final


def final_state(k, v, log_f):
    cum = jnp.cumsum(log_f.astype(F32), axis=-2)
    return jnp.einsum('bhtd,bhtv->bhdv', k * jnp.exp(cum[..., -1:, :] - cum), v)


def bidirectional_recurrence(q_x, v_x, k_x, lf_x, q_c, v_c, k_c, lf_c, chunk, per_dim, ctx_out):
    b, h, _, dk = k_c[0].shape
    dv = v_c.shape[-1]
    outs_x, outs_c = [], []
    for d in range(2):
        rev = (lambda a: jnp.flip(a, axis=2)) if d else (lambda a: a)
        if ctx_out:
            o_c, s_c = chunk_recurrence(rev(q_c), rev(k_c[d]), rev(v_c), rev(lf_c[d]),
                                        jnp.zeros((b, h, dk, dv), F32), chunk, per_dim)
            outs_c.append(rev(o_c))
        else:
            s_c = final_state(rev(k_c[d]), rev(v_c), rev(lf_c[d]))
        o_x, _ = chunk_recurrence(rev(q_x), rev(k_x[d]), rev(v_x), rev(lf_x[d]), s_c, chunk, per_dim)
        outs_x.append(rev(o_x))
    return outs_x[0] + outs_x[1], (outs_c[0] + outs_c[1] if ctx_out else None)


def hgrn_log_forget(z, lb):
    lb = lb.astype(F32)
    return jnp.logaddexp(jnp.log(lb), jnp.log1p(-lb) + jax.nn.log_sigmoid(z.astype(F32)))


def hgrn2_inputs(p, lb):
    q = heads(jax.nn.silu(p[0]), HG_HEADS)
    lf = [hgrn_log_forget(heads(p[1 + d], HG_HEADS), lb[d].reshape(HG_HEADS, 1, HG_KEY_DIM)) for d in range(2)]
    k = [-jnp.expm1(f) for f in lf]
    return q, heads(p[3], HG_HEADS), k, lf


def even_mixer(hx, hc, w_in, w_out, rpb, lb, norm_g, ctx_out):
    px = jnp.split(hx @ w_in, EVEN_CUTS, axis=-1)
    pc = jnp.split(hc @ w_in, EVEN_CUTS, axis=-1)
    na_q, na_k, na_v = (heads(p, NA_HEADS) for p in px[:3])
    nc_q, nc_k, nc_v = (heads(p, NA_HEADS) for p in pc[:3])
    a_x = neighbourhood_attention(na_q, na_k, na_v, nc_k, nc_v, rpb)
    qx, vx, kx, lfx = hgrn2_inputs(px[3:7], lb)
    qc, vc, kc, lfc = hgrn2_inputs(pc[3:7], lb)
    ox, oc = bidirectional_recurrence(qx, vx, kx, lfx, qc, vc, kc, lfc, HG_CHUNK, True, ctx_out)
    g_x = merge(rms_norm(ox, norm_g) * jax.nn.silu(heads(px[7], HG_HEADS)))
    yx = jnp.concatenate([merge(a_x), g_x], -1) @ w_out
    if not ctx_out:
        return yx, None
    a_c = full_attention(nc_q, nc_k, nc_v)
    g_c = merge(rms_norm(oc, norm_g) * jax.nn.silu(heads(pc[7], HG_HEADS)))
    return yx, jnp.concatenate([merge(a_c), g_c], -1) @ w_out


def odd_mixer(hx, hc, w_in, w_out, log_decay, ctx_out):
    b, n, _ = hx.shape
    l = hc.shape[1]
    qx, kx, vx, gx = jnp.split(hx @ w_in, ODD_CUTS, axis=-1)
    t = jnp.arange(n)
    rows, cols = t // GRID_W, t % GRID_W
    qx = rope_2d(heads(qx, RET_HEADS), rows, cols)
    kx = rope_2d(heads(kx, RET_HEADS), rows, cols) * RET_QK_DIM ** -0.5
    vx = heads(vx, RET_HEADS)
    if ctx_out:
        qc, kc, vc, gc = jnp.split(hc @ w_in, ODD_CUTS, axis=-1)
        qc = heads(qc, RET_HEADS)
    else:
        kc, vc = jnp.split(hc @ w_in[:, ODD_CUTS[0]:ODD_CUTS[2]], [RET_QK_W], axis=-1)
        qc = None
    kc = heads(kc, RET_HEADS) * RET_QK_DIM ** -0.5
    vc = heads(vc, RET_HEADS)
    decay = lambda tl: [jnp.broadcast_to(log_decay[d].astype(F32)[None, :, None, None], (b, RET_HEADS, tl, 1))
                        for d in range(2)]
    ox, oc = bidirectional_recurrence(qx, vx, [kx, kx], decay(n), qc, vc, [kc, kc], decay(l),
                                      RET_CHUNK, False, ctx_out)
    yx = (merge(group_norm(ox)) * jax.nn.silu(gx)) @ w_out
    if not ctx_out:
        return yx, None
    return yx, (merge(group_norm(oc)) * jax.nn.silu(gc)) @ w_out


def setup_inputs(seed: int = 0) -> dict:
    key = jax.random.key(seed)
    keys = iter(jax.random.split(key, 32))

    def nrm(shape, scale):
        return jax.random.normal(next(keys), shape, F32) * scale

    d = D_MODEL
    decay0 = jnp.log1p(-jnp.exp2(-5.0 - jnp.arange(RET_HEADS, dtype=F32)))
    return {
        'x': nrm((BATCH, SEQ, d), 1.0),
        'c': nrm((BATCH, d), 1.0),
        'ctx': nrm((BATCH, CTX_LEN, d), 1.0),
        'c_ctx': nrm((d,), 1.0),
        'ada_w': nrm((DEPTH, d, 6 * d), 0.5 * d ** -0.5),
        'ada_b': nrm((DEPTH, 6 * d), 0.02),
        'ln_g': 1.0 + nrm((DEPTH, 2, d), 0.02),
        'ln_b': nrm((DEPTH, 2, d), 0.02),
        'e_w_in': nrm((N_EVEN, d, EVEN_IN), d ** -0.5),
        'e_w_out': nrm((N_EVEN, EVEN_MIX, d), BETA * EVEN_MIX ** -0.5),
        'na_rpb': nrm((N_EVEN, NA_HEADS, 2 * NA_KH - 1, 2 * NA_KW - 1), 0.05),
        'hg_lb_logits': nrm((2, N_EVEN, HG_WIDTH), 0.5),
        'hg_norm_g': 1.0 + nrm((N_EVEN, HG_KEY_DIM), 0.02),
        'ffn_w1': nrm((N_EVEN, d, FFN_DIM), d ** -0.5),
        'ffn_w3': nrm((N_EVEN, d, FFN_DIM), d ** -0.5),
        'ffn_w2': nrm((N_EVEN, FFN_DIM, d), BETA * FFN_DIM ** -0.5),
        'o_w_in': nrm((N_ODD, d, ODD_IN), d ** -0.5),
        'o_w_out': nrm((N_ODD, RET_V_W, d), BETA * RET_V_W ** -0.5),
        'ret_log_decay': decay0 * jax.random.uniform(next(keys), (N_ODD, 2, RET_HEADS), F32, 0.9, 1.1),
        'router_w': nrm((N_ODD, d, N_EXPERTS), d ** -0.5),
        'router_b': nrm((N_ODD, N_EXPERTS), 0.01),
        'moe_w1': nrm((N_ODD, N_EXPERTS, d, EXPERT_DIM), d ** -0.5),
        'moe_w3': nrm((N_ODD, N_EXPERTS, d, EXPERT_DIM), d ** -0.5),
        'moe_w2': nrm((N_ODD, N_EXPERTS, EXPERT_DIM, d), BETA * EXPERT_DIM ** -0.5),
    }


def reference(x, c, ctx, c_ctx, ada_w, ada_b, ln_g, ln_b, e_w_in, e_w_out, na_rpb, hg_lb_logits, hg_norm_g,
              ffn_w1, ffn_w3, ffn_w2, o_w_in, o_w_out, ret_log_decay, router_w, router_b, moe_w1, moe_w3, moe_w2):
    lb_cum = jnp.cumsum(jax.nn.softmax(hg_lb_logits.astype(F32), axis=1), axis=1)
    lower_bounds = lb_cum - lb_cum[:, :1]
    s_x = jax.nn.silu(c)
    s_c = jax.nn.silu(c_ctx)
    for layer in range(DEPTH):
        j = layer // 2
        last = layer == DEPTH - 1
        mx = [m[:, None, :] for m in jnp.split(s_x @ ada_w[layer] + ada_b[layer], 6, axis=-1)]
        mc = jnp.split(s_c @ ada_w[layer] + ada_b[layer], 6, axis=-1)
        hx = modulate(x, mx[0], mx[1])
        hc = modulate(ctx, mc[0], mc[1])
        if layer % 2 == 0:
            yx, yc = even_mixer(hx, hc, e_w_in[j], e_w_out[j], na_rpb[j], lower_bounds[:, j], hg_norm_g[j], not last)
            ffn = lambda h: swiglu(h, ffn_w1[j], ffn_w3[j], ffn_w2[j])
        else:
            yx, yc = odd_mixer(hx, hc, o_w_in[j], o_w_out[j], ret_log_decay[j], not last)
            ffn = lambda h: moe_swiglu(h, router_w[j], router_b[j], moe_w1[j], moe_w3[j], moe_w2[j])
        x = layer_norm(ALPHA * x + mx[2] * yx, ln_g[layer, 0], ln_b[layer, 0])
        x = layer_norm(ALPHA * x + mx[5] * ffn(modulate(x, mx[3], mx[4])), ln_g[layer, 1], ln_b[layer, 1])
        if not last:
            ctx = layer_norm(ALPHA * ctx + mc[2] * yc, ln_g[layer, 0], ln_b[layer, 0])
            ctx = layer_norm(ALPHA * ctx + mc[5] * ffn(modulate(ctx, mc[3], mc[4])), ln_g[layer, 1], ln_b[layer, 1])
    return x
```

```python
from contextlib import ExitStack

import numpy as np
import concourse.bass as bass
import concourse.mybir as mybir
from concourse.bass_utils import run_bass_kernel_spmd

F32 = mybir.dt.float32
BF16 = mybir.dt.bfloat16
AF = mybir.ActivationFunctionType
ALU = mybir.AluOpType
AX = mybir.AxisListType


class Prog:
    ENGS = ("sync", "act", "dve", "pool", "pe")
    NDMA = 16
    EPOCH = 8192

    def __init__(self, nc, same_engine_sync=None):
        import os as _os
        if same_engine_sync is None:
            same_engine_sync = _os.environ.get("SAME_SYNC", "act,dve,pool")
        self.nc = nc
        self.ops = {e: [] for e in self.ENGS}
        self.cnt = {e: 0 for e in self.ENGS}
        self.last_w = {}
        self.readers = {}
        self.waited = {e: {} for e in self.ENGS}
        self.ndma = 0
        self.dma_hist = {}
        self.same = same_engine_sync
        self.final_events = []
        self.semnames = set()

    def _deps(self, eng, reads, writes):
        deps = set()
        for k in reads:
            lw = self.last_w.get(k)
            if lw is not None:
                deps.add(lw)
        for k in writes:
            lw = self.last_w.get(k)
            if lw is not None:
                deps.add(lw)
            for r in self.readers.get(k, ()):
                deps.add(r)
        out = []
        best = {}
        for (s, v) in deps:
            if s.split("#")[0] == eng and eng not in self.same:
                continue
            if best.get(s, 0) < v:
                best[s] = v
        for s, v in best.items():
            if self.waited[eng].get(s, 0) < v:
                self.waited[eng][s] = v
                out.append((s, v))
        return out

    def _commit(self, ev, reads, writes):
        for k in reads:
            self.readers.setdefault(k, []).append(ev)
        for k in writes:
            self.last_w[k] = ev
            self.readers[k] = []

    def op(self, eng, fn, reads=(), writes=()):
        waits = self._deps(eng, reads, writes)
        self.cnt[eng] += 1
        n = self.cnt[eng]
        ev = ("%s#%d" % (eng, (n - 1) // self.EPOCH), (n - 1) % self.EPOCH + 1)
        self.semnames.add(ev[0])
        self.ops[eng].append(("op", fn, waits, ev))
        self._commit(ev, reads, writes)
        return ev

    def dma(self, out, in_, reads=(), writes=(), q="sync", **kw):
        i = self.ndma
        self.ndma += 1
        s = "dma%d" % (i % self.NDMA)
        v = 16 * (i // self.NDMA + 1)
        waits = self._deps(q, reads, writes)
        if v > 16 and self.waited[q].get(s, 0) < v - 16:
            self.waited[q][s] = v - 16
            waits.append((s, v - 16))
        ev = (s, v)
        self.ops[q].append(("dma", (out, in_, kw), waits, ev))
        self._commit(ev, reads, writes)
        return ev

    def finish(self, eng="sync"):
        evs = []
        n = self.ndma
        for j in range(min(n, self.NDMA)):
            last = ((n - 1 - j) // self.NDMA) * self.NDMA + j
            evs.append(("dma%d" % j, 16 * (last // self.NDMA + 1)))
        for e in self.ENGS:
            n = self.cnt[e]
            if n > 0:
                evs.append(("%s#%d" % (e, (n - 1) // self.EPOCH), (n - 1) % self.EPOCH + 1))
        self.ops[eng].append(("wait", None, evs, None))

    def wait_all(self, eng, events):
        self.ops[eng].append(("wait", None, list(events), None))

    def emit(self):
        nc = self.nc
        from contextlib import ExitStack
        with ExitStack() as st:
            sems = {}
            for sn in sorted(self.semnames):
                sems[sn] = st.enter_context(nc.semaphore("s_" + sn.replace("#", "_")))
            for i in range(self.NDMA):
                sems["dma%d" % i] = st.enter_context(nc.semaphore("s_dma%d" % i))
            block = st.enter_context(nc.Block())
            emap = {"sync": block.sync, "act": block.scalar, "dve": block.vector,
                    "pool": block.gpsimd, "pe": block.tensor}

            def make(ename):
                def body(eng):
                    for kind, fn, waits, ev in self.ops[ename]:
                        for (s, v) in waits:
                            eng.wait_ge(sems[s], v)
                        if kind == "op":
                            ins = fn(eng)
                            ins.then_inc(sems[ev[0]], 1)
                        elif kind == "dma":
                            out, in_, kw = fn
                            eng.dma_start(out=out, in_=in_, **kw).then_inc(sems[ev[0]], 16)
                return body
            for e in self.ENGS:
                if self.ops[e]:
                    emap[e](make(e))


def build_ada():
    nc = bass.Bass("TRN2", target_bir_lowering=False)
    cT = nc.dram_tensor("cT", [128, 8, 5], F32, kind="ExternalInput").ap()
    w = nc.dram_tensor("w", [1024, 3072], F32, kind="ExternalInput").ap()
    b = nc.dram_tensor("b", [1, 3072], F32, kind="ExternalInput").ap()
    m = nc.dram_tensor("m", [5, 3072], F32, kind="ExternalOutput").ap()
    with ExitStack() as st:
        sb = lambda n, s, d: st.enter_context(nc.sbuf_tensor(n, s, d))
        ct = sb("ct", [128, 8, 5], F32); sT = sb("sT", [128, 8, 5], F32)
        wt = [sb("wt%d" % i, [128, 8, 512], F32) for i in range(2)]
        bt = sb("bt", [5, 3072], F32); ot = sb("ot", [5, 3072], F32)
        ps = [st.enter_context(nc.psum_tensor("ps%d" % i, [5, 512], F32)) for i in range(2)]
        P = Prog(nc)
        P.dma(ct[:], cT[:, :, :], writes=["ct"])
        P.dma(bt[:], b[0:1, :].to_broadcast([5, 3072]), writes=["bt"])
        P.op("act", lambda e: e.activation(out=sT[:], in_=ct[:], func=AF.Silu), reads=["ct"], writes=["sT"])
        wv = w.rearrange("(k p) n -> p k n", p=128)
        for j in range(6):
            bi = j % 2
            P.dma(wt[bi][:], wv[:, :, j * 512:(j + 1) * 512], writes=[("wt", bi)], q="sync" if bi == 0 else "pool")
            for k in range(8):
                P.op("pe", lambda e, k=k, bi=bi: e.matmul(ps[bi][:], lhsT=sT[:, k, :], rhs=wt[bi][:, k, :], start=(k == 0), stop=(k == 7)),
                     reads=["sT", ("wt", bi)], writes=[("ps", bi)])
            P.op("dve", lambda e, j=j, bi=bi: e.tensor_tensor(out=ot[:, j * 512:(j + 1) * 512], in0=ps[bi][:], in1=bt[:, j * 512:(j + 1) * 512], op=ALU.add),
                 reads=[("ps", bi), "bt"], writes=["ot"])
        e1 = P.dma(m[:, :], ot[:], reads=["ot"])
        P.wait_all("sync", [e1])
        P.emit()
    return nc

def run_ada(c, c_ctx, ada_w, ada_b):
    nc = build_ada()
    cc = np.concatenate([c, c_ctx[None]], 0)
    cT = np.ascontiguousarray(cc.T.reshape(8, 128, 5).transpose(1, 0, 2))
    maps = []
    for core in range(8):
        l, h = core // 2, core % 2
        maps.append({"cT": cT, "w": np.ascontiguousarray(ada_w[l][:, h * 3072:(h + 1) * 3072]),
                     "b": np.ascontiguousarray(ada_b[l][None, h * 3072:(h + 1) * 3072])})
    res = run_bass_kernel_spmd(nc, maps, core_ids=list(range(8)))
    out = np.zeros((4, 5, 6144), np.float32)
    for core in range(8):
        l, h = core // 2, core % 2
        out[l][:, h * 3072:(h + 1) * 3072] = res.results[core]["m"]
    return out


import os
DBGMODE = os.environ.get('DENSE_DBG', '')

ALPHA_C = (2 * 4) ** 0.25
NT = 17
GROUPS = [[0, 1, 2, 3, 4, 5], [6, 7, 8, 9, 10, 11], [12, 13, 14, 15, 16]]
GT = 6
LN_EPS = 1e-5
DBG = False
NORM_EPS = 1e-6


def build_dense(post, pre, ntiles=NT, groups=GROUPS):
    nc = bass.Bass("TRN2", target_bir_lowering=False)
    R = ntiles * 128
    din = lambda n, s: nc.dram_tensor(n, s, F32, kind="ExternalInput").ap()
    x_d = din("x", [R, 1024])
    ident_d = din("ident", [128, 128])
    if post:
        modTp_d = din("modTp", [128, 2, 6, 8])
        gateB_d = din("gateB", [128, 2, 2, 1024])
        lnGB_d = din("lnGB", [128, 2, 2, 1024])
        xo_d = nc.dram_tensor("xo", [R, 1024], F32, kind="ExternalOutput").ap()
        if DBG:
            dbg_d = nc.dram_tensor("dbg", [R, 1024], F32, kind="ExternalOutput").ap()
            dbg3_d = nc.dram_tensor("dbg3", [128, 1024], F32, kind="ExternalOutput").ap()
            dbg2_d = nc.dram_tensor("dbg2", [R, 1024], F32, kind="ExternalOutput").ap()
        if post == "even":
            ax_d = din("ax", [R, 512]); of_d = din("of", [R, 512]); ob_d = din("ob", [R, 512]); gr_d = din("gr", [R, 512])
            ngB_d = din("ngB", [128, 512])
            CM = 8; NE = 1; FD = 2816
        else:
            o_d = din("o", [R, 2048]); gr_d = din("gr", [R, 2048])
            rw_d = din("rw", [1024, 8]); rbB_d = din("rbB", [128, 8])
            CM = 16; NE = 8; FD = 3584
        wout_d = din("wout", [CM * 128, 1024])
        w1_d = din("w1", [NE, 1024, FD]); w3_d = din("w3", [NE, 1024, FD]); w2_d = din("w2", [NE, FD, 1024])
    if pre:
        modTq_d = din("modTq", [128, 2, 6, 8])
        NO = 4096 if pre == "even" else 6144
        win_d = din("win", [1024, NO])
        proj_d = nc.dram_tensor("proj", [R, NO], F32, kind="ExternalOutput").ap()

    with ExitStack() as st:
        sb = lambda n, s, d: st.enter_context(nc.sbuf_tensor(n, s, d))
        pst = lambda n, s: st.enter_context(nc.psum_tensor(n, s, F32))
        P = Prog(nc)
        X = sb("X", [128, GT, 1024], F32)
        hT = sb("hT", [128, 8, GT * 128], BF16)
        ident = sb("ident_s", [128, 128], F32)
        wbf = [sb("wbf%d" % i, [128, 4096], BF16) for i in range(5)]
        tr = [pst("tr%d" % i, [128, 512]) for i in range(2)]
        h1p = [pst("h1p%d" % i, [128, 512]) for i in range(2)]
        h3p = [pst("h3p%d" % i, [128, 512]) for i in range(2)]
        yp = [pst("yp%d" % i, [128, 512]) for i in range(2)]
        stats = sb("stats", [128, 24], F32)
        mv = sb("mv", [128, 4, 2], F32)
        rstd = sb("rstd", [128, 4], F32)
        tmp = sb("tmp", [128, 1024], F32)
        P.dma(ident[:], ident_d[:, :], writes=["ident"])
        if post:
            modTp = sb("modTp_s", [128, 2, 6, 8], F32)
            gateB = sb("gateB_s", [128, 2, 2, 1024], F32)
            lnGB = sb("lnGB_s", [128, 2, 2, 1024], F32)
            Yacc = sb("Yacc", [128, GT, 1024], F32)
            aT = sb("aT", [128, 4, GT * 128], BF16)
            s1 = [sb("s1_%d" % i, [128, 512], F32) for i in range(2)]
            woutb = sb("woutb", [128, CM, 1024], BF16)
            mixT = sb("mixT", [128, CM, 128], BF16)
            P.dma(modTp[:], modTp_d[:, :, :, :], writes=["modTp"])
            P.dma(gateB[:], gateB_d[:, :, :, :], writes=["gateB"])
            P.dma(lnGB[:], lnGB_d[:, :, :, :], writes=["lnGB"])
            P.op("dve", lambda e: e.tensor_scalar_add(out=modTp[:, :, 4, :], in0=modTp[:, :, 4, :], scalar1=1.0), reads=["modTp"], writes=["modTp"])
            if post == "even":
                ngB = sb("ngB_s", [128, 512], F32)
                P.dma(ngB[:], ngB_d[:, :], writes=["ngB"])
                mi = [sb("mi%d" % i, [128, 512], F32) for i in range(4)]
                mixtm = sb("mixtm", [128, 1024], F32)
            else:
                rw = sb("rw_s", [128, 8, 8], F32)
                rbB = sb("rbB_s", [128, 8], F32)
                P.dma(rw[:], rw_d.rearrange("(k p) n -> p k n", p=128), writes=["rw"])
                P.dma(rbB[:], rbB_d[:, :], writes=["rbB"])
                mo = sb("mo", [128, 2048], F32); mg = sb("mg", [128, 2048], F32)
                hT32 = sb("hT32", [128, 8, 128], F32)
                lg = sb("lg", [128, 8], F32); m8 = sb("m8", [128, 8], F32); cwt = sb("cwt", [128, GT, 8], F32)
                nm1 = sb("nm1", [128, 1], F32); den = sb("den", [128, 1], F32)
        if pre:
            modTq = sb("modTq_s", [128, 2, 6, 8], F32)
            P.dma(modTq[:], modTq_d[:, :, :, :], writes=["modTq"])
            P.op("dve", lambda e: e.tensor_scalar_add(out=modTq[:, :, 1, :], in0=modTq[:, :, 1, :], scalar1=1.0), reads=["modTq"], writes=["modTq"])
            po = [sb("po%d" % i, [128, 512], F32) for i in range(1)]

        wctr = [0]
        pbc = [0]
        cast_rr = [0]

        def load_w(src_ap, n_free):
            i = wctr[0]; wctr[0] += 1
            b = i % 5
            dview = wbf[b][:, 0:n_free]
            if len(src_ap.shape) == 3:
                dview = dview.rearrange("p (a b) -> p a b", b=src_ap.shape[2])
            P.dma(dview, src_ap, writes=[("wbf", b)], q="pool")
            return wbf[b], ("wbf", b)

        def rsqrt_(dst, src, eps, rkeys, mul=1.0):
            P.op("dve", lambda e: e.tensor_scalar(out=dst, in0=src, scalar1=mul, scalar2=eps, op0=ALU.mult, op1=ALU.add), reads=rkeys, writes=["rstd"])
            P.op("act", lambda e: e.activation(out=dst, in_=dst, func=AF.Sqrt), reads=["rstd"], writes=["rstd"])
            P.op("dve", lambda e: e.reciprocal(out=dst, in_=dst), reads=["rstd"], writes=["rstd"])

        def layer_norm(Xt, xkey, li):
            for c in range(2):
                P.op("dve", lambda e, c=c: e.bn_stats(out=stats[:, c * 6:(c + 1) * 6], in_=Xt[:, c * 512:(c + 1) * 512]), reads=[xkey], writes=["stats"])
            P.op("dve", lambda e: e.bn_aggr(out=mv[:, 0, :], in_=stats[:, 0:12]), reads=["stats"], writes=["mv"])
            rsqrt_(rstd[:, 0:1], mv[:, 0, 1:2], LN_EPS, ["mv"])
            P.op("dve", lambda e: e.tensor_scalar(out=Xt, in0=Xt, scalar1=mv[:, 0, 0:1], scalar2=rstd[:, 0:1], op0=ALU.subtract, op1=ALU.mult), reads=[xkey, "mv", "rstd"], writes=[xkey])
            P.op("dve", lambda e: e.tensor_tensor(out=Xt, in0=Xt, in1=lnGB[:, li, 0, :], op=ALU.mult), reads=[xkey, "lnGB"], writes=[xkey])
            P.op("dve", lambda e: e.tensor_tensor(out=Xt, in0=Xt, in1=lnGB[:, li, 1, :], op=ALU.add), reads=[xkey, "lnGB"], writes=[xkey])

        def make_hT(i, tset, modT, mkey_, isc, ish, want32=False):
            for k in range(8):
                tb = k % 2
                P.op("pe", lambda e, k=k, tb=tb: e.transpose(out=tr[tb][:, 0:128], in_=X[:, i, k * 128:(k + 1) * 128], identity=ident[:]),
                     reads=[("X", i), "ident"], writes=[("tr", tb)])
                P.op("act", lambda e, k=k, tb=tb: e.activation(out=hT[:, k, i * 128:(i + 1) * 128], in_=tr[tb][:, 0:128], func=AF.Identity,
                                                             scale=modT[:, tset, isc, k:k + 1], bias=modT[:, tset, ish, k:k + 1]),
                     reads=[("tr", tb), mkey_], writes=[("hT", i)])
                if want32:
                    P.op("act", lambda e, k=k, tb=tb: e.activation(out=hT32[:, k, :], in_=tr[tb][:, 0:128], func=AF.Identity,
                                                                 scale=modT[:, tset, isc, k:k + 1], bias=modT[:, tset, ish, k:k + 1]),
                         reads=[("tr", tb), mkey_], writes=["hT32"])

        for g, tiles in enumerate(groups):
            nt = len(tiles); T = nt * 128
            for i, t in enumerate(tiles):
                P.dma(X[:, i, :], x_d[t * 128:(t + 1) * 128, :], writes=[("X", i)])
            if post:
                wv = wout_d.rearrange("(c p) n -> p c n", p=128)
                for c0 in range(0, CM, 4):
                    P.dma(woutb[:, c0:c0 + 4, :], wv[:, c0:c0 + 4, :], writes=["woutb"], q="pool")
                for i, t in enumerate(tiles):
                    tset = 1 if t == ntiles - 1 else 0
                    rows = slice(t * 128, (t + 1) * 128)
                    if post == "even":
                        for j, d in enumerate((ax_d, of_d, ob_d, gr_d)):
                            P.dma(mi[j][:], d[rows, :], writes=[("mi", j)], q="sync")
                        P.op("dve", lambda e: e.tensor_tensor(out=mi[1][:], in0=mi[1][:], in1=mi[2][:], op=ALU.add), reads=[("mi", 1), ("mi", 2)], writes=[("mi", 1)])
                        for h in range(4):
                            P.op("act", lambda e, h=h: e.activation(out=tmp[:, h * 128:(h + 1) * 128], in_=mi[1][:, h * 128:(h + 1) * 128], func=AF.Square, accum_out=rstd[:, h:h + 1]),
                                 reads=[("mi", 1)], writes=["tmp", "rstd"])
                        rsqrt_(rstd[:, 0:4], rstd[:, 0:4], NORM_EPS, ["rstd"], mul=1.0 / 128)
                        P.op("act", lambda e: e.activation(out=mi[3][:], in_=mi[3][:], func=AF.Silu), reads=[("mi", 3)], writes=[("mi", 3)])
                        P.op("dve", lambda e: e.tensor_copy(out=mixtm[:, 0:512], in_=mi[0][:]), reads=[("mi", 0)], writes=["mixtm"])
                        for h in range(4):
                            P.op("dve", lambda e, h=h: e.scalar_tensor_tensor(out=mixtm[:, 512 + h * 128:512 + (h + 1) * 128], in0=mi[1][:, h * 128:(h + 1) * 128], scalar=rstd[:, h:h + 1],
                                                                            in1=ngB[:, h * 128:(h + 1) * 128], op0=ALU.mult, op1=ALU.mult), reads=[("mi", 1), "rstd", "ngB"], writes=["mixtm"])
                        P.op("dve", lambda e: e.tensor_tensor(out=mixtm[:, 512:1024], in0=mixtm[:, 512:1024], in1=mi[3][:], op=ALU.mult), reads=["mixtm", ("mi", 3)], writes=["mixtm"])
                        msrc, mkey = mixtm, "mixtm"
                    else:
                        P.dma(mo[:], o_d[rows, :], writes=["mo"], q="sync")
                        P.dma(mg[:], gr_d[rows, :], writes=["mg"], q="sync")
                        for h in range(4):
                            P.op("dve", lambda e, h=h: e.bn_stats(out=stats[:, h * 6:(h + 1) * 6], in_=mo[:, h * 512:(h + 1) * 512]), reads=["mo"], writes=["stats"])
                            P.op("dve", lambda e, h=h: e.bn_aggr(out=mv[:, h, :], in_=stats[:, h * 6:(h + 1) * 6]), reads=["stats"], writes=["mv"])
                        rsqrt_(rstd[:, 0:4], mv[:, :, 1], LN_EPS, ["mv"])
                        P.op("act", lambda e: e.activation(out=mg[:], in_=mg[:], func=AF.Silu), reads=["mg"], writes=["mg"])
                        for h in range(4):
                            P.op("dve", lambda e, h=h: e.tensor_scalar(out=mo[:, h * 512:(h + 1) * 512], in0=mo[:, h * 512:(h + 1) * 512], scalar1=mv[:, h, 0:1], scalar2=rstd[:, h:h + 1],
                                                                     op0=ALU.subtract, op1=ALU.mult), reads=["mo", "mv", "rstd"], writes=["mo"])
                        P.op("dve", lambda e: e.tensor_tensor(out=mo[:], in0=mo[:], in1=mg[:], op=ALU.mult), reads=["mo", "mg"], writes=["mo"])
                        msrc, mkey = mo, "mo"
                    for c in range(CM):
                        tb = c % 2
                        P.op("pe", lambda e, c=c, tb=tb, msrc=msrc: e.transpose(out=tr[tb][:, 0:128], in_=msrc[:, c * 128:(c + 1) * 128], identity=ident[:]),
                             reads=[mkey, "ident"], writes=[("tr", tb)])
                        P.op("act", lambda e, c=c, tb=tb: e.activation(out=mixT[:, c, :], in_=tr[tb][:, 0:128], func=AF.Copy), reads=[("tr", tb)], writes=["mixT"])
                    for hf in range(2):
                        for c in range(CM):
                            P.op("pe", lambda e, c=c, hf=hf: e.matmul(yp[hf][:], lhsT=mixT[:, c, :], rhs=woutb[:, c, hf * 512:(hf + 1) * 512], start=(c == 0), stop=(c == CM - 1)),
                                 reads=["mixT", "woutb"], writes=[("yp", hf)])
                        P.op("dve", lambda e, hf=hf, tset=tset: e.tensor_tensor(out=tmp[:, hf * 512:(hf + 1) * 512], in0=yp[hf][:], in1=gateB[:, tset, 0, hf * 512:(hf + 1) * 512], op=ALU.mult),
                             reads=[("yp", hf), "gateB"], writes=["tmp"])
                    P.op("dve", lambda e, i=i: e.scalar_tensor_tensor(out=X[:, i, :], in0=X[:, i, :], scalar=ALPHA_C, in1=tmp[:], op0=ALU.mult, op1=ALU.add),
                         reads=[("X", i), "tmp"], writes=[("X", i)])
                    layer_norm(X[:, i, :], ("X", i), 0)
                    if DBG:
                        P.dma(dbg_d[t * 128:(t + 1) * 128, :], X[:, i, :], reads=[("X", i)], q="sync")
                for i, t in enumerate(tiles):
                    tset = 1 if t == ntiles - 1 else 0
                    make_hT(i, tset, modTp, "modTp", 4, 3, want32=(post == "odd" and 'no32' not in DBGMODE))
                    P.op("dve", lambda e, i=i: e.tensor_scalar_mul(out=X[:, i, :], in0=X[:, i, :], scalar1=ALPHA_C), reads=[("X", i)], writes=[("X", i)])
                    if post == "odd" and 'noroute' not in DBGMODE:
                        for k in range(8):
                            P.op("pe", lambda e, k=k: e.matmul(yp[0][:, 0:8], lhsT=hT32[:, k, :], rhs=rw[:, k, :], start=(k == 0), stop=(k == 7)), reads=["hT32", "rw"], writes=[("yp", 0)])
                        P.op("dve", lambda e: e.tensor_tensor(out=lg[:], in0=yp[0][:, 0:8], in1=rbB[:], op=ALU.add), reads=[("yp", 0), "rbB"], writes=["lg"])
                        P.op("dve", lambda e: e.max(out=m8[:], in_=lg[:]), reads=["lg"], writes=["m8"])
                        P.op("dve", lambda e: e.tensor_scalar_mul(out=nm1[:], in0=m8[:, 0:1], scalar1=-1.0), reads=["m8"], writes=["nm1"])
                        P.op("act", lambda e, i=i: e.activation(out=cwt[:, i, :], in_=lg[:], func=AF.Exp, bias=nm1[:, 0:1], scale=1.0), reads=["lg", "nm1"], writes=[("cw", i)])
                        P.op("dve", lambda e: e.tensor_scalar(out=lg[:], in0=lg[:], scalar1=m8[:, 1:2], scalar2=None, op0=ALU.is_ge), reads=["lg", "m8"], writes=["lg"])
                        P.op("dve", lambda e, i=i: e.tensor_tensor(out=cwt[:, i, :], in0=cwt[:, i, :], in1=lg[:], op=ALU.mult), reads=[("cw", i), "lg"], writes=[("cw", i)])
                        P.op("dve", lambda e, i=i: e.reduce_sum(out=den[:], in_=cwt[:, i, :], axis=AX.X), reads=[("cw", i)], writes=["den"])
                        P.op("dve", lambda e: e.reciprocal(out=den[:], in_=den[:]), reads=["den"], writes=["den"])
                        P.op("dve", lambda e, i=i: e.tensor_scalar_mul(out=cwt[:, i, :], in0=cwt[:, i, :], scalar1=den[:, 0:1]), reads=[("cw", i), "den"], writes=[("cw", i)])
                first = True
                for ex in range(0 if 'noffn' in DBGMODE else NE):
                    for f0 in range(0, FD, 512):
                        fb = min(512, FD - f0); nch = fb // 128
                        w1b, k1 = load_w(w1_d[ex].rearrange("(k p) n -> p k n", p=128)[:, :, f0:f0 + fb], 8 * fb)
                        w3b, k3 = load_w(w3_d[ex].rearrange("(k p) n -> p k n", p=128)[:, :, f0:f0 + fb], 8 * fb)
                        w2b, k2 = load_w(w2_d[ex][f0:f0 + fb, :].rearrange("(c p) n -> p c n", p=128), nch * 1024)
                        w1v = w1b[:, 0:8 * fb].rearrange("p (k n) -> p k n", n=fb)
                        w3v = w3b[:, 0:8 * fb].rearrange("p (k n) -> p k n", n=fb)
                        w2v = w2b[:, 0:nch * 1024].rearrange("p (c n) -> p c n", n=1024)
                        hkeys = [("hT", i) for i in range(nt)]
                        subs = [(s0, min(512, T - s0)) for s0 in range(0, T, 512)]
                        for j in range(nch):
                            for (s0, sn) in subs:
                                pb = pbc[0] % 2; pbc[0] += 1
                                for k in range(8):
                                    P.op("pe", lambda e, j=j, k=k, pb=pb, w1v=w1v, s0=s0, sn=sn: e.matmul(h1p[pb][:, 0:sn], lhsT=w1v[:, k, j * 128:(j + 1) * 128], rhs=hT[:, k, s0:s0 + sn], start=(k == 0), stop=(k == 7)),
                                         reads=[k1] + hkeys, writes=[("h1p", pb)])
                                for k in range(8):
                                    P.op("pe", lambda e, j=j, k=k, pb=pb, w3v=w3v, s0=s0, sn=sn: e.matmul(h3p[pb][:, 0:sn], lhsT=w3v[:, k, j * 128:(j + 1) * 128], rhs=hT[:, k, s0:s0 + sn], start=(k == 0), stop=(k == 7)),
                                         reads=[k3] + hkeys, writes=[("h3p", pb)])
                                P.op("act", lambda e, pb=pb, sn=sn: e.activation(out=s1[pb][:, 0:sn], in_=h1p[pb][:, 0:sn], func=AF.Silu), reads=[("h1p", pb)], writes=[("s1", pb)])
                                P.op("dve", lambda e, pb=pb, j=j, s0=s0, sn=sn: e.tensor_tensor(out=aT[:, j, s0:s0 + sn], in0=s1[pb][:, 0:sn], in1=h3p[pb][:, 0:sn], op=ALU.mult), reads=[("s1", pb), ("h3p", pb)], writes=[("aT", j)])
                        akeys = [("aT", j) for j in range(nch)]
                        if DBG and g == 0 and ex == 0 and f0 == 0:
                            P.op("dve", lambda e: e.tensor_copy(out=tmp[:, 0:512], in_=aT[:, 0, :]), reads=[("aT", 0)], writes=["tmp"])
                            P.op("dve", lambda e: e.tensor_copy(out=tmp[:, 512:1024], in_=hT[:, 0, :]), reads=[("hT", 0), ("hT", 1), ("hT", 2), ("hT", 3)], writes=["tmp"])
                            P.dma(dbg3_d[:, :], tmp[:], reads=["tmp"], q="sync")
                        for i in range(nt):
                            for hf in range(2):
                                for j in range(nch):
                                    P.op("pe", lambda e, i=i, hf=hf, j=j, w2v=w2v, nch=nch: e.matmul(yp[hf][:], lhsT=aT[:, j, i * 128:(i + 1) * 128], rhs=w2v[:, j, hf * 512:(hf + 1) * 512], start=(j == 0), stop=(j == nch - 1)),
                                         reads=[k2] + akeys, writes=[("yp", hf)])
                                ya = Yacc[:, i, hf * 512:(hf + 1) * 512]
                                if post == "odd":
                                    if first:
                                        P.op("dve", lambda e, hf=hf, ya=ya, i=i, ex=ex: e.tensor_scalar_mul(out=ya, in0=yp[hf][:], scalar1=cwt[:, i, ex:ex + 1]), reads=[("yp", hf), ("cw", i)], writes=[("Y", i)])
                                    else:
                                        P.op("dve", lambda e, hf=hf, ya=ya, i=i, ex=ex: e.scalar_tensor_tensor(out=ya, in0=yp[hf][:], scalar=cwt[:, i, ex:ex + 1], in1=ya, op0=ALU.mult, op1=ALU.add),
                                             reads=[("yp", hf), ("cw", i), ("Y", i)], writes=[("Y", i)])
                                else:
                                    if first:
                                        P.op("dve", lambda e, hf=hf, ya=ya: e.tensor_copy(out=ya, in_=yp[hf][:]), reads=[("yp", hf)], writes=[("Y", i)])
                                    else:
                                        P.op("dve", lambda e, hf=hf, ya=ya: e.tensor_tensor(out=ya, in0=yp[hf][:], in1=ya, op=ALU.add), reads=[("yp", hf), ("Y", i)], writes=[("Y", i)])
                        first = False
                for i, t in enumerate(tiles):
                    tset = 1 if t == ntiles - 1 else 0
                    if 'noffn' in DBGMODE:
                        P.op("dve", lambda e, i=i: e.memset(Yacc[:, i, :], 0.0), writes=[("Y", i)])
                    if DBG:
                        P.dma(dbg2_d[t * 128:(t + 1) * 128, :], Yacc[:, i, :], reads=[("Y", i)], q="sync")
                    P.op("dve", lambda e, i=i, tset=tset: e.tensor_tensor(out=Yacc[:, i, :], in0=Yacc[:, i, :], in1=gateB[:, tset, 1, :], op=ALU.mult), reads=[("Y", i), "gateB"], writes=[("Y", i)])
                    P.op("dve", lambda e, i=i: e.tensor_tensor(out=X[:, i, :], in0=X[:, i, :], in1=Yacc[:, i, :], op=ALU.add), reads=[("X", i), ("Y", i)], writes=[("X", i)])
                    layer_norm(X[:, i, :], ("X", i), 1)
                    P.dma(xo_d[t * 128:(t + 1) * 128, :], X[:, i, :], reads=[("X", i)], q="sync")
            if pre:
                for i, t in enumerate(tiles):
                    tset = 1 if t == ntiles - 1 else 0
                    make_hT(i, tset, modTq, "modTq", 1, 0)
                wv = win_d.rearrange("(k p) n -> p k n", p=128)
                pc = 0
                for n0 in range(0, NO, 512):
                    wb_, wk = load_w(wv[:, :, n0:n0 + 512], 4096)
                    wvv = wb_[:, :].rearrange("p (k n) -> p k n", n=512)
                    for i, t in enumerate(tiles):
                        hf = pc % 2; ob = 0; pc += 1
                        for k in range(8):
                            P.op("pe", lambda e, i=i, k=k, hf=hf, wvv=wvv: e.matmul(yp[hf][:], lhsT=hT[:, k, i * 128:(i + 1) * 128], rhs=wvv[:, k, :], start=(k == 0), stop=(k == 7)),
                                 reads=[wk, ("hT", i)], writes=[("yp", hf)])
                        P.op("act", lambda e, hf=hf, ob=ob: e.activation(out=po[ob][:], in_=yp[hf][:], func=AF.Copy), reads=[("yp", hf)], writes=[("po", ob)])
                        P.dma(proj_d[t * 128:(t + 1) * 128, n0:n0 + 512], po[ob][:], reads=[("po", ob)], q="sync")
        P.finish()
        P.emit()
    return nc


S_RET = 4352
NU_RET = 2


def ret_consts():
    f32 = np.float32
    ki = np.arange(128)[:, None]; qi = np.arange(512)[None, :]
    Mqk = (qi - ki).astype(f32)
    Rp = np.zeros((4, 128, 512), f32); Rn = np.zeros((4, 128, 512), f32); Eq = np.zeros((4, 128, 512), f32)
    for v in range(4):
        d = qi - ki - 128 * v
        Rp[v] = np.maximum(d, 0); Rn[v] = np.maximum(-d, 0); Eq[v] = (d == 0)
    strc = np.ascontiguousarray(np.stack([Rp, Rn, Eq], 0).transpose(2, 0, 1, 3))
    t = np.arange(4096); rows = t // 64; cols = t % 64
    jj = np.arange(128) % 64
    inv = (10000.0 ** (-(jj.astype(np.float64)) / 64))
    cosT = np.ones((2, 128, S_RET), f32); sinT = np.zeros((2, 128, S_RET), f32)
    for c, pos in enumerate((rows, cols)):
        ang = (pos[None, :].astype(np.float32) * inv.astype(np.float32)[:, None]).astype(np.float32)
        cosT[c, :, 256:] = np.cos(ang)
        sgn = np.where(np.arange(128) < 64, -1.0, 1.0)[:, None]
        sinT[c, :, 256:] = np.sin(ang) * sgn
    n128 = (128.0 * np.arange(48)).astype(f32)
    return {"Mqk": Mqk, "strc": strc, "cosT": cosT, "sinT": sinT,
            "n128": np.ascontiguousarray(np.broadcast_to(n128[None], (128, 48)))}


def build_ret():
    nc = bass.Bass("TRN2", target_bir_lowering=False)
    din = lambda n, s: nc.dram_tensor(n, s, F32, kind="ExternalInput").ap()
    S = S_RET
    qT_d = din("qT", [NU_RET, 2, 128, S]); qsT_d = din("qsT", [NU_RET, 2, 128, S])
    kT_d = din("kT", [NU_RET, 2, 128, S]); ksT_d = din("ksT", [NU_RET, 2, 128, S])
    v_d = din("v", [NU_RET, S, 512]); ld_d = din("ld", [NU_RET, 2])
    Mqk_d = din("Mqk", [128, 512]); strc_d = din("strc", [128, 3, 4, 512])
    cosT_d = din("cosT", [2, 128, S]); sinT_d = din("sinT", [2, 128, S]); n128_d = din("n128", [128, 48])
    o_d = nc.dram_tensor("o", [NU_RET, S, 512], F32, kind="ExternalOutput").ap()
    NTK = S // 128
    with ExitStack() as st:
        sb = lambda n, s, d: st.enter_context(nc.sbuf_tensor(n, s, d))
        pst = lambda n, s: st.enter_context(nc.psum_tensor(n, s, F32))
        P = Prog(nc)
        Mqk = sb("Mqk_s", [128, 512], F32); strc = sb("strc_s", [128, 3, 4, 512], F32); n128 = sb("n128_s", [128, 48], F32)
        P.dma(Mqk[:], Mqk_d[:, :], writes=["Mqk"]); P.dma(strc[:], strc_d[:, :, :, :], writes=["strc"]); P.dma(n128[:], n128_d[:, :], writes=["n128"])
        qr = sb("qr", [128, 2, S], BF16); kr = sb("kr", [128, 2, S], BF16); vb = sb("vb", [128, NTK, 512], BF16)
        CH = 1088
        ra = sb("ra", [128, CH], F32); rb_ = sb("rb", [128, CH], F32); rc = sb("rc", [128, CH], F32); rd = sb("rd", [128, CH], F32)
        vst = [sb("vst%d" % i, [128, 512], F32) for i in range(2)]
        ld = sb("ld_s", [128, 2], F32); nld = sb("nld", [128, 2], F32)
        bF = sb("bF", [128, 48], F32); bB = sb("bB", [128, 48], F32)
        Dstr = sb("Dstr", [128, 4, 512], F32); e1 = sb("e1", [128, 512], F32); e2 = sb("e2", [128, 512], F32)
        mk = [sb("mk%d" % i, [128, 512], F32) for i in range(2)]; mk2 = [sb("mk2_%d" % i, [128, 512], F32) for i in range(2)]
        At = [sb("At%d" % i, [128, 512], BF16) for i in range(2)]
        ot = [sb("ot%d" % i, [128, 512], F32) for i in range(2)]
        sp = [pst("sp%d" % i, [128, 512]) for i in range(2)]
        op_ = [pst("op%d" % i, [128, 512]) for i in range(4)]
        octr = [0]
        for u in range(NU_RET):
            P.dma(ld[:], ld_d[u:u + 1, :].to_broadcast([128, 2]), writes=["ld"])
            P.op("dve", lambda e: e.tensor_scalar(out=nld[:], in0=ld[:], scalar1=-1.0, scalar2=None, op0=ALU.mult), reads=["ld"], writes=["nld"])
            P.op("dve", lambda e: e.tensor_scalar(out=bF[:], in0=n128[:], scalar1=ld[:, 0:1], scalar2=None, op0=ALU.mult), reads=["ld", "n128"], writes=["bF"])
            P.op("dve", lambda e: e.tensor_scalar(out=bB[:], in0=n128[:], scalar1=ld[:, 1:2], scalar2=None, op0=ALU.mult), reads=["ld", "n128"], writes=["bB"])
            for v in range(4):
                P.op("act", lambda e, v=v: e.activation(out=e1[:], in_=strc[:, 0, v, :], func=AF.Exp, scale=ld[:, 0:1]), reads=["strc", "ld"], writes=["e1"])
                P.op("act", lambda e, v=v: e.activation(out=e2[:], in_=strc[:, 1, v, :], func=AF.Exp, scale=ld[:, 1:2]), reads=["strc", "ld"], writes=["e2"])
                P.op("dve", lambda e, v=v: e.tensor_tensor(out=Dstr[:, v, :], in0=e1[:], in1=e2[:], op=ALU.mult), reads=["e1", "e2"], writes=["Dstr"])
                P.op("dve", lambda e, v=v: e.tensor_tensor(out=Dstr[:, v, :], in0=Dstr[:, v, :], in1=strc[:, 2, v, :], op=ALU.add), reads=["Dstr", "strc"], writes=["Dstr"])
            for (src, ssrc, dst, dkey) in ((qT_d, qsT_d, qr, "qr"), (kT_d, ksT_d, kr, "kr")):
                for c in range(2):
                    for t0 in range(0, S, CH):
                        P.dma(ra[:], src[u, c, :, t0:t0 + CH], writes=["ra"], q="sync")
                        P.dma(rb_[:], ssrc[u, c, :, t0:t0 + CH], writes=["rb"], q="pool")
                        P.dma(rc[:], cosT_d[c, :, t0:t0 + CH], writes=["rc"], q="sync")
                        P.dma(rd[:], sinT_d[c, :, t0:t0 + CH], writes=["rd"], q="pool")
                        P.op("dve", lambda e: e.tensor_tensor(out=ra[:], in0=ra[:], in1=rc[:], op=ALU.mult), reads=["ra", "rc"], writes=["ra"])
                        P.op("pool", lambda e: e.tensor_tensor(out=rb_[:], in0=rb_[:], in1=rd[:], op=ALU.mult), reads=["rb", "rd"], writes=["rb"])
                        P.op("dve", lambda e, dst=dst, c=c, t0=t0: e.tensor_tensor(out=dst[:, c, t0:t0 + CH], in0=ra[:], in1=rb_[:], op=ALU.add), reads=["ra", "rb"], writes=[dkey])
            for t in range(NTK):
                s_ = t % 2
                P.dma(vst[s_][:], v_d[u, t * 128:(t + 1) * 128, :], writes=[("vst", s_)], q="sync" if s_ == 0 else "pool")
                P.op("act", lambda e, t=t, s_=s_: e.activation(out=vb[:, t, :], in_=vst[s_][:], func=AF.Copy), reads=[("vst", s_)], writes=["vb"])
            blocks = [(0, 256, True)] + [(256 + 512 * i, 512, False) for i in range(8)]
            bc = 0
            for (q0, nq, isctx) in blocks:
                nsub = nq // 128
                ktiles = [0, 1] if isctx else list(range(NTK))
                for ki_, kt in enumerate(ktiles):
                    pb = bc % 2; bc += 1
                    for c in range(2):
                        P.op("pe", lambda e, c=c, kt=kt, q0=q0, nq=nq, pb=pb: e.matmul(sp[pb][:, 0:nq], lhsT=kr[:, c, kt * 128:(kt + 1) * 128], rhs=qr[:, c, q0:q0 + nq], start=(c == 0), stop=(c == 1)),
                             reads=["qr", "kr"], writes=[("sp", pb)])
                    if isctx:
                        msk = Dstr[:, kt, 0:nq]; mkeys = ["Dstr"]
                    else:
                        if kt < 2:
                            qx0 = q0 - 256
                            nf = (qx0 - (kt * 128 - 256)) // 128
                            nb = ((4096 + kt * 128) - qx0) // 128
                            P.op("act", lambda e, pb=pb, nf=nf: e.activation(out=mk[pb][:], in_=Mqk[:], func=AF.Exp, scale=ld[:, 0:1], bias=bF[:, nf:nf + 1]), reads=["Mqk", "ld", "bF"], writes=[("mk", pb)])
                            P.op("act", lambda e, pb=pb, nb=nb: e.activation(out=mk2[pb][:], in_=Mqk[:], func=AF.Exp, scale=nld[:, 1:2], bias=bB[:, nb:nb + 1]), reads=["Mqk", "nld", "bB"], writes=[("mk2", pb)])
                            P.op("pool", lambda e, pb=pb: e.tensor_tensor(out=mk[pb][:], in0=mk[pb][:], in1=mk2[pb][:], op=ALU.add), reads=[("mk", pb), ("mk2", pb)], writes=[("mk", pb)])
                            msk = mk[pb][:, :]; mkeys = [("mk", pb)]
                        else:
                            k0 = kt * 128
                            if q0 >= k0 + 128:
                                n = (q0 - k0) // 128
                                P.op("act", lambda e, pb=pb, n=n: e.activation(out=mk[pb][:], in_=Mqk[:], func=AF.Exp, scale=ld[:, 0:1], bias=bF[:, n:n + 1]), reads=["Mqk", "ld", "bF"], writes=[("mk", pb)])
                                msk = mk[pb][:, :]; mkeys = [("mk", pb)]
                            elif q0 + 512 <= k0:
                                n = (k0 - q0) // 128
                                P.op("act", lambda e, pb=pb, n=n: e.activation(out=mk[pb][:], in_=Mqk[:], func=AF.Exp, scale=nld[:, 1:2], bias=bB[:, n:n + 1]), reads=["Mqk", "nld", "bB"], writes=[("mk", pb)])
                                msk = mk[pb][:, :]; mkeys = [("mk", pb)]
                            else:
                                v = (k0 - q0) // 128
                                msk = Dstr[:, v, :]; mkeys = ["Dstr"]
                    P.op("dve", lambda e, pb=pb, nq=nq, msk=msk: e.scalar_tensor_tensor(out=At[pb][:, 0:nq], in0=sp[pb][:, 0:nq], scalar=1.0 / 16, in1=msk, op0=ALU.mult, op1=ALU.mult),
                         reads=[("sp", pb)] + mkeys, writes=[("At", pb)])
                    for s in range(nsub):
                        P.op("pe", lambda e, s=s, pb=pb, kt=kt, first=(ki_ == 0), last=(ki_ == len(ktiles) - 1): e.matmul(op_[s][:], lhsT=At[pb][:, s * 128:(s + 1) * 128], rhs=vb[:, kt, :], start=first, stop=last),
                             reads=[("At", pb), "vb"], writes=[("op", s)])
                for s in range(nsub):
                    ob = octr[0] % 2; octr[0] += 1
                    P.op("act", lambda e, s=s, ob=ob: e.activation(out=ot[ob][:], in_=op_[s][:], func=AF.Copy), reads=[("op", s)], writes=[("ot", ob)])
                    P.dma(o_d[u, q0 + s * 128:q0 + (s + 1) * 128, :], ot[ob][:], reads=[("ot", ob)], q="sync")
        P.finish(); P.emit()
    return nc


def ret_ref(q, k, v, ld):
    S = q.shape[0]
    A = (q.astype(np.float64) @ k.astype(np.float64).T) / 16
    pos_f = np.concatenate([np.arange(256) - 256, np.arange(4096)]).astype(np.float64)
    pos_b = np.concatenate([np.arange(256) + 4096, np.arange(4096)]).astype(np.float64)
    df = pos_f[:, None] - pos_f[None, :]
    db = pos_b[None, :] - pos_b[:, None]
    D = np.where(df >= 0, np.exp(ld[0] * np.maximum(df, 0)), 0) + np.where(db >= 0, np.exp(ld[1] * np.maximum(db, 0)), 0)
    D[:256, 256:] = 0
    return (A * D) @ v.astype(np.float64)


NU_NA = 4
S_NA = 4352
NEG = -30000.0


def na_variants():
    var_of = {}; reps = []
    for I in range(8):
        jlo, jhi = max(0, 4 * I - 2), min(31, 4 * I + 5)
        for j in range(jlo, jhi + 1):
            if I == 0:
                key = ("a", j)
            elif I == 7:
                key = ("z", j)
            else:
                key = ("m", j - 4 * I)
            if key not in var_of:
                var_of[key] = len(reps); reps.append((I, j))
    return var_of, reps


def na_tables(rpb_h):
    var_of, reps = na_variants()
    kp = np.arange(128); q = np.arange(512)
    a = kp // 64; kc = kp % 64; m = q // 64; qc = q % 64
    Bg = np.zeros((128, len(reps), 512), np.float32); M = np.zeros((128, len(reps), 512), np.float32)
    c_start = np.clip(qc - 8, 0, 48)
    for vi, (I, j) in enumerate(reps):
        kr = (2 * j + a)[:, None]; qr = (8 * I + m)[None, :]
        s = np.clip(qr - 4, 0, 56)
        rowok = (kr >= s) & (kr < s + 8)
        colok = (kc[:, None] >= c_start[None, :]) & (kc[:, None] < c_start[None, :] + 16)
        ok = rowok & colok
        dr = np.clip(kr - qr + 7, 0, 14); dc = np.clip(kc[:, None] - qc[None, :], -15, 15) + 15
        Bg[:, vi, :] = rpb_h[dr, dc]
        M[:, vi, :] = np.where(ok, 0.0, NEG)
    return Bg, M


def build_na():
    nc = bass.Bass("TRN2", target_bir_lowering=False)
    din = lambda n, s: nc.dram_tensor(n, s, F32, kind="ExternalInput").ap()
    S = S_NA
    qT_d = din("qT", [NU_NA, 64, S]); kT_d = din("kT", [NU_NA, 64, S]); v_d = din("v", [NU_NA, S, 64])
    Bg_d = din("Bg", [128, 20, 512]); M_d = din("M", [128, 20, 512])
    o_d = nc.dram_tensor("o", [NU_NA, S, 64], F32, kind="ExternalOutput").ap()
    var_of, reps = na_variants()
    with ExitStack() as st:
        sb = lambda n, s, d: st.enter_context(nc.sbuf_tensor(n, s, d))
        pst = lambda n, s: st.enter_context(nc.psum_tensor(n, s, F32))
        P = Prog(nc)
        Bt = sb("Bt", [128, 20, 512], F32)
        stg = [sb("stg%d" % i, [128, 2560], F32) for i in range(2)]
        for c in range(4):
            P.dma(Bt[:, c * 5:(c + 1) * 5, :], Bg_d[:, c * 5:(c + 1) * 5, :], writes=[("Bt", c)], q="sync")
            P.dma(stg[c % 2][:, :].rearrange("p (a b) -> p a b", b=512), M_d[:, c * 5:(c + 1) * 5, :], writes=[("stg", c % 2)], q="pool")
            P.op("dve", lambda e, c=c: e.tensor_tensor(out=Bt[:, c * 5:(c + 1) * 5, :], in0=Bt[:, c * 5:(c + 1) * 5, :], in1=stg[c % 2][:, :].rearrange("p (a b) -> p a b", b=512), op=ALU.add),
                 reads=[("Bt", c), ("stg", c % 2)], writes=[("Bt", c)])
        Bkeys = [("Bt", c) for c in range(4)]
        qb = sb("qb", [64, S], BF16); kb = sb("kb", [64, S], BF16); va = sb("va", [128, 34, 65], BF16)
        vst = sb("vstg", [128, 34, 64], F32)
        sbt = [sb("sbt%d" % i, [128, 512], F32) for i in range(2)]
        Et = [sb("Et%d" % i, [128, 512], BF16) for i in range(2)]
        rden = sb("rden", [128, 1], F32); ot = [sb("ot%d" % i, [128, 64], F32) for i in range(2)]
        sp = [pst("sp%d" % i, [128, 512]) for i in range(2)]
        op_ = [pst("op%d" % i, [128, 512]) for i in range(4)]
        P.op("pool", lambda e: e.memset(va[:, :, 64:65], 1.0), writes=["va"])
        octr = [0]; bc = 0
        for u in range(NU_NA):
            for (src, dst, dk) in ((qT_d, qb, "qb"), (kT_d, kb, "kb")):
                for h2 in range(2):
                    s_ = h2
                    P.dma(stg[s_][0:64, 0:2176], src[u, :, h2 * 2176:(h2 + 1) * 2176], writes=[("stg", s_)], q="sync" if h2 == 0 else "pool")
                    P.op("act", lambda e, dst=dst, h2=h2, s_=s_: e.activation(out=dst[:, h2 * 2176:(h2 + 1) * 2176], in_=stg[s_][0:64, 0:2176], func=AF.Copy), reads=[("stg", s_)], writes=[dk])
            P.dma(vst[:], v_d[u].rearrange("(t p) d -> p t d", p=128), writes=["vst"], q="sync")
            P.op("dve", lambda e: e.tensor_copy(out=va[:, :, 0:64], in_=vst[:]), reads=["vst"], writes=["va"])
            blocks = [("C", 0, 256)] + [(I, 256 + 512 * I, 512) for I in range(8)]
            for (I, q0, nq) in blocks:
                nsub = nq // 128
                if I == "C":
                    kts = [(0, None), (1, None)]
                else:
                    kts = [(0, None), (1, None)]
                    for j in range(max(0, 4 * I - 2), min(31, 4 * I + 5) + 1):
                        key = ("a", j) if I == 0 else (("z", j) if I == 7 else ("m", j - 4 * I))
                        kts.append((2 + j, var_of[key]))
                for ki_, (kt, vi) in enumerate(kts):
                    pb = bc % 2; bc += 1
                    P.op("pe", lambda e, kt=kt, q0=q0, nq=nq, pb=pb: e.matmul(sp[pb][:, 0:nq], lhsT=kb[:, kt * 128:(kt + 1) * 128], rhs=qb[:, q0:q0 + nq], start=True, stop=True),
                         reads=["qb", "kb"], writes=[("sp", pb)])
                    if vi is None:
                        P.op("act", lambda e, pb=pb, nq=nq: e.activation(out=Et[pb][:, 0:nq], in_=sp[pb][:, 0:nq], func=AF.Exp, scale=0.125), reads=[("sp", pb)], writes=[("Et", pb)])
                    else:
                        P.op("dve", lambda e, pb=pb, vi=vi: e.scalar_tensor_tensor(out=sbt[pb][:], in0=sp[pb][:], scalar=0.125, in1=Bt[:, vi, :], op0=ALU.mult, op1=ALU.add),
                             reads=[("sp", pb)] + Bkeys, writes=[("sbt", pb)])
                        P.op("act", lambda e, pb=pb: e.activation(out=Et[pb][:], in_=sbt[pb][:], func=AF.Exp), reads=[("sbt", pb)], writes=[("Et", pb)])
                    for s in range(nsub):
                        P.op("pe", lambda e, s=s, pb=pb, kt=kt, first=(ki_ == 0), last=(ki_ == len(kts) - 1): e.matmul(op_[s][:, 0:65], lhsT=Et[pb][:, s * 128:(s + 1) * 128], rhs=va[:, kt, :], start=first, stop=last),
                             reads=[("Et", pb), "va"], writes=[("op", s)])
                for s in range(nsub):
                    ob = octr[0] % 2; octr[0] += 1
                    P.op("dve", lambda e, s=s: e.reciprocal(out=rden[:], in_=op_[s][:, 64:65]), reads=[("op", s)], writes=["rden"])
                    P.op("dve", lambda e, s=s, ob=ob: e.tensor_scalar(out=ot[ob][:], in0=op_[s][:, 0:64], scalar1=rden[:, 0:1], scalar2=None, op0=ALU.mult), reads=[("op", s), "rden"], writes=[("ot", ob)])
                    P.dma(o_d[u, q0 + s * 128:q0 + (s + 1) * 128, :], ot[ob][:], reads=[("ot", ob)], q="sync")
        P.finish(); P.emit()
    return nc


def na_ref(q, k, v, qc, kc, vc, rpb_h):
    q = q.astype(np.float64) * 0.125; k = k.astype(np.float64); v = v.astype(np.float64)
    kc = kc.astype(np.float64); vc = vc.astype(np.float64)
    out = np.zeros((4096, 64))
    col = np.arange(64)
    for r in range(64):
        s = min(max(r - 4, 0), 56)
        keys = k[s * 64:(s + 8) * 64]; vals = v[s * 64:(s + 8) * 64]
        qq = q[r * 64:(r + 1) * 64]
        sc = qq @ keys.T
        kr = s + np.arange(8).repeat(64); kcol = np.tile(col, 8)
        cst = np.clip(col - 8, 0, 48)
        ok = (kcol[None, :] >= cst[:, None]) & (kcol[None, :] < cst[:, None] + 16)
        dr = kr - r + 7; dc = np.clip(kcol[None, :] - col[:, None], -15, 15) + 15
        sc = np.where(ok, sc + rpb_h[dr[None, :].repeat(64, 0), dc], -np.inf)
        scc = qq @ kc.T
        al = np.concatenate([sc, scc], 1); al = al - al.max(1, keepdims=True); p = np.exp(al); p /= p.sum(1, keepdims=True)
        out[r * 64:(r + 1) * 64] = p[:, :512] @ vals + p[:, 512:] @ vc
    sc = (qc.astype(np.float64) * 0.125) @ kc.T; sc -= sc.max(1, keepdims=True); p = np.exp(sc); p /= p.sum(1, keepdims=True)
    return out, p @ vc


import os
HGV = 2
NU_HG = 4
S_HG = 4352


def hg_consts():
    import ml_dtypes
    sel = np.zeros((128, 128, 128), np.float32)
    selT = np.zeros((128, 128, 128), np.float32)
    for vi in range(128):
        sel[vi, vi, :] = 1.0
        selT[:, vi, vi] = 1.0
    return {"sel": sel.reshape(128, 128 * 128), "selT": selT.reshape(128, 128 * 128)}


def build_hg(use_lb):
    nc = bass.Bass("TRN2", target_bir_lowering=False)
    din = lambda n, s: nc.dram_tensor(n, s, F32, kind="ExternalInput").ap()
    S = S_HG
    zq_d = din("zqT", [NU_HG, 128, S]); zf_d = din("zfT", [NU_HG, 128, S]); vT_d = din("vT", [NU_HG, 128, S]); lbl_d = din("lbl", [NU_HG, 128, 2])
    sel_d = din("sel", [128, 128 * 128]); selT_d = din("selT", [128, 128 * 128])
    o_d = nc.dram_tensor("oT", [NU_HG, 128, S], F32, kind="ExternalOutput").ap()
    with ExitStack() as st:
        sb = lambda n, s, d: st.enter_context(nc.sbuf_tensor(n, s, d))
        pst = lambda n, s: st.enter_context(nc.psum_tensor(n, s, F32))
        P = Prog(nc)
        sel = sb("sel_s", [128, 128, 128], BF16); selT = sb("selT_s", [128, 128, 128], BF16)
        stg = sb("stg", [128, 4352], F32)
        for (src, dst, dk) in ((sel_d, sel, "sel"), (selT_d, selT, "selT")):
            for c in range(4):
                P.dma(stg[:, 0:4096], src[:, c * 4096:(c + 1) * 4096], writes=["stg"], q="sync")
                P.op("dve", lambda e, dst=dst, c=c: e.tensor_copy(out=dst[:, c * 32:(c + 1) * 32, :], in_=stg[:, 0:4096].rearrange("p (a b) -> p a b", b=128)), reads=["stg"], writes=[dk])
        ft = sb("ft", [128, S], F32); kt_ = sb("kt", [128, S], F32); qt = sb("qt", [128, S], F32); vTb = sb("vTb", [128, S], BF16)
        lbl = sb("lbl_s", [128, 2], F32); lb = sb("lb", [128, 1], F32); oml = sb("oml", [128, 1], F32)
        state = sb("state", [128, 128], F32)
        NB = 6; NBP = 4
        vbs = [sb("vbs_%d" % i, [128, 512], F32) for i in range(NB)]
        d1 = [sb("d1_%d" % i, [128, 512], F32) for i in range(NB)]
        Sv = [sb("Sv_%d" % i, [128, 512], F32) for i in range(NB)]
        qs = [sb("qs_%d" % i, [128, 512], BF16) for i in range(NB)]
        ot = [sb("ot_%d" % i, [128, 512], F32) for i in range(2)]
        vbp = [pst("vbp%d" % i, [128, 512]) for i in range(NBP)]
        opp = [pst("opp%d" % i, [128, 512]) for i in range(2)]
        cc = 0
        for u in range(NU_HG):
            if use_lb:
                P.dma(lbl[:], lbl_d[u, :, :], writes=["lbl"])
                P.op("dve", lambda e: e.tensor_tensor(out=lb[:], in0=lbl[:, 1:2], in1=lbl[:, 0:1], op=ALU.subtract), reads=["lbl"], writes=["lb"])
                P.op("act", lambda e: e.activation(out=lb[:], in_=lb[:], func=AF.Sigmoid), reads=["lb"], writes=["lb"])
                P.op("dve", lambda e: e.tensor_scalar(out=oml[:], in0=lb[:], scalar1=-1.0, scalar2=1.0, op0=ALU.mult, op1=ALU.add), reads=["lb"], writes=["oml"])
            P.dma(stg[:], zf_d[u, :, :], writes=["stg"], q="sync")
            P.op("act", lambda e: e.activation(out=ft[:], in_=stg[:], func=AF.Sigmoid), reads=["stg"], writes=["ft"])
            if use_lb:
                P.op("dve", lambda e: e.tensor_scalar(out=ft[:], in0=ft[:], scalar1=oml[:, 0:1], scalar2=lb[:, 0:1], op0=ALU.mult, op1=ALU.add), reads=["ft", "oml", "lb"], writes=["ft"])
            P.op("dve", lambda e: e.tensor_scalar(out=kt_[:], in0=ft[:], scalar1=-1.0, scalar2=1.0, op0=ALU.mult, op1=ALU.add), reads=["ft"], writes=["kt"])
            P.dma(stg[:], zq_d[u, :, :], writes=["stg"], q="sync")
            P.op("act", lambda e: e.activation(out=qt[:], in_=stg[:], func=AF.Silu), reads=["stg"], writes=["qt"])
            P.dma(vTb[:], vT_d[u, :, :], writes=["vTb"], q="pool")
            P.op("pool", lambda e: e.memset(state[:], 0.0), writes=[("state", vi) for vi in range(128)])
            items = []
            for t0 in range(0, S, 512):
                n = min(512, S - t0)
                ob = cc % 2; cc += 1
                for vi in range(128):
                    items.append((t0, n, ob, vi))
            G = len(items)

            def stage(k, g):
                t0, n, ob, vi = items[g]
                b = g % NB; pb = g % NBP
                if k == 0:
                    P.op("pe", lambda e: e.matmul(vbp[pb][:, 0:n], lhsT=sel[:, vi, :], rhs=vTb[:, t0:t0 + n], start=True, stop=True), reads=["sel", "vTb"], writes=[("vbp", pb)])
                elif k == 1:
                    if HGV != 2:
                        P.op("act", lambda e: e.activation(out=vbs[b][:, 0:n], in_=vbp[pb][:, 0:n], func=AF.Copy), reads=[("vbp", pb)], writes=[("vbs", b)])
                elif k == 2:
                    if HGV != 2:
                        P.op("pool", lambda e: e.tensor_tensor(out=d1[b][:, 0:n], in0=kt_[:, t0:t0 + n], in1=vbs[b][:, 0:n], op=ALU.mult), reads=["kt", ("vbs", b)], writes=[("d1", b)])
                    else:
                        P.op("dve", lambda e: e.tensor_tensor(out=d1[b][:, 0:n], in0=kt_[:, t0:t0 + n], in1=vbp[pb][:, 0:n], op=ALU.mult), reads=["kt", ("vbp", pb)], writes=[("d1", b)])
                elif k == 3:
                    P.op("dve", lambda e: e.tensor_tensor_scan(out=Sv[b][:, 0:n], data0=ft[:, t0:t0 + n], data1=d1[b][:, 0:n], initial=state[:, vi:vi + 1], op0=ALU.mult, op1=ALU.add),
                         reads=["ft", ("d1", b), ("state", vi)], writes=[("Sv", b)])
                elif k == 4:
                    P.op("act", lambda e: e.activation(out=state[:, vi:vi + 1], in_=Sv[b][:, n - 1:n], func=AF.Copy), reads=[("Sv", b)], writes=[("state", vi)])
                    qeng = ("pool" if vi % 3 == 0 else "dve") if HGV == 0 else ("dve" if HGV == 1 else "pool")
                    P.op(qeng, lambda e: e.tensor_tensor(out=qs[b][:, 0:n], in0=qt[:, t0:t0 + n], in1=Sv[b][:, 0:n], op=ALU.mult), reads=["qt", ("Sv", b)], writes=[("qs", b)])
                elif k == 5:
                    P.op("pe", lambda e: e.matmul(opp[ob][:, 0:n], lhsT=selT[:, vi, :], rhs=qs[b][:, 0:n], start=(vi == 0), stop=(vi == 127)), reads=["selT", ("qs", b)], writes=[("opp", ob)])
                    if vi == 127:
                        P.op("act", lambda e: e.activation(out=ot[ob][:, 0:n], in_=opp[ob][:, 0:n], func=AF.Copy), reads=[("opp", ob)], writes=[("ot", ob)])
                        P.dma(o_d[u, :, t0:t0 + n], ot[ob][:, 0:n], reads=[("ot", ob)], q="sync")

            for step in range(G + 5):
                for k in range(6):
                    g = step - k
                    if 0 <= g < G:
                        stage(k, g)
        P.finish(); P.emit()
    return nc


def hg_ref(zq, zf, v, lb):
    zq = zq.astype(np.float64); zf = zf.astype(np.float64); v = v.astype(np.float64)
    q = zq / (1 + np.exp(-zq)); f = lb + (1 - lb) / (1 + np.exp(-zf)); k = 1 - f
    St = np.zeros((128, 128)); o = np.zeros((zq.shape[0], 128))
    for t in range(zq.shape[0]):
        St = f[t][:, None] * St + k[t][:, None] * v[t][None, :]
        o[t] = q[t] @ St
    return o


_CACHE = {}


def _prog(key, fn):
    if key not in _CACHE:
        _CACHE[key] = fn()
    return _CACHE[key]


def _bc(a, shape):
    return np.ascontiguousarray(np.broadcast_to(a, shape)).astype(np.float32)


def _rows_core(xf, cf, c):
    b, h = c // 2, c % 2
    return np.ascontiguousarray(np.concatenate([xf[b, h * 2048:(h + 1) * 2048], cf[b, h * 128:(h + 1) * 128]], 0))


def _unrows(outs, W):
    xf = np.zeros((4, 4096, W), np.float32); cf = np.zeros((4, 256, W), np.float32)
    for c in range(8):
        b, h = c // 2, c % 2
        xf[b, h * 2048:(h + 1) * 2048] = outs[c][:2048]
        cf[b, h * 128:(h + 1) * 128] = outs[c][2048:]
    return xf, cf


def _modT(m, l, b):
    mods = np.stack([m[l, b].reshape(6, 1024), m[l, 4].reshape(6, 1024)], 0)
    modT = np.ascontiguousarray(mods.reshape(2, 6, 8, 128).transpose(3, 0, 1, 2))
    gateB = _bc(mods[:, [2, 5], :][None], (128, 2, 2, 1024))
    return modT, gateB


def _run(nc, maps):
    res = run_bass_kernel_spmd(nc, maps, core_ids=list(range(8)))
    return res.results


def kernel(x, c, ctx, c_ctx, ada_w, ada_b, ln_g, ln_b, e_w_in, e_w_out, na_rpb, hg_lb_logits, hg_norm_g,
           ffn_w1, ffn_w3, ffn_w2, o_w_in, o_w_out, ret_log_decay, router_w, router_b, moe_w1, moe_w3, moe_w2):
    f32 = np.float32
    A = lambda a: np.ascontiguousarray(np.asarray(a, dtype=f32))
    x = A(x); ctx = A(ctx)
    ident = np.eye(128, dtype=f32)
    nc_ada = _prog("ada", build_ada)
    cc = np.concatenate([A(c), A(c_ctx)[None]], 0)
    cT = np.ascontiguousarray(cc.T.reshape(8, 128, 5).transpose(1, 0, 2))
    maps = []
    for core in range(8):
        l, h = core // 2, core % 2
        maps.append({"cT": cT, "w": A(ada_w[l][:, h * 3072:(h + 1) * 3072]), "b": A(ada_b[l][None, h * 3072:(h + 1) * 3072])})
    r = _run(nc_ada, maps)
    m = np.zeros((4, 5, 6144), f32)
    for core in range(8):
        l, h = core // 2, core % 2
        m[l][:, h * 3072:(h + 1) * 3072] = r[core]["m"]

    def dense(post, pre, l_post, l_pre, xf, cf, extra):
        nc = _prog(("dense", post, pre), lambda: build_dense(post, pre))
        maps = []
        for core in range(8):
            b = core // 2
            d = {"x": _rows_core(xf, cf, core), "ident": ident}
            if post:
                modT, gateB = _modT(m, l_post, b)
                d["modTp"] = modT; d["gateB"] = gateB
                d["lnGB"] = _bc(np.stack([A(ln_g[l_post]), A(ln_b[l_post])], 1)[None], (128, 2, 2, 1024))
                for k_, v_ in extra.items():
                    if isinstance(v_, tuple):
                        d[k_] = _rows_core(v_[0], v_[1], core)
                    else:
                        d[k_] = v_
            if pre:
                modT, _ = _modT(m, l_pre, b)
                d["modTq"] = modT
                d["win"] = A(e_w_in[l_pre // 2]) if pre == "even" else A(o_w_in[l_pre // 2])
            maps.append(d)
        r = _run(nc, maps)
        xo = co = px = pc = None
        if post:
            xo, co = _unrows([r[c_]["xo"] for c_ in range(8)], 1024)
        if pre:
            NO = 4096 if pre == "even" else 6144
            px, pc = _unrows([r[c_]["proj"] for c_ in range(8)], NO)
        return xo, co, px, pc

    _, _, px, pc = dense(None, "even", None, 0, x, ctx, {})
    for l in range(4):
        j = l // 2
        nxt = None if l == 3 else ("odd" if l % 2 == 0 else "even")
        if l % 2 == 0:
            nc_na = _prog("na", build_na)
            maps = []
            for h in range(8):
                Bg, M = na_tables(A(na_rpb[j][h]))
                sl = lambda a, o: a[:, :, o + h * 64:o + (h + 1) * 64]
                qcat = np.concatenate([sl(pc, 0), sl(px, 0)], 1)
                kcat = np.concatenate([sl(pc, 512), sl(px, 512)], 1)
                vcat = np.concatenate([sl(pc, 1024), sl(px, 1024)], 1)
                maps.append({"qT": np.ascontiguousarray(qcat.transpose(0, 2, 1)), "kT": np.ascontiguousarray(kcat.transpose(0, 2, 1)),
                             "v": np.ascontiguousarray(vcat), "Bg": Bg, "M": M})
            r = _run(nc_na, maps)
            a_x = np.zeros((4, 4096, 512), f32); a_c = np.zeros((4, 256, 512), f32)
            for h in range(8):
                o = r[h]["o"]
                a_x[:, :, h * 64:(h + 1) * 64] = o[:, 256:]; a_c[:, :, h * 64:(h + 1) * 64] = o[:, :256]
            nc_hg = _prog(("hg", j), lambda: build_hg(j == 1))
            hc = hg_consts()
            maps = []
            for core in range(8):
                zq = np.zeros((4, 128, 4352), f32); zf = np.zeros((4, 128, 4352), f32); vT = np.zeros((4, 128, 4352), f32); lbl = np.zeros((4, 128, 2), f32)
                for i in range(4):
                    n = core * 4 + i
                    b, h, dr = n // 8, (n // 2) % 4, n % 2
                    def seq(o):
                        cpart = pc[b][:, o + h * 128:o + (h + 1) * 128]; xpart = px[b][:, o + h * 128:o + (h + 1) * 128]
                        if dr == 1:
                            cpart = cpart[::-1]; xpart = xpart[::-1]
                        return np.concatenate([cpart, xpart], 0).T
                    zq[i] = seq(1536); zf[i] = seq(2048 + 512 * dr); vT[i] = seq(3072)
                    lbl[i] = A(hg_lb_logits[dr][:, h * 128:(h + 1) * 128]).T
                d = dict(hc); d.update(zqT=zq, zfT=zf, vT=vT, lbl=lbl)
                maps.append(d)
            r = _run(nc_hg, maps)
            of_x = np.zeros((4, 4096, 512), f32); ob_x = np.zeros((4, 4096, 512), f32)
            of_c = np.zeros((4, 256, 512), f32); ob_c = np.zeros((4, 256, 512), f32)
            for core in range(8):
                for i in range(4):
                    n = core * 4 + i
                    b, h, dr = n // 8, (n // 2) % 4, n % 2
                    o = r[core]["oT"][i].T
                    oc_, ox_ = o[:256], o[256:]
                    if dr == 1:
                        ob_c[b][:, h * 128:(h + 1) * 128] = oc_[::-1]; ob_x[b][:, h * 128:(h + 1) * 128] = ox_[::-1]
                    else:
                        of_c[b][:, h * 128:(h + 1) * 128] = oc_; of_x[b][:, h * 128:(h + 1) * 128] = ox_
            extra = {"ax": (a_x, a_c), "of": (of_x, of_c), "ob": (ob_x, ob_c), "gr": (px[:, :, 3584:4096], pc[:, :, 3584:4096]),
                     "ngB": _bc(np.tile(A(hg_norm_g[j]), 4)[None], (128, 512)), "wout": A(e_w_out[j]),
                     "w1": A(ffn_w1[j])[None], "w3": A(ffn_w3[j])[None], "w2": A(ffn_w2[j])[None]}
            x, ctx, px, pc = dense("even", nxt, l, l + 1, x, ctx, extra)
        else:
            nc_ret = _prog("ret", build_ret)
            rc = ret_consts()
            perm = np.concatenate([(np.arange(128) + 64) % 128, 128 + (np.arange(128) + 64) % 128])
            maps = []
            for core in range(8):
                qT = np.zeros((2, 2, 128, 4352), f32); qsT = np.zeros_like(qT); kT = np.zeros_like(qT); ksT = np.zeros_like(qT)
                vv = np.zeros((2, 4352, 512), f32); ld = np.zeros((2, 2), f32)
                for i in range(2):
                    n = core * 2 + i
                    b, h = n // 4, n % 4
                    qq = np.concatenate([pc[b][:, h * 256:(h + 1) * 256], px[b][:, h * 256:(h + 1) * 256]], 0)
                    kk = np.concatenate([pc[b][:, 1024 + h * 256:1024 + (h + 1) * 256], px[b][:, 1024 + h * 256:1024 + (h + 1) * 256]], 0)
                    qT[i] = qq.T.reshape(2, 128, 4352); qsT[i] = qq[:, perm].T.reshape(2, 128, 4352)
                    kT[i] = kk.T.reshape(2, 128, 4352); ksT[i] = kk[:, perm].T.reshape(2, 128, 4352)
                    vv[i] = np.concatenate([pc[b][:, 2048 + h * 512:2048 + (h + 1) * 512], px[b][:, 2048 + h * 512:2048 + (h + 1) * 512]], 0)
                    ld[i] = A(ret_log_decay[j])[:, h]
                d = dict(rc); d.update(qT=qT, qsT=qsT, kT=kT, ksT=ksT, v=vv, ld=ld)
                maps.append(d)
            r = _run(nc_ret, maps)
            o_x = np.zeros((4, 4096, 2048), f32); o_c = np.zeros((4, 256, 2048), f32)
            for core in range(8):
                for i in range(2):
                    n = core * 2 + i
                    b, h = n // 4, n % 4
                    o = r[core]["o"][i]
                    o_c[b][:, h * 512:(h + 1) * 512] = o[:256]; o_x[b][:, h * 512:(h + 1) * 512] = o[256:]
            extra = {"o": (o_x, o_c), "gr": (px[:, :, 4096:6144], pc[:, :, 4096:6144]),
                     "rw": A(router_w[j]), "rbB": _bc(A(router_b[j])[None], (128, 8)), "wout": A(o_w_out[j]),
                     "w1": A(moe_w1[j]), "w3": A(moe_w3[j]), "w2": A(moe_w2[j])}
            x, ctx, px, pc = dense("odd", nxt, l, l + 1, x, ctx, extra)
    return x.astype(np.float32)
```

```python
from contextlib import ExitStack

import numpy as np
import concourse.bass as bass
import concourse.mybir as mybir
from concourse.bass_utils import run_bass_kernel_spmd

F32 = mybir.dt.float32
BF16 = mybir.dt.bfloat16
AF = mybir.ActivationFunctionType
ALU = mybir.AluOpType
AX = mybir.AxisListType


class Prog:
    ENGS = ("sync", "act", "dve", "pool", "pe")
    NDMA = 16
    EPOCH = 8192

    def __init__(self, nc, same_engine_sync=None):
        import os as _os
        if same_engine_sync is None:
            same_engine_sync = _os.environ.get("SAME_SYNC", "act,dve,pool")
        self.nc = nc
        self.ops = {e: [] for e in self.ENGS}
        self.cnt = {e: 0 for e in self.ENGS}
        self.last_w = {}
        self.readers = {}
        self.waited = {e: {} for e in self.ENGS}
        self.ndma = 0
        self.dma_hist = {}
        self.same = same_engine_sync
        self.final_events = []
        self.semnames = set()

    def _deps(self, eng, reads, writes):
        deps = set()
        for k in reads:
            lw = self.last_w.get(k)
            if lw is not None:
                deps.add(lw)
        for k in writes:
            lw = self.last_w.get(k)
            if lw is not None:
                deps.add(lw)
            for r in self.readers.get(k, ()):
                deps.add(r)
        out = []
        best = {}
        for (s, v) in deps:
            if s.split("#")[0] == eng and eng not in self.same:
                continue
            if best.get(s, 0) < v:
                best[s] = v
        for s, v in best.items():
            if self.waited[eng].get(s, 0) < v:
                self.waited[eng][s] = v
                out.append((s, v))
        return out

    def _commit(self, ev, reads, writes):
        for k in reads:
            self.readers.setdefault(k, []).append(ev)
        for k in writes:
            self.last_w[k] = ev
            self.readers[k] = []

    def op(self, eng, fn, reads=(), writes=()):
        waits = self._deps(eng, reads, writes)
        self.cnt[eng] += 1
        n = self.cnt[eng]
        ev = ("%s#%d" % (eng, (n - 1) // self.EPOCH), (n - 1) % self.EPOCH + 1)
        self.semnames.add(ev[0])
        self.ops[eng].append(("op", fn, waits, ev))
        self._commit(ev, reads, writes)
        return ev

    def dma(self, out, in_, reads=(), writes=(), q="sync", **kw):
        i = self.ndma
        self.ndma += 1
        s = "dma%d" % (i % self.NDMA)
        v = 16 * (i // self.NDMA + 1)
        waits = self._deps(q, reads, writes)
        if v > 16 and self.waited[q].get(s, 0) < v - 16:
            self.waited[q][s] = v - 16
            waits.append((s, v - 16))
        ev = (s, v)
        self.ops[q].append(("dma", (out, in_, kw), waits, ev))
        self._commit(ev, reads, writes)
        return ev

    def finish(self, eng="sync"):
        evs = []
        n = self.ndma
        for j in range(min(n, self.NDMA)):
            last = ((n - 1 - j) // self.NDMA) * self.NDMA + j
            evs.append(("dma%d" % j, 16 * (last // self.NDMA + 1)))
        for e in self.ENGS:
            n = self.cnt[e]
            if n > 0:
                evs.append(("%s#%d" % (e, (n - 1) // self.EPOCH), (n - 1) % self.EPOCH + 1))
        self.ops[eng].append(("wait", None, evs, None))

    def wait_all(self, eng, events):
        self.ops[eng].append(("wait", None, list(events), None))

    def emit(self):
        nc = self.nc
        from contextlib import ExitStack
        with ExitStack() as st:
            sems = {}
            for sn in sorted(self.semnames):
                sems[sn] = st.enter_context(nc.semaphore("s_" + sn.replace("#", "_")))
            for i in range(self.NDMA):
                sems["dma%d" % i] = st.enter_context(nc.semaphore("s_dma%d" % i))
            block = st.enter_context(nc.Block())
            emap = {"sync": block.sync, "act": block.scalar, "dve": block.vector,
                    "pool": block.gpsimd, "pe": block.tensor}

            def make(ename):
                def body(eng):
                    for kind, fn, waits, ev in self.ops[ename]:
                        for (s, v) in waits:
                            eng.wait_ge(sems[s], v)
                        if kind == "op":
                            ins = fn(eng)
                            ins.then_inc(sems[ev[0]], 1)
                        elif kind == "dma":
                            out, in_, kw = fn
                            eng.dma_start(out=out, in_=in_, **kw).then_inc(sems[ev[0]], 16)
                return body
            for e in self.ENGS:
                if self.ops[e]:
                    emap[e](make(e))


def build_ada():
    nc = bass.Bass("TRN2", target_bir_lowering=False)
    cT = nc.dram_tensor("cT", [128, 8, 5], F32, kind="ExternalInput").ap()
    w = nc.dram_tensor("w", [1024, 3072], F32, kind="ExternalInput").ap()
    b = nc.dram_tensor("b", [1, 3072], F32, kind="ExternalInput").ap()
    m = nc.dram_tensor("m", [5, 3072], F32, kind="ExternalOutput").ap()
    with ExitStack() as st:
        sb = lambda n, s, d: st.enter_context(nc.sbuf_tensor(n, s, d))
        ct = sb("ct", [128, 8, 5], F32); sT = sb("sT", [128, 8, 5], F32)
        wt = [sb("wt%d" % i, [128, 8, 512], F32) for i in range(2)]
        bt = sb("bt", [5, 3072], F32); ot = sb("ot", [5, 3072], F32)
        ps = [st.enter_context(nc.psum_tensor("ps%d" % i, [5, 512], F32)) for i in range(2)]
        P = Prog(nc)
        P.dma(ct[:], cT[:, :, :], writes=["ct"])
        P.dma(bt[:], b[0:1, :].to_broadcast([5, 3072]), writes=["bt"])
        P.op("act", lambda e: e.activation(out=sT[:], in_=ct[:], func=AF.Silu), reads=["ct"], writes=["sT"])
        wv = w.rearrange("(k p) n -> p k n", p=128)
        for j in range(6):
            bi = j % 2
            P.dma(wt[bi][:], wv[:, :, j * 512:(j + 1) * 512], writes=[("wt", bi)], q="sync" if bi == 0 else "pool")
            for k in range(8):
                P.op("pe", lambda e, k=k, bi=bi: e.matmul(ps[bi][:], lhsT=sT[:, k, :], rhs=wt[bi][:, k, :], start=(k == 0), stop=(k == 7)),
                     reads=["sT", ("wt", bi)], writes=[("ps", bi)])
            P.op("dve", lambda e, j=j, bi=bi: e.tensor_tensor(out=ot[:, j * 512:(j + 1) * 512], in0=ps[bi][:], in1=bt[:, j * 512:(j + 1) * 512], op=ALU.add),
                 reads=[("ps", bi), "bt"], writes=["ot"])
        e1 = P.dma(m[:, :], ot[:], reads=["ot"])
        P.wait_all("sync", [e1])
        P.emit()
    return nc

def run_ada(c, c_ctx, ada_w, ada_b):
    nc = build_ada()
    cc = np.concatenate([c, c_ctx[None]], 0)
    cT = np.ascontiguousarray(cc.T.reshape(8, 128, 5).transpose(1, 0, 2))
    maps = []
    for core in range(8):
        l, h = core // 2, core % 2
        maps.append({"cT": cT, "w": np.ascontiguousarray(ada_w[l][:, h * 3072:(h + 1) * 3072]),
                     "b": np.ascontiguousarray(ada_b[l][None, h * 3072:(h + 1) * 3072])})
    res = run_bass_kernel_spmd(nc, maps, core_ids=list(range(8)))
    out = np.zeros((4, 5, 6144), np.float32)
    for core in range(8):
        l, h = core // 2, core % 2
        out[l][:, h * 3072:(h + 1) * 3072] = res.results[core]["m"]
    return out


import os
DBGMODE = os.environ.get('DENSE_DBG', '')

ALPHA_C = (2 * 4) ** 0.25
NT = 17
GROUPS = [[0, 1, 2, 3, 4, 5], [6, 7, 8, 9, 10, 11], [12, 13, 14, 15, 16]]
GT = 6
LN_EPS = 1e-5
DBG = False
NORM_EPS = 1e-6


def build_dense(post, pre, ntiles=NT, groups=GROUPS):
    nc = bass.Bass("TRN2", target_bir_lowering=False)
    R = ntiles * 128
    din = lambda n, s: nc.dram_tensor(n, s, F32, kind="ExternalInput").ap()
    x_d = din("x", [R, 1024])
    ident_d = din("ident", [128, 128])
    if post:
        modTp_d = din("modTp", [128, 2, 6, 8])
        gateB_d = din("gateB", [128, 2, 2, 1024])
        lnGB_d = din("lnGB", [128, 2, 2, 1024])
        xo_d = nc.dram_tensor("xo", [R, 1024], F32, kind="ExternalOutput").ap()
        if DBG:
            dbg_d = nc.dram_tensor("dbg", [R, 1024], F32, kind="ExternalOutput").ap()
            dbg3_d = nc.dram_tensor("dbg3", [128, 1024], F32, kind="ExternalOutput").ap()
            dbg2_d = nc.dram_tensor("dbg2", [R, 1024], F32, kind="ExternalOutput").ap()
        if post == "even":
            ax_d = din("ax", [R, 512]); of_d = din("of", [R, 512]); ob_d = din("ob", [R, 512]); gr_d = din("gr", [R, 512])
            ngB_d = din("ngB", [128, 512])
            CM = 8; NE = 1; FD = 2816
        else:
            o_d = din("o", [R, 2048]); gr_d = din("gr", [R, 2048])
            rw_d = din("rw", [1024, 8]); rbB_d = din("rbB", [128, 8])
            CM = 16; NE = 8; FD = 3584
        wout_d = din("wout", [CM * 128, 1024])
        w1_d = din("w1", [NE, 1024, FD]); w3_d = din("w3", [NE, 1024, FD]); w2_d = din("w2", [NE, FD, 1024])
    if pre:
        modTq_d = din("modTq", [128, 2, 6, 8])
        NO = 4096 if pre == "even" else 6144
        win_d = din("win", [1024, NO])
        proj_d = nc.dram_tensor("proj", [R, NO], F32, kind="ExternalOutput").ap()

    with ExitStack() as st:
        sb = lambda n, s, d: st.enter_context(nc.sbuf_tensor(n, s, d))
        pst = lambda n, s: st.enter_context(nc.psum_tensor(n, s, F32))
        P = Prog(nc)
        X = sb("X", [128, GT, 1024], F32)
        hT = sb("hT", [128, 8, GT * 128], BF16)
        ident = sb("ident_s", [128, 128], F32)
        wbf = [sb("wbf%d" % i, [128, 4096], BF16) for i in range(5)]
        tr = [pst("tr%d" % i, [128, 512]) for i in range(2)]
        h1p = [pst("h1p%d" % i, [128, 512]) for i in range(2)]
        h3p = [pst("h3p%d" % i, [128, 512]) for i in range(2)]
        yp = [pst("yp%d" % i, [128, 512]) for i in range(2)]
        stats = sb("stats", [128, 24], F32)
        mv = sb("mv", [128, 4, 2], F32)
        rstd = sb("rstd", [128, 4], F32)
        tmp = sb("tmp", [128, 1024], F32)
        P.dma(ident[:], ident_d[:, :], writes=["ident"])
        if post:
            modTp = sb("modTp_s", [128, 2, 6, 8], F32)
            gateB = sb("gateB_s", [128, 2, 2, 1024], F32)
            lnGB = sb("lnGB_s", [128, 2, 2, 1024], F32)
            Yacc = sb("Yacc", [128, GT, 1024], F32)
            aT = sb("aT", [128, 4, GT * 128], BF16)
            s1 = [sb("s1_%d" % i, [128, 512], F32) for i in range(2)]
            woutb = sb("woutb", [128, CM, 1024], BF16)
            mixT = sb("mixT", [128, CM, 128], BF16)
            P.dma(modTp[:], modTp_d[:, :, :, :], writes=["modTp"])
            P.dma(gateB[:], gateB_d[:, :, :, :], writes=["gateB"])
            P.dma(lnGB[:], lnGB_d[:, :, :, :], writes=["lnGB"])
            P.op("dve", lambda e: e.tensor_scalar_add(out=modTp[:, :, 4, :], in0=modTp[:, :, 4, :], scalar1=1.0), reads=["modTp"], writes=["modTp"])
            if post == "even":
                ngB = sb("ngB_s", [128, 512], F32)
                P.dma(ngB[:], ngB_d[:, :], writes=["ngB"])
                mi = [sb("mi%d" % i, [128, 512], F32) for i in range(4)]
                mixtm = sb("mixtm", [128, 1024], F32)
            else:
                rw = sb("rw_s", [128, 8, 8], F32)
                rbB = sb("rbB_s", [128, 8], F32)
                P.dma(rw[:], rw_d.rearrange("(k p) n -> p k n", p=128), writes=["rw"])
                P.dma(rbB[:], rbB_d[:, :], writes=["rbB"])
                mo = sb("mo", [128, 2048], F32); mg = sb("mg", [128, 2048], F32)
                hT32 = sb("hT32", [128, 8, 128], F32)
                lg = sb("lg", [128, 8], F32); m8 = sb("m8", [128, 8], F32); cwt = sb("cwt", [128, GT, 8], F32)
                nm1 = sb("nm1", [128, 1], F32); den = sb("den", [128, 1], F32)
        if pre:
            modTq = sb("modTq_s", [128, 2, 6, 8], F32)
            P.dma(modTq[:], modTq_d[:, :, :, :], writes=["modTq"])
            P.op("dve", lambda e: e.tensor_scalar_add(out=modTq[:, :, 1, :], in0=modTq[:, :, 1, :], scalar1=1.0), reads=["modTq"], writes=["modTq"])
            po = [sb("po%d" % i, [128, 512], F32) for i in range(1)]

        wctr = [0]
        pbc = [0]
        cast_rr = [0]

        def load_w(src_ap, n_free):
            i = wctr[0]; wctr[0] += 1
            b = i % 5
            dview = wbf[b][:, 0:n_free]
            if len(src_ap.shape) == 3:
                dview = dview.rearrange("p (a b) -> p a b", b=src_ap.shape[2])
            P.dma(dview, src_ap, writes=[("wbf", b)], q="pool")
            return wbf[b], ("wbf", b)

        def rsqrt_(dst, src, eps, rkeys, mul=1.0):
            P.op("dve", lambda e: e.tensor_scalar(out=dst, in0=src, scalar1=mul, scalar2=eps, op0=ALU.mult, op1=ALU.add), reads=rkeys, writes=["rstd"])
            P.op("act", lambda e: e.activation(out=dst, in_=dst, func=AF.Sqrt), reads=["rstd"], writes=["rstd"])
            P.op("dve", lambda e: e.reciprocal(out=dst, in_=dst), reads=["rstd"], writes=["rstd"])

        def layer_norm(Xt, xkey, li):
            for c in range(2):
                P.op("dve", lambda e, c=c: e.bn_stats(out=stats[:, c * 6:(c + 1) * 6], in_=Xt[:, c * 512:(c + 1) * 512]), reads=[xkey], writes=["stats"])
            P.op("dve", lambda e: e.bn_aggr(out=mv[:, 0, :], in_=stats[:, 0:12]), reads=["stats"], writes=["mv"])
            rsqrt_(rstd[:, 0:1], mv[:, 0, 1:2], LN_EPS, ["mv"])
            P.op("dve", lambda e: e.tensor_scalar(out=Xt, in0=Xt, scalar1=mv[:, 0, 0:1], scalar2=rstd[:, 0:1], op0=ALU.subtract, op1=ALU.mult), reads=[xkey, "mv", "rstd"], writes=[xkey])
            P.op("dve", lambda e: e.tensor_tensor(out=Xt, in0=Xt, in1=lnGB[:, li, 0, :], op=ALU.mult), reads=[xkey, "lnGB"], writes=[xkey])
            P.op("dve", lambda e: e.tensor_tensor(out=Xt, in0=Xt, in1=lnGB[:, li, 1, :], op=ALU.add), reads=[xkey, "lnGB"], writes=[xkey])

        def make_hT(i, tset, modT, mkey_, isc, ish, want32=False):
            for k in range(8):
                tb = k % 2
                P.op("pe", lambda e, k=k, tb=tb: e.transpose(out=tr[tb][:, 0:128], in_=X[:, i, k * 128:(k + 1) * 128], identity=ident[:]),
                     reads=[("X", i), "ident"], writes=[("tr", tb)])
                P.op("act", lambda e, k=k, tb=tb: e.activation(out=hT[:, k, i * 128:(i + 1) * 128], in_=tr[tb][:, 0:128], func=AF.Identity,
                                                             scale=modT[:, tset, isc, k:k + 1], bias=modT[:, tset, ish, k:k + 1]),
                     reads=[("tr", tb), mkey_], writes=[("hT", i)])
                if want32:
                    P.op("act", lambda e, k=k, tb=tb: e.activation(out=hT32[:, k, :], in_=tr[tb][:, 0:128], func=AF.Identity,
                                                                 scale=modT[:, tset, isc, k:k + 1], bias=modT[:, tset, ish, k:k + 1]),
                         reads=[("tr", tb), mkey_], writes=["hT32"])

        for g, tiles in enumerate(groups):
            nt = len(tiles); T = nt * 128
            for i, t in enumerate(tiles):
                P.dma(X[:, i, :], x_d[t * 128:(t + 1) * 128, :], writes=[("X", i)])
            if post:
                wv = wout_d.rearrange("(c p) n -> p c n", p=128)
                for c0 in range(0, CM, 4):
                    P.dma(woutb[:, c0:c0 + 4, :], wv[:, c0:c0 + 4, :], writes=["woutb"], q="pool")
                for i, t in enumerate(tiles):
                    tset = 1 if t == ntiles - 1 else 0
                    rows = slice(t * 128, (t + 1) * 128)
                    if post == "even":
                        for j, d in enumerate((ax_d, of_d, ob_d, gr_d)):
                            P.dma(mi[j][:], d[rows, :], writes=[("mi", j)], q="sync")
                        P.op("dve", lambda e: e.tensor_tensor(out=mi[1][:], in0=mi[1][:], in1=mi[2][:], op=ALU.add), reads=[("mi", 1), ("mi", 2)], writes=[("mi", 1)])
                        for h in range(4):
                            P.op("act", lambda e, h=h: e.activation(out=tmp[:, h * 128:(h + 1) * 128], in_=mi[1][:, h * 128:(h + 1) * 128], func=AF.Square, accum_out=rstd[:, h:h + 1]),
                                 reads=[("mi", 1)], writes=["tmp", "rstd"])
                        rsqrt_(rstd[:, 0:4], rstd[:, 0:4], NORM_EPS, ["rstd"], mul=1.0 / 128)
                        P.op("act", lambda e: e.activation(out=mi[3][:], in_=mi[3][:], func=AF.Silu), reads=[("mi", 3)], writes=[("mi", 3)])
                        P.op("dve", lambda e: e.tensor_copy(out=mixtm[:, 0:512], in_=mi[0][:]), reads=[("mi", 0)], writes=["mixtm"])
                        for h in range(4):
                            P.op("dve", lambda e, h=h: e.scalar_tensor_tensor(out=mixtm[:, 512 + h * 128:512 + (h + 1) * 128], in0=mi[1][:, h * 128:(h + 1) * 128], scalar=rstd[:, h:h + 1],
                                                                            in1=ngB[:, h * 128:(h + 1) * 128], op0=ALU.mult, op1=ALU.mult), reads=[("mi", 1), "rstd", "ngB"], writes=["mixtm"])
                        P.op("dve", lambda e: e.tensor_tensor(out=mixtm[:, 512:1024], in0=mixtm[:, 512:1024], in1=mi[3][:], op=ALU.mult), reads=["mixtm", ("mi", 3)], writes=["mixtm"])
                        msrc, mkey = mixtm, "mixtm"
                    else:
                        P.dma(mo[:], o_d[rows, :], writes=["mo"], q="sync")
                        P.dma(mg[:], gr_d[rows, :], writes=["mg"], q="sync")
                        for h in range(4):
                            P.op("dve", lambda e, h=h: e.bn_stats(out=stats[:, h * 6:(h + 1) * 6], in_=mo[:, h * 512:(h + 1) * 512]), reads=["mo"], writes=["stats"])
                            P.op("dve", lambda e, h=h: e.bn_aggr(out=mv[:, h, :], in_=stats[:, h * 6:(h + 1) * 6]), reads=["stats"], writes=["mv"])
                        rsqrt_(rstd[:, 0:4], mv[:, :, 1], LN_EPS, ["mv"])
                        P.op("act", lambda e: e.activation(out=mg[:], in_=mg[:], func=AF.Silu), reads=["mg"], writes=["mg"])
                        for h in range(4):
                            P.op("dve", lambda e, h=h: e.tensor_scalar(out=mo[:, h * 512:(h + 1) * 512], in0=mo[:, h * 512:(h + 1) * 512], scalar1=mv[:, h, 0:1], scalar2=rstd[:, h:h + 1],
                                                                     op0=ALU.subtract, op1=ALU.mult), reads=["mo", "mv", "rstd"], writes=["mo"])
                        P.op("dve", lambda e: e.tensor_tensor(out=mo[:], in0=mo[:], in1=mg[:], op=ALU.mult), reads=["mo", "mg"], writes=["mo"])
                        msrc, mkey = mo, "mo"
                    for c in range(CM):
                        tb = c % 2
                        P.op("pe", lambda e, c=c, tb=tb, msrc=msrc: e.transpose(out=tr[tb][:, 0:128], in_=msrc[:, c * 128:(c + 1) * 128], identity=ident[:]),
                             reads=[mkey, "ident"], writes=[("tr", tb)])
                        P.op("act", lambda e, c=c, tb=tb: e.activation(out=mixT[:, c, :], in_=tr[tb][:, 0:128], func=AF.Copy), reads=[("tr", tb)], writes=["mixT"])
                    for hf in range(2):
                        for c in range(CM):
                            P.op("pe", lambda e, c=c, hf=hf: e.matmul(yp[hf][:], lhsT=mixT[:, c, :], rhs=woutb[:, c, hf * 512:(hf + 1) * 512], start=(c == 0), stop=(c == CM - 1)),
                                 reads=["mixT", "woutb"], writes=[("yp", hf)])
                        P.op("dve", lambda e, hf=hf, tset=tset: e.tensor_tensor(out=tmp[:, hf * 512:(hf + 1) * 512], in0=yp[hf][:], in1=gateB[:, tset, 0, hf * 512:(hf + 1) * 512], op=ALU.mult),
                             reads=[("yp", hf), "gateB"], writes=["tmp"])
                    P.op("dve", lambda e, i=i: e.scalar_tensor_tensor(out=X[:, i, :], in0=X[:, i, :], scalar=ALPHA_C, in1=tmp[:], op0=ALU.mult, op1=ALU.add),
                         reads=[("X", i), "tmp"], writes=[("X", i)])
                    layer_norm(X[:, i, :], ("X", i), 0)
                    if DBG:
                        P.dma(dbg_d[t * 128:(t + 1) * 128, :], X[:, i, :], reads=[("X", i)], q="sync")
                for i, t in enumerate(tiles):
                    tset = 1 if t == ntiles - 1 else 0
                    make_hT(i, tset, modTp, "modTp", 4, 3, want32=(post == "odd" and 'no32' not in DBGMODE))
                    P.op("dve", lambda e, i=i: e.tensor_scalar_mul(out=X[:, i, :], in0=X[:, i, :], scalar1=ALPHA_C), reads=[("X", i)], writes=[("X", i)])
                    if post == "odd" and 'noroute' not in DBGMODE:
                        for k in range(8):
                            P.op("pe", lambda e, k=k: e.matmul(yp[0][:, 0:8], lhsT=hT32[:, k, :], rhs=rw[:, k, :], start=(k == 0), stop=(k == 7)), reads=["hT32", "rw"], writes=[("yp", 0)])
                        P.op("dve", lambda e: e.tensor_tensor(out=lg[:], in0=yp[0][:, 0:8], in1=rbB[:], op=ALU.add), reads=[("yp", 0), "rbB"], writes=["lg"])
                        P.op("dve", lambda e: e.max(out=m8[:], in_=lg[:]), reads=["lg"], writes=["m8"])
                        P.op("dve", lambda e: e.tensor_scalar_mul(out=nm1[:], in0=m8[:, 0:1], scalar1=-1.0), reads=["m8"], writes=["nm1"])
                        P.op("act", lambda e, i=i: e.activation(out=cwt[:, i, :], in_=lg[:], func=AF.Exp, bias=nm1[:, 0:1], scale=1.0), reads=["lg", "nm1"], writes=[("cw", i)])
                        P.op("dve", lambda e: e.tensor_scalar(out=lg[:], in0=lg[:], scalar1=m8[:, 1:2], scalar2=None, op0=ALU.is_ge), reads=["lg", "m8"], writes=["lg"])
                        P.op("dve", lambda e, i=i: e.tensor_tensor(out=cwt[:, i, :], in0=cwt[:, i, :], in1=lg[:], op=ALU.mult), reads=[("cw", i), "lg"], writes=[("cw", i)])
                        P.op("dve", lambda e, i=i: e.reduce_sum(out=den[:], in_=cwt[:, i, :], axis=AX.X), reads=[("cw", i)], writes=["den"])
                        P.op("dve", lambda e: e.reciprocal(out=den[:], in_=den[:]), reads=["den"], writes=["den"])
                        P.op("dve", lambda e, i=i: e.tensor_scalar_mul(out=cwt[:, i, :], in0=cwt[:, i, :], scalar1=den[:, 0:1]), reads=[("cw", i), "den"], writes=[("cw", i)])
                first = True
                for ex in range(0 if 'noffn' in DBGMODE else NE):
                    for f0 in range(0, FD, 512):
                        fb = min(512, FD - f0); nch = fb // 128
                        w1b, k1 = load_w(w1_d[ex].rearrange("(k p) n -> p k n", p=128)[:, :, f0:f0 + fb], 8 * fb)
                        w3b, k3 = load_w(w3_d[ex].rearrange("(k p) n -> p k n", p=128)[:, :, f0:f0 + fb], 8 * fb)
                        w2b, k2 = load_w(w2_d[ex][f0:f0 + fb, :].rearrange("(c p) n -> p c n", p=128), nch * 1024)
                        w1v = w1b[:, 0:8 * fb].rearrange("p (k n) -> p k n", n=fb)
                        w3v = w3b[:, 0:8 * fb].rearrange("p (k n) -> p k n", n=fb)
                        w2v = w2b[:, 0:nch * 1024].rearrange("p (c n) -> p c n", n=1024)
                        hkeys = [("hT", i) for i in range(nt)]
                        subs = [(s0, min(512, T - s0)) for s0 in range(0, T, 512)]
                        for j in range(nch):
                            for (s0, sn) in subs:
                                pb = pbc[0] % 2; pbc[0] += 1
                                for k in range(8):
                                    P.op("pe", lambda e, j=j, k=k, pb=pb, w1v=w1v, s0=s0, sn=sn: e.matmul(h1p[pb][:, 0:sn], lhsT=w1v[:, k, j * 128:(j + 1) * 128], rhs=hT[:, k, s0:s0 + sn], start=(k == 0), stop=(k == 7)),
                                         reads=[k1] + hkeys, writes=[("h1p", pb)])
                                for k in range(8):
                                    P.op("pe", lambda e, j=j, k=k, pb=pb, w3v=w3v, s0=s0, sn=sn: e.matmul(h3p[pb][:, 0:sn], lhsT=w3v[:, k, j * 128:(j + 1) * 128], rhs=hT[:, k, s0:s0 + sn], start=(k == 0), stop=(k == 7)),
                                         reads=[k3] + hkeys, writes=[("h3p", pb)])
                                P.op("act", lambda e, pb=pb, sn=sn: e.activation(out=s1[pb][:, 0:sn], in_=h1p[pb][:, 0:sn], func=AF.Silu), reads=[("h1p", pb)], writes=[("s1", pb)])
                                P.op("dve", lambda e, pb=pb, j=j, s0=s0, sn=sn: e.tensor_tensor(out=aT[:, j, s0:s0 + sn], in0=s1[pb][:, 0:sn], in1=h3p[pb][:, 0:sn], op=ALU.mult), reads=[("s1", pb), ("h3p", pb)], writes=[("aT", j)])
                        akeys = [("aT", j) for j in range(nch)]
                        if DBG and g == 0 and ex == 0 and f0 == 0:
                            P.op("dve", lambda e: e.tensor_copy(out=tmp[:, 0:512], in_=aT[:, 0, :]), reads=[("aT", 0)], writes=["tmp"])
                            P.op("dve", lambda e: e.tensor_copy(out=tmp[:, 512:1024], in_=hT[:, 0, :]), reads=[("hT", 0), ("hT", 1), ("hT", 2), ("hT", 3)], writes=["tmp"])
                            P.dma(dbg3_d[:, :], tmp[:], reads=["tmp"], q="sync")
                        for i in range(nt):
                            for hf in range(2):
                                for j in range(nch):
                                    P.op("pe", lambda e, i=i, hf=hf, j=j, w2v=w2v, nch=nch: e.matmul(yp[hf][:], lhsT=aT[:, j, i * 128:(i + 1) * 128], rhs=w2v[:, j, hf * 512:(hf + 1) * 512], start=(j == 0), stop=(j == nch - 1)),
                                         reads=[k2] + akeys, writes=[("yp", hf)])
                                ya = Yacc[:, i, hf * 512:(hf + 1) * 512]
                                if post == "odd":
                                    if first:
                                        P.op("dve", lambda e, hf=hf, ya=ya, i=i, ex=ex: e.tensor_scalar_mul(out=ya, in0=yp[hf][:], scalar1=cwt[:, i, ex:ex + 1]), reads=[("yp", hf), ("cw", i)], writes=[("Y", i)])
                                    else:
                                        P.op("dve", lambda e, hf=hf, ya=ya, i=i, ex=ex: e.scalar_tensor_tensor(out=ya, in0=yp[hf][:], scalar=cwt[:, i, ex:ex + 1], in1=ya, op0=ALU.mult, op1=ALU.add),
                                             reads=[("yp", hf), ("cw", i), ("Y", i)], writes=[("Y", i)])
                                else:
                                    if first:
                                        P.op("dve", lambda e, hf=hf, ya=ya: e.tensor_copy(out=ya, in_=yp[hf][:]), reads=[("yp", hf)], writes=[("Y", i)])
                                    else:
                                        P.op("dve", lambda e, hf=hf, ya=ya: e.tensor_tensor(out=ya, in0=yp[hf][:], in1=ya, op=ALU.add), reads=[("yp", hf), ("Y", i)], writes=[("Y", i)])
                        first = False
                for i, t in enumerate(tiles):
                    tset = 1 if t == ntiles - 1 else 0
                    if 'noffn' in DBGMODE:
                        P.op("dve", lambda e, i=i: e.memset(Yacc[:, i, :], 0.0), writes=[("Y", i)])
                    if DBG:
                        P.dma(dbg2_d[t * 128:(t + 1) * 128, :], Yacc[:, i, :], reads=[("Y", i)], q="sync")
                    P.op("dve", lambda e, i=i, tset=tset: e.tensor_tensor(out=Yacc[:, i, :], in0=Yacc[:, i, :], in1=gateB[:, tset, 1, :], op=ALU.mult), reads=[("Y", i), "gateB"], writes=[("Y", i)])
                    P.op("dve", lambda e, i=i: e.tensor_tensor(out=X[:, i, :], in0=X[:, i, :], in1=Yacc[:, i, :], op=ALU.add), reads=[("X", i), ("Y", i)], writes=[("X", i)])
                    layer_norm(X[:, i, :], ("X", i), 1)
                    P.dma(xo_d[t * 128:(t + 1) * 128, :], X[:, i, :], reads=[("X", i)], q="sync")
            if pre:
                for i, t in enumerate(tiles):
                    tset = 1 if t == ntiles - 1 else 0
                    make_hT(i, tset, modTq, "modTq", 1, 0)
                wv = win_d.rearrange("(k p) n -> p k n", p=128)
                pc = 0
                for n0 in range(0, NO, 512):
                    wb_, wk = load_w(wv[:, :, n0:n0 + 512], 4096)
                    wvv = wb_[:, :].rearrange("p (k n) -> p k n", n=512)
                    for i, t in enumerate(tiles):
                        hf = pc % 2; ob = 0; pc += 1
                        for k in range(8):
                            P.op("pe", lambda e, i=i, k=k, hf=hf, wvv=wvv: e.matmul(yp[hf][:], lhsT=hT[:, k, i * 128:(i + 1) * 128], rhs=wvv[:, k, :], start=(k == 0), stop=(k == 7)),
                                 reads=[wk, ("hT", i)], writes=[("yp", hf)])
                        P.op("act", lambda e, hf=hf, ob=ob: e.activation(out=po[ob][:], in_=yp[hf][:], func=AF.Copy), reads=[("yp", hf)], writes=[("po", ob)])
                        P.dma(proj_d[t * 128:(t + 1) * 128, n0:n0 + 512], po[ob][:], reads=[("po", ob)], q="sync")
        P.finish()
        P.emit()
    return nc


S_RET = 4352
NU_RET = 2


def ret_consts():
    f32 = np.float32
    ki = np.arange(128)[:, None]; qi = np.arange(512)[None, :]
    Mqk = (qi - ki).astype(f32)
    Rp = np.zeros((4, 128, 512), f32); Rn = np.zeros((4, 128, 512), f32); Eq = np.zeros((4, 128, 512), f32)
    for v in range(4):
        d = qi - ki - 128 * v
        Rp[v] = np.maximum(d, 0); Rn[v] = np.maximum(-d, 0); Eq[v] = (d == 0)
    strc = np.ascontiguousarray(np.stack([Rp, Rn, Eq], 0).transpose(2, 0, 1, 3))
    t = np.arange(4096); rows = t // 64; cols = t % 64
    jj = np.arange(128) % 64
    inv = (10000.0 ** (-(jj.astype(np.float64)) / 64))
    cosT = np.ones((2, 128, S_RET), f32); sinT = np.zeros((2, 128, S_RET), f32)
    for c, pos in enumerate((rows, cols)):
        ang = (pos[None, :].astype(np.float32) * inv.astype(np.float32)[:, None]).astype(np.float32)
        cosT[c, :, 256:] = np.cos(ang)
        sgn = np.where(np.arange(128) < 64, -1.0, 1.0)[:, None]
        sinT[c, :, 256:] = np.sin(ang) * sgn
    n128 = (128.0 * np.arange(48)).astype(f32)
    return {"Mqk": Mqk, "strc": strc, "cosT": cosT, "sinT": sinT,
            "n128": np.ascontiguousarray(np.broadcast_to(n128[None], (128, 48)))}


def build_ret():
    nc = bass.Bass("TRN2", target_bir_lowering=False)
    din = lambda n, s: nc.dram_tensor(n, s, F32, kind="ExternalInput").ap()
    S = S_RET
    qT_d = din("qT", [NU_RET, 2, 128, S]); qsT_d = din("qsT", [NU_RET, 2, 128, S])
    kT_d = din("kT", [NU_RET, 2, 128, S]); ksT_d = din("ksT", [NU_RET, 2, 128, S])
    v_d = din("v", [NU_RET, S, 512]); ld_d = din("ld", [NU_RET, 2])
    Mqk_d = din("Mqk", [128, 512]); strc_d = din("strc", [128, 3, 4, 512])
    cosT_d = din("cosT", [2, 128, S]); sinT_d = din("sinT", [2, 128, S]); n128_d = din("n128", [128, 48])
    o_d = nc.dram_tensor("o", [NU_RET, S, 512], F32, kind="ExternalOutput").ap()
    NTK = S // 128
    with ExitStack() as st:
        sb = lambda n, s, d: st.enter_context(nc.sbuf_tensor(n, s, d))
        pst = lambda n, s: st.enter_context(nc.psum_tensor(n, s, F32))
        P = Prog(nc)
        Mqk = sb("Mqk_s", [128, 512], F32); strc = sb("strc_s", [128, 3, 4, 512], F32); n128 = sb("n128_s", [128, 48], F32)
        P.dma(Mqk[:], Mqk_d[:, :], writes=["Mqk"]); P.dma(strc[:], strc_d[:, :, :, :], writes=["strc"]); P.dma(n128[:], n128_d[:, :], writes=["n128"])
        qr = sb("qr", [128, 2, S], BF16); kr = sb("kr", [128, 2, S], BF16); vb = sb("vb", [128, NTK, 512], BF16)
        CH = 1088
        ra = sb("ra", [128, CH], F32); rb_ = sb("rb", [128, CH], F32); rc = sb("rc", [128, CH], F32); rd = sb("rd", [128, CH], F32)
        vst = [sb("vst%d" % i, [128, 512], F32) for i in range(2)]
        ld = sb("ld_s", [128, 2], F32); nld = sb("nld", [128, 2], F32)
        bF = sb("bF", [128, 48], F32); bB = sb("bB", [128, 48], F32)
        Dstr = sb("Dstr", [128, 4, 512], F32); e1 = sb("e1", [128, 512], F32); e2 = sb("e2", [128, 512], F32)
        mk = [sb("mk%d" % i, [128, 512], F32) for i in range(2)]; mk2 = [sb("mk2_%d" % i, [128, 512], F32) for i in range(2)]
        At = [sb("At%d" % i, [128, 512], BF16) for i in range(2)]
        ot = [sb("ot%d" % i, [128, 512], F32) for i in range(2)]
        sp = [pst("sp%d" % i, [128, 512]) for i in range(2)]
        op_ = [pst("op%d" % i, [128, 512]) for i in range(4)]
        octr = [0]
        for u in range(NU_RET):
            P.dma(ld[:], ld_d[u:u + 1, :].to_broadcast([128, 2]), writes=["ld"])
            P.op("dve", lambda e: e.tensor_scalar(out=nld[:], in0=ld[:], scalar1=-1.0, scalar2=None, op0=ALU.mult), reads=["ld"], writes=["nld"])
            P.op("dve", lambda e: e.tensor_scalar(out=bF[:], in0=n128[:], scalar1=ld[:, 0:1], scalar2=None, op0=ALU.mult), reads=["ld", "n128"], writes=["bF"])
            P.op("dve", lambda e: e.tensor_scalar(out=bB[:], in0=n128[:], scalar1=ld[:, 1:2], scalar2=None, op0=ALU.mult), reads=["ld", "n128"], writes=["bB"])
            for v in range(4):
                P.op("act", lambda e, v=v: e.activation(out=e1[:], in_=strc[:, 0, v, :], func=AF.Exp, scale=ld[:, 0:1]), reads=["strc", "ld"], writes=["e1"])
                P.op("act", lambda e, v=v: e.activation(out=e2[:], in_=strc[:, 1, v, :], func=AF.Exp, scale=ld[:, 1:2]), reads=["strc", "ld"], writes=["e2"])
                P.op("dve", lambda e, v=v: e.tensor_tensor(out=Dstr[:, v, :], in0=e1[:], in1=e2[:], op=ALU.mult), reads=["e1", "e2"], writes=["Dstr"])
                P.op("dve", lambda e, v=v: e.tensor_tensor(out=Dstr[:, v, :], in0=Dstr[:, v, :], in1=strc[:, 2, v, :], op=ALU.add), reads=["Dstr", "strc"], writes=["Dstr"])
            for (src, ssrc, dst, dkey) in ((qT_d, qsT_d, qr, "qr"), (kT_d, ksT_d, kr, "kr")):
                for c in range(2):
                    for t0 in range(0, S, CH):
                        P.dma(ra[:], src[u, c, :, t0:t0 + CH], writes=["ra"], q="sync")
                        P.dma(rb_[:], ssrc[u, c, :, t0:t0 + CH], writes=["rb"], q="pool")
                        P.dma(rc[:], cosT_d[c, :, t0:t0 + CH], writes=["rc"], q="sync")
                        P.dma(rd[:], sinT_d[c, :, t0:t0 + CH], writes=["rd"], q="pool")
                        P.op("dve", lambda e: e.tensor_tensor(out=ra[:], in0=ra[:], in1=rc[:], op=ALU.mult), reads=["ra", "rc"], writes=["ra"])
                        P.op("pool", lambda e: e.tensor_tensor(out=rb_[:], in0=rb_[:], in1=rd[:], op=ALU.mult), reads=["rb", "rd"], writes=["rb"])
                        P.op("dve", lambda e, dst=dst, c=c, t0=t0: e.tensor_tensor(out=dst[:, c, t0:t0 + CH], in0=ra[:], in1=rb_[:], op=ALU.add), reads=["ra", "rb"], writes=[dkey])
            for t in range(NTK):
                s_ = t % 2
                P.dma(vst[s_][:], v_d[u, t * 128:(t + 1) * 128, :], writes=[("vst", s_)], q="sync" if s_ == 0 else "pool")
                P.op("act", lambda e, t=t, s_=s_: e.activation(out=vb[:, t, :], in_=vst[s_][:], func=AF.Copy), reads=[("vst", s_)], writes=["vb"])
            blocks = [(0, 256, True)] + [(256 + 512 * i, 512, False) for i in range(8)]
            bc = 0
            for (q0, nq, isctx) in blocks:
                nsub = nq // 128
                ktiles = [0, 1] if isctx else list(range(NTK))
                for ki_, kt in enumerate(ktiles):
                    pb = bc % 2; bc += 1
                    for c in range(2):
                        P.op("pe", lambda e, c=c, kt=kt, q0=q0, nq=nq, pb=pb: e.matmul(sp[pb][:, 0:nq], lhsT=kr[:, c, kt * 128:(kt + 1) * 128], rhs=qr[:, c, q0:q0 + nq], start=(c == 0), stop=(c == 1)),
                             reads=["qr", "kr"], writes=[("sp", pb)])
                    if isctx:
                        msk = Dstr[:, kt, 0:nq]; mkeys = ["Dstr"]
                    else:
                        if kt < 2:
                            qx0 = q0 - 256
                            nf = (qx0 - (kt * 128 - 256)) // 128
                            nb = ((4096 + kt * 128) - qx0) // 128
                            P.op("act", lambda e, pb=pb, nf=nf: e.activation(out=mk[pb][:], in_=Mqk[:], func=AF.Exp, scale=ld[:, 0:1], bias=bF[:, nf:nf + 1]), reads=["Mqk", "ld", "bF"], writes=[("mk", pb)])
                            P.op("act", lambda e, pb=pb, nb=nb: e.activation(out=mk2[pb][:], in_=Mqk[:], func=AF.Exp, scale=nld[:, 1:2], bias=bB[:, nb:nb + 1]), reads=["Mqk", "nld", "bB"], writes=[("mk2", pb)])
                            P.op("pool", lambda e, pb=pb: e.tensor_tensor(out=mk[pb][:], in0=mk[pb][:], in1=mk2[pb][:], op=ALU.add), reads=[("mk", pb), ("mk2", pb)], writes=[("mk", pb)])
                            msk = mk[pb][:, :]; mkeys = [("mk", pb)]
                        else:
                            k0 = kt * 128
                            if q0 >= k0 + 128:
                                n = (q0 - k0) // 128
                                P.op("act", lambda e, pb=pb, n=n: e.activation(out=mk[pb][:], in_=Mqk[:], func=AF.Exp, scale=ld[:, 0:1], bias=bF[:, n:n + 1]), reads=["Mqk", "ld", "bF"], writes=[("mk", pb)])
                                msk = mk[pb][:, :]; mkeys = [("mk", pb)]
                            elif q0 + 512 <= k0:
                                n = (k0 - q0) // 128
                                P.op("act", lambda e, pb=pb, n=n: e.activation(out=mk[pb][:], in_=Mqk[:], func=AF.Exp, scale=nld[:, 1:2], bias=bB[:, n:n + 1]), reads=["Mqk", "nld", "bB"], writes=[("mk", pb)])
                                msk = mk[pb][:, :]; mkeys = [("mk", pb)]
                            else:
                                v = (k0 - q0) // 128
                                msk = Dstr[:, v, :]; mkeys = ["Dstr"]
                    P.op("dve", lambda e, pb=pb, nq=nq, msk=msk: e.scalar_tensor_tensor(out=At[pb][:, 0:nq], in0=sp[pb][:, 0:nq], scalar=1.0 / 16, in1=msk, op0=ALU.mult, op1=ALU.mult),
                         reads=[("sp", pb)] + mkeys, writes=[("At", pb)])
                    for s in range(nsub):
                        P.op("pe", lambda e, s=s, pb=pb, kt=kt, first=(ki_ == 0), last=(ki_ == len(ktiles) - 1): e.matmul(op_[s][:], lhsT=At[pb][:, s * 128:(s + 1) * 128], rhs=vb[:, kt, :], start=first, stop=last),
                             reads=[("At", pb), "vb"], writes=[("op", s)])
                for s in range(nsub):
                    ob = octr[0] % 2; octr[0] += 1
                    P.op("act", lambda e, s=s, ob=ob: e.activation(out=ot[ob][:], in_=op_[s][:], func=AF.Copy), reads=[("op", s)], writes=[("ot", ob)])
                    P.dma(o_d[u, q0 + s * 128:q0 + (s + 1) * 128, :], ot[ob][:], reads=[("ot", ob)], q="sync")
        P.finish(); P.emit()
    return nc


def ret_ref(q, k, v, ld):
    S = q.shape[0]
    A = (q.astype(np.float64) @ k.astype(np.float64).T) / 16
    pos_f = np.concatenate([np.arange(256) - 256, np.arange(4096)]).astype(np.float64)
    pos_b = np.concatenate([np.arange(256) + 4096, np.arange(4096)]).astype(np.float64)
    df = pos_f[:, None] - pos_f[None, :]
    db = pos_b[None, :] - pos_b[:, None]
    D = np.where(df >= 0, np.exp(ld[0] * np.maximum(df, 0)), 0) + np.where(db >= 0, np.exp(ld[1] * np.maximum(db, 0)), 0)
    D[:256, 256:] = 0
    return (A * D) @ v.astype(np.float64)


NU_NA = 4
S_NA = 4352
NEG = -30000.0


def na_variants():
    var_of = {}; reps = []
    for I in range(8):
        jlo, jhi = max(0, 4 * I - 2), min(31, 4 * I + 5)
        for j in range(jlo, jhi + 1):
            if I == 0:
                key = ("a", j)
            elif I == 7:
                key = ("z", j)
            else:
                key = ("m", j - 4 * I)
            if key not in var_of:
                var_of[key] = len(reps); reps.append((I, j))
    return var_of, reps


def na_tables(rpb_h):
    var_of, reps = na_variants()
    kp = np.arange(128); q = np.arange(512)
    a = kp // 64; kc = kp % 64; m = q // 64; qc = q % 64
    Bg = np.zeros((128, len(reps), 512), np.float32); M = np.zeros((128, len(reps), 512), np.float32)
    c_start = np.clip(qc - 8, 0, 48)
    for vi, (I, j) in enumerate(reps):
        kr = (2 * j + a)[:, None]; qr = (8 * I + m)[None, :]
        s = np.clip(qr - 4, 0, 56)
        rowok = (kr >= s) & (kr < s + 8)
        colok = (kc[:, None] >= c_start[None, :]) & (kc[:, None] < c_start[None, :] + 16)
        ok = rowok & colok
        dr = np.clip(kr - qr + 7, 0, 14); dc = np.clip(kc[:, None] - qc[None, :], -15, 15) + 15
        Bg[:, vi, :] = rpb_h[dr, dc]
        M[:, vi, :] = np.where(ok, 0.0, NEG)
    return Bg, M


def build_na():
    nc = bass.Bass("TRN2", target_bir_lowering=False)
    din = lambda n, s: nc.dram_tensor(n, s, F32, kind="ExternalInput").ap()
    S = S_NA
    qT_d = din("qT", [NU_NA, 64, S]); kT_d = din("kT", [NU_NA, 64, S]); v_d = din("v", [NU_NA, S, 64])
    Bg_d = din("Bg", [128, 20, 512]); M_d = din("M", [128, 20, 512])
    o_d = nc.dram_tensor("o", [NU_NA, S, 64], F32, kind="ExternalOutput").ap()
    var_of, reps = na_variants()
    with ExitStack() as st:
        sb = lambda n, s, d: st.enter_context(nc.sbuf_tensor(n, s, d))
        pst = lambda n, s: st.enter_context(nc.psum_tensor(n, s, F32))
        P = Prog(nc)
        Bt = sb("Bt", [128, 20, 512], F32)
        stg = [sb("stg%d" % i, [128, 2560], F32) for i in range(2)]
        for c in range(4):
            P.dma(Bt[:, c * 5:(c + 1) * 5, :], Bg_d[:, c * 5:(c + 1) * 5, :], writes=[("Bt", c)], q="sync")
            P.dma(stg[c % 2][:, :].rearrange("p (a b) -> p a b", b=512), M_d[:, c * 5:(c + 1) * 5, :], writes=[("stg", c % 2)], q="pool")
            P.op("dve", lambda e, c=c: e.tensor_tensor(out=Bt[:, c * 5:(c + 1) * 5, :], in0=Bt[:, c * 5:(c + 1) * 5, :], in1=stg[c % 2][:, :].rearrange("p (a b) -> p a b", b=512), op=ALU.add),
                 reads=[("Bt", c), ("stg", c % 2)], writes=[("Bt", c)])
        Bkeys = [("Bt", c) for c in range(4)]
        qb = sb("qb", [64, S], BF16); kb = sb("kb", [64, S], BF16); va = sb("va", [128, 34, 65], BF16)
        vst = sb("vstg", [128, 34, 64], F32)
        sbt = [sb("sbt%d" % i, [128, 512], F32) for i in range(2)]
        Et = [sb("Et%d" % i, [128, 512], BF16) for i in range(2)]
        rden = sb("rden", [128, 1], F32); ot = [sb("ot%d" % i, [128, 64], F32) for i in range(2)]
        sp = [pst("sp%d" % i, [128, 512]) for i in range(2)]
        op_ = [pst("op%d" % i, [128, 512]) for i in range(4)]
        P.op("pool", lambda e: e.memset(va[:, :, 64:65], 1.0), writes=["va"])
        octr = [0]; bc = 0
        for u in range(NU_NA):
            for (src, dst, dk) in ((qT_d, qb, "qb"), (kT_d, kb, "kb")):
                for h2 in range(2):
                    s_ = h2
                    P.dma(stg[s_][0:64, 0:2176], src[u, :, h2 * 2176:(h2 + 1) * 2176], writes=[("stg", s_)], q="sync" if h2 == 0 else "pool")
                    P.op("act", lambda e, dst=dst, h2=h2, s_=s_: e.activation(out=dst[:, h2 * 2176:(h2 + 1) * 2176], in_=stg[s_][0:64, 0:2176], func=AF.Copy), reads=[("stg", s_)], writes=[dk])
            P.dma(vst[:], v_d[u].rearrange("(t p) d -> p t d", p=128), writes=["vst"], q="sync")
            P.op("dve", lambda e: e.tensor_copy(out=va[:, :, 0:64], in_=vst[:]), reads=["vst"], writes=["va"])
            blocks = [("C", 0, 256)] + [(I, 256 + 512 * I, 512) for I in range(8)]
            for (I, q0, nq) in blocks:
                nsub = nq // 128
                if I == "C":
                    kts = [(0, None), (1, None)]
                else:
                    kts = [(0, None), (1, None)]
                    for j in range(max(0, 4 * I - 2), min(31, 4 * I + 5) + 1):
                        key = ("a", j) if I == 0 else (("z", j) if I == 7 else ("m", j - 4 * I))
                        kts.append((2 + j, var_of[key]))
                for ki_, (kt, vi) in enumerate(kts):
                    pb = bc % 2; bc += 1
                    P.op("pe", lambda e, kt=kt, q0=q0, nq=nq, pb=pb: e.matmul(sp[pb][:, 0:nq], lhsT=kb[:, kt * 128:(kt + 1) * 128], rhs=qb[:, q0:q0 + nq], start=True, stop=True),
                         reads=["qb", "kb"], writes=[("sp", pb)])
                    if vi is None:
                        P.op("act", lambda e, pb=pb, nq=nq: e.activation(out=Et[pb][:, 0:nq], in_=sp[pb][:, 0:nq], func=AF.Exp, scale=0.125), reads=[("sp", pb)], writes=[("Et", pb)])
                    else:
                        P.op("dve", lambda e, pb=pb, vi=vi: e.scalar_tensor_tensor(out=sbt[pb][:], in0=sp[pb][:], scalar=0.125, in1=Bt[:, vi, :], op0=ALU.mult, op1=ALU.add),
                             reads=[("sp", pb)] + Bkeys, writes=[("sbt", pb)])
                        P.op("act", lambda e, pb=pb: e.activation(out=Et[pb][:], in_=sbt[pb][:], func=AF.Exp), reads=[("sbt", pb)], writes=[("Et", pb)])
                    for s in range(nsub):
                        P.op("pe", lambda e, s=s, pb=pb, kt=kt, first=(ki_ == 0), last=(ki_ == len(kts) - 1): e.matmul(op_[s][:, 0:65], lhsT=Et[pb][:, s * 128:(s + 1) * 128], rhs=va[:, kt, :], start=first, stop=last),
                             reads=[("Et", pb), "va"], writes=[("op", s)])
                for s in range(nsub):
                    ob = octr[0] % 2; octr[0] += 1
                    P.op("dve", lambda e, s=s: e.reciprocal(out=rden[:], in_=op_[s][:, 64:65]), reads=[("op", s)], writes=["rden"])
                    P.op("dve", lambda e, s=s, ob=ob: e.tensor_scalar(out=ot[ob][:], in0=op_[s][:, 0:64], scalar1=rden[:, 0:1], scalar2=None, op0=ALU.mult), reads=[("op", s), "rden"], writes=[("ot", ob)])
                    P.dma(o_d[u, q0 + s * 128:q0 + (s + 1) * 128, :], ot[ob][:], reads=[("ot", ob)], q="sync")
        P.finish(); P.emit()
    return nc


def na_ref(q, k, v, qc, kc, vc, rpb_h):
    q = q.astype(np.float64) * 0.125; k = k.astype(np.float64); v = v.astype(np.float64)
    kc = kc.astype(np.float64); vc = vc.astype(np.float64)
    out = np.zeros((4096, 64))
    col = np.arange(64)
    for r in range(64):
        s = min(max(r - 4, 0), 56)
        keys = k[s * 64:(s + 8) * 64]; vals = v[s * 64:(s + 8) * 64]
        qq = q[r * 64:(r + 1) * 64]
        sc = qq @ keys.T
        kr = s + np.arange(8).repeat(64); kcol = np.tile(col, 8)
        cst = np.clip(col - 8, 0, 48)
        ok = (kcol[None, :] >= cst[:, None]) & (kcol[None, :] < cst[:, None] + 16)
        dr = kr - r + 7; dc = np.clip(kcol[None, :] - col[:, None], -15, 15) + 15
        sc = np.where(ok, sc + rpb_h[dr[None, :].repeat(64, 0), dc], -np.inf)
        scc = qq @ kc.T
        al = np.concatenate([sc, scc], 1); al = al - al.max(1, keepdims=True); p = np.exp(al); p /= p.sum(1, keepdims=True)
        out[r * 64:(r + 1) * 64] = p[:, :512] @ vals + p[:, 512:] @ vc
    sc = (qc.astype(np.float64) * 0.125) @ kc.T; sc -= sc.max(1, keepdims=True); p = np.exp(sc); p /= p.sum(1, keepdims=True)
    return out, p @ vc


import os
HGV = 2
HGQ = int(os.environ.get('HGQ', '0'))
NU_HG = 4
S_HG = 4352


def hg_consts():
    import ml_dtypes
    sel = np.zeros((128, 128, 128), np.float32)
    selT = np.zeros((128, 128, 128), np.float32)
    for vi in range(128):
        sel[vi, vi, :] = 1.0
        selT[:, vi, vi] = 1.0
    return {"sel": sel.reshape(128, 128 * 128), "selT": selT.reshape(128, 128 * 128),
            "io": np.ascontiguousarray(np.stack([np.eye(128, dtype=np.float32), np.ones((128, 128), np.float32)], 1))}


def build_hg(use_lb):
    nc = bass.Bass("TRN2", target_bir_lowering=False)
    din = lambda n, s: nc.dram_tensor(n, s, F32, kind="ExternalInput").ap()
    S = S_HG
    zq_d = din("zqT", [NU_HG, 128, S]); zf_d = din("zfT", [NU_HG, 128, S]); vT_d = din("vT", [NU_HG, 128, S]); lbl_d = din("lbl", [NU_HG, 128, 2])
    sel_d = din("sel", [128, 128 * 128]); selT_d = din("selT", [128, 128 * 128]); io_d = din("io", [128, 2, 128])
    o_d = nc.dram_tensor("oT", [NU_HG, 128, S], F32, kind="ExternalOutput").ap()
    with ExitStack() as st:
        sb = lambda n, s, d: st.enter_context(nc.sbuf_tensor(n, s, d))
        pst = lambda n, s: st.enter_context(nc.psum_tensor(n, s, F32))
        P = Prog(nc)
        sel = sb("sel_s", [128, 128, 128], BF16); selT = sb("selT_s", [128, 128, 128], BF16)
        stg = sb("stg", [128, 4352], F32)
        for (src, dst, dk) in ((sel_d, sel, "sel"), (selT_d, selT, "selT")):
            for c in range(4):
                P.dma(stg[:, 0:4096], src[:, c * 4096:(c + 1) * 4096], writes=["stg"], q="sync")
                P.op("dve", lambda e, dst=dst, c=c: e.tensor_copy(out=dst[:, c * 32:(c + 1) * 32, :], in_=stg[:, 0:4096].rearrange("p (a b) -> p a b", b=128)), reads=["stg"], writes=[dk])
        ft = sb("ft", [128, S], F32); qt = sb("qt", [128, S], F32); vTb = sb("vTb", [128, S], BF16); vTl = sb("vTl", [128, S], BF16); vt32 = sb("vt32", [128, S + 1], F32)
        io = sb("io_s", [128, 2, 128], F32); Dg = sb("Dg", [128, 128], F32)
        P.dma(io[:], io_d[:, :, :], writes=["io"])
        lbl = sb("lbl_s", [128, 2], F32); lb = sb("lb", [128, 1], F32); oml = sb("oml", [128, 1], F32)
        state = sb("state", [128, 128], F32)
        NB = 6; NBP = 4
        Sv = [sb("Sv_%d" % i, [128, 512], F32) for i in range(NB)]
        qs = [sb("qs_%d" % i, [128, 512], BF16) for i in range(NB)]
        ot = [sb("ot_%d" % i, [128, 512], F32) for i in range(2)]
        tmpc = sb("tmpc", [128, 512], F32)
        vbp = [pst("vbp%d" % i, [128, 512]) for i in range(NBP)]
        opp = [pst("opp%d" % i, [128, 512]) for i in range(2)]
        qsp = pst("qsp", [128, 512])
        cc = 0
        for u in range(NU_HG):
            if use_lb:
                P.dma(lbl[:], lbl_d[u, :, :], writes=["lbl"])
                P.op("dve", lambda e: e.tensor_tensor(out=lb[:], in0=lbl[:, 1:2], in1=lbl[:, 0:1], op=ALU.subtract), reads=["lbl"], writes=["lb"])
                P.op("act", lambda e: e.activation(out=lb[:], in_=lb[:], func=AF.Sigmoid), reads=["lb"], writes=["lb"])
                P.op("dve", lambda e: e.tensor_scalar(out=oml[:], in0=lb[:], scalar1=-1.0, scalar2=1.0, op0=ALU.mult, op1=ALU.add), reads=["lb"], writes=["oml"])
            P.dma(stg[:], zf_d[u, :, :], writes=["stg"], q="sync")
            P.op("act", lambda e: e.activation(out=ft[:], in_=stg[:], func=AF.Sigmoid), reads=["stg"], writes=["ft"])
            if use_lb:
                P.op("dve", lambda e: e.tensor_scalar(out=ft[:], in0=ft[:], scalar1=oml[:, 0:1], scalar2=lb[:, 0:1], op0=ALU.mult, op1=ALU.add), reads=["ft", "oml", "lb"], writes=["ft"])
            P.dma(stg[:], zq_d[u, :, :], writes=["stg"], q="sync")
            P.op("act", lambda e: e.activation(out=qt[:], in_=stg[:], func=AF.Silu), reads=["stg"], writes=["qt"])
            P.dma(vt32[:, 0:S], vT_d[u, :, :], writes=["vt32"], q="sync")
            P.op("dve", lambda e: e.memset(vt32[:, S:S + 1], 0.0), reads=["vt32"], writes=["vt32"])
            P.op("dve", lambda e: e.tensor_tensor(out=stg[:, 0:S], in0=vt32[:, 0:S], in1=vt32[:, 1:S + 1], op=ALU.subtract), reads=["vt32", "stg"], writes=["stg"])
            P.op("dve", lambda e: e.tensor_copy(out=vTb[:, 0:S], in_=stg[:, 0:S]), reads=["stg"], writes=["vTb"])
            P.op("dve", lambda e: e.tensor_tensor(out=vTl[:, 0:S], in0=stg[:, 0:S], in1=vTb[:, 0:S], op=ALU.subtract), reads=["stg", "vTb"], writes=["vTl"])
            P.op("dve", lambda e: e.tensor_scalar(out=Dg[:], in0=io[:, 0, :], scalar1=vt32[:, 0:1], scalar2=-1.0, op0=ALU.mult, op1=ALU.mult), reads=["io", "vt32"], writes=["Dg"])
            P.op("pe", lambda e: e.matmul(qsp[:, 0:128], lhsT=io[:, 1, :], rhs=Dg[:], start=True, stop=True), reads=["io", "Dg"], writes=["qsp"])
            P.op("act", lambda e: e.activation(out=state[:], in_=qsp[:, 0:128], func=AF.Copy), reads=["qsp"], writes=[("state", vi) for vi in range(128)])
            items = []
            for t0 in range(0, S, 512):
                n = min(512, S - t0)
                ob = cc % 2; cc += 1
                for vi in range(128):
                    items.append((t0, n, ob, vi))
            G = len(items)

            def stage(k, g):
                t0, n, ob, vi = items[g]
                b = g % NB; pb = g % NBP
                if k == 0:
                    P.op("pe", lambda e: e.matmul(vbp[pb][:, 0:n], lhsT=sel[:, vi, :], rhs=vTb[:, t0:t0 + n], start=True, stop=False), reads=["sel", "vTb"], writes=[("vbp", pb)])
                    P.op("pe", lambda e: e.matmul(vbp[pb][:, 0:n], lhsT=sel[:, vi, :], rhs=vTl[:, t0:t0 + n], start=False, stop=True), reads=["sel", "vTl"], writes=[("vbp", pb)])
                elif k == 1:
                    P.op("dve", lambda e: e.tensor_tensor_scan(out=Sv[b][:, 0:n], data0=ft[:, t0:t0 + n], data1=vbp[pb][:, 0:n], initial=state[:, vi:vi + 1], op0=ALU.mult, op1=ALU.add),
                         reads=["ft", ("vbp", pb), ("state", vi)], writes=[("Sv", b)])
                elif k == 2:
                    P.op("act", lambda e: e.activation(out=state[:, vi:vi + 1], in_=Sv[b][:, n - 1:n], func=AF.Copy), reads=[("Sv", b)], writes=[("state", vi)])
                    qeng = "dve" if (HGQ and vi % HGQ == 0) else "pool"
                    P.op(qeng, lambda e: e.tensor_tensor(out=qs[b][:, 0:n], in0=qt[:, t0:t0 + n], in1=Sv[b][:, 0:n], op=ALU.mult), reads=["qt", ("Sv", b)], writes=[("qs", b)])
                elif k == 3:
                    P.op("pe", lambda e: e.matmul(opp[ob][:, 0:n], lhsT=selT[:, vi, :], rhs=qs[b][:, 0:n], start=(vi == 0), stop=(vi == 127)), reads=["selT", ("qs", b)], writes=[("opp", ob)])
                    if vi == 127:
                        P.op("pe", lambda e: e.matmul(qsp[:, 0:n], lhsT=io[:, 1, :], rhs=qt[:, t0:t0 + n], start=True, stop=True), reads=["io", "qt"], writes=["qsp"])
                        P.op("dve", lambda e: e.tensor_tensor(out=tmpc[:, 0:n], in0=vt32[:, t0 + 1:t0 + n + 1], in1=qsp[:, 0:n], op=ALU.mult), reads=["vt32", "qsp"], writes=["tmpc"])
                        P.op("dve", lambda e: e.tensor_tensor(out=ot[ob][:, 0:n], in0=opp[ob][:, 0:n], in1=tmpc[:, 0:n], op=ALU.add), reads=[("opp", ob), "tmpc"], writes=[("ot", ob)])
                        P.dma(o_d[u, :, t0:t0 + n], ot[ob][:, 0:n], reads=[("ot", ob)], q="sync")

            for step in range(G + 3):
                for k in range(4):
                    g = step - k
                    if 0 <= g < G:
                        stage(k, g)
        P.finish(); P.emit()
    return nc


def hg_ref(zq, zf, v, lb):
    zq = zq.astype(np.float64); zf = zf.astype(np.float64); v = v.astype(np.float64)
    q = zq / (1 + np.exp(-zq)); f = lb + (1 - lb) / (1 + np.exp(-zf)); k = 1 - f
    St = np.zeros((128, 128)); o = np.zeros((zq.shape[0], 128))
    for t in range(zq.shape[0]):
        St = f[t][:, None] * St + k[t][:, None] * v[t][None, :]
        o[t] = q[t] @ St
    return o


_CACHE = {}


def _prog(key, fn):
    if key not in _CACHE:
        _CACHE[key] = fn()
    return _CACHE[key]


def _bc(a, shape):
    return np.ascontiguousarray(np.broadcast_to(a, shape)).astype(np.float32)


def _rows_core(xf, cf, c):
    b, h = c // 2, c % 2
    return np.ascontiguousarray(np.concatenate([xf[b, h * 2048:(h + 1) * 2048], cf[b, h * 128:(h + 1) * 128]], 0))


def _unrows(outs, W):
    xf = np.zeros((4, 4096, W), np.float32); cf = np.zeros((4, 256, W), np.float32)
    for c in range(8):
        b, h = c // 2, c % 2
        xf[b, h * 2048:(h + 1) * 2048] = outs[c][:2048]
        cf[b, h * 128:(h + 1) * 128] = outs[c][2048:]
    return xf, cf


def _modT(m, l, b):
    mods = np.stack([m[l, b].reshape(6, 1024), m[l, 4].reshape(6, 1024)], 0)
    modT = np.ascontiguousarray(mods.reshape(2, 6, 8, 128).transpose(3, 0, 1, 2))
    gateB = _bc(mods[:, [2, 5], :][None], (128, 2, 2, 1024))
    return modT, gateB


def _run(nc, maps):
    res = run_bass_kernel_spmd(nc, maps, core_ids=list(range(8)))
    return res.results


def kernel(x, c, ctx, c_ctx, ada_w, ada_b, ln_g, ln_b, e_w_in, e_w_out, na_rpb, hg_lb_logits, hg_norm_g,
           ffn_w1, ffn_w3, ffn_w2, o_w_in, o_w_out, ret_log_decay, router_w, router_b, moe_w1, moe_w3, moe_w2):
    f32 = np.float32
    A = lambda a: np.ascontiguousarray(np.asarray(a, dtype=f32))
    x = A(x); ctx = A(ctx)
    ident = np.eye(128, dtype=f32)
    nc_ada = _prog("ada", build_ada)
    cc = np.concatenate([A(c), A(c_ctx)[None]], 0)
    cT = np.ascontiguousarray(cc.T.reshape(8, 128, 5).transpose(1, 0, 2))
    maps = []
    for core in range(8):
        l, h = core // 2, core % 2
        maps.append({"cT": cT, "w": A(ada_w[l][:, h * 3072:(h + 1) * 3072]), "b": A(ada_b[l][None, h * 3072:(h + 1) * 3072])})
    r = _run(nc_ada, maps)
    m = np.zeros((4, 5, 6144), f32)
    for core in range(8):
        l, h = core // 2, core % 2
        m[l][:, h * 3072:(h + 1) * 3072] = r[core]["m"]

    def dense(post, pre, l_post, l_pre, xf, cf, extra):
        nc = _prog(("dense", post, pre), lambda: build_dense(post, pre))
        maps = []
        for core in range(8):
            b = core // 2
            d = {"x": _rows_core(xf, cf, core), "ident": ident}
            if post:
                modT, gateB = _modT(m, l_post, b)
                d["modTp"] = modT; d["gateB"] = gateB
                d["lnGB"] = _bc(np.stack([A(ln_g[l_post]), A(ln_b[l_post])], 1)[None], (128, 2, 2, 1024))
                for k_, v_ in extra.items():
                    if isinstance(v_, tuple):
                        d[k_] = _rows_core(v_[0], v_[1], core)
                    else:
                        d[k_] = v_
            if pre:
                modT, _ = _modT(m, l_pre, b)
                d["modTq"] = modT
                d["win"] = A(e_w_in[l_pre // 2]) if pre == "even" else A(o_w_in[l_pre // 2])
            maps.append(d)
        r = _run(nc, maps)
        xo = co = px = pc = None
        if post:
            xo, co = _unrows([r[c_]["xo"] for c_ in range(8)], 1024)
        if pre:
            NO = 4096 if pre == "even" else 6144
            px, pc = _unrows([r[c_]["proj"] for c_ in range(8)], NO)
        return xo, co, px, pc

    _, _, px, pc = dense(None, "even", None, 0, x, ctx, {})
    for l in range(4):
        j = l // 2
        nxt = None if l == 3 else ("odd" if l % 2 == 0 else "even")
        if l % 2 == 0:
            nc_na = _prog("na", build_na)
            maps = []
            for h in range(8):
                Bg, M = na_tables(A(na_rpb[j][h]))
                sl = lambda a, o: a[:, :, o + h * 64:o + (h + 1) * 64]
                qcat = np.concatenate([sl(pc, 0), sl(px, 0)], 1)
                kcat = np.concatenate([sl(pc, 512), sl(px, 512)], 1)
                vcat = np.concatenate([sl(pc, 1024), sl(px, 1024)], 1)
                maps.append({"qT": np.ascontiguousarray(qcat.transpose(0, 2, 1)), "kT": np.ascontiguousarray(kcat.transpose(0, 2, 1)),
                             "v": np.ascontiguousarray(vcat), "Bg": Bg, "M": M})
            r = _run(nc_na, maps)
            a_x = np.zeros((4, 4096, 512), f32); a_c = np.zeros((4, 256, 512), f32)
            for h in range(8):
                o = r[h]["o"]
                a_x[:, :, h * 64:(h + 1) * 64] = o[:, 256:]; a_c[:, :, h * 64:(h + 1) * 64] = o[:, :256]
            nc_hg = _prog(("hg", j), lambda: build_hg(j == 1))
            hc = hg_consts()
            maps = []
            for core in range(8):
                zq = np.zeros((4, 128, 4352), f32); zf = np.zeros((4, 128, 4352), f32); vT = np.zeros((4, 128, 4352), f32); lbl = np.zeros((4, 128, 2), f32)
                for i in range(4):
                    n = core * 4 + i
                    b, h, dr = n // 8, (n // 2) % 4, n % 2
                    def seq(o):
                        cpart = pc[b][:, o + h * 128:o + (h + 1) * 128]; xpart = px[b][:, o + h * 128:o + (h + 1) * 128]
                        if dr == 1:
                            cpart = cpart[::-1]; xpart = xpart[::-1]
                        return np.concatenate([cpart, xpart], 0).T
                    zq[i] = seq(1536); zf[i] = seq(2048 + 512 * dr); vT[i] = seq(3072)
                    lbl[i] = A(hg_lb_logits[dr][:, h * 128:(h + 1) * 128]).T
                d = dict(hc); d.update(zqT=zq, zfT=zf, vT=vT, lbl=lbl)
                maps.append(d)
            r = _run(nc_hg, maps)
            of_x = np.zeros((4, 4096, 512), f32); ob_x = np.zeros((4, 4096, 512), f32)
            of_c = np.zeros((4, 256, 512), f32); ob_c = np.zeros((4, 256, 512), f32)
            for core in range(8):
                for i in range(4):
                    n = core * 4 + i
                    b, h, dr = n // 8, (n // 2) % 4, n % 2
                    o = r[core]["oT"][i].T
                    oc_, ox_ = o[:256], o[256:]
                    if dr == 1:
                        ob_c[b][:, h * 128:(h + 1) * 128] = oc_[::-1]; ob_x[b][:, h * 128:(h + 1) * 128] = ox_[::-1]
                    else:
                        of_c[b][:, h * 128:(h + 1) * 128] = oc_; of_x[b][:, h * 128:(h + 1) * 128] = ox_
            extra = {"ax": (a_x, a_c), "of": (of_x, of_c), "ob": (ob_x, ob_c), "gr": (px[:, :, 3584:4096], pc[:, :, 3584:4096]),
                     "ngB": _bc(np.tile(A(hg_norm_g[j]), 4)[None], (128, 512)), "wout": A(e_w_out[j]),
                     "w1": A(ffn_w1[j])[None], "w3": A(ffn_w3[j])[None], "w2": A(ffn_w2[j])[None]}
            x, ctx, px, pc = dense("even", nxt, l, l + 1, x, ctx, extra)
        else:
            nc_ret = _prog("ret", build_ret)
            rc = ret_consts()
            perm = np.concatenate([(np.arange(128) + 64) % 128, 128 + (np.arange(128) + 64) % 128])
            maps = []
            for core in range(8):
                qT = np.zeros((2, 2, 128, 4352), f32); qsT = np.zeros_like(qT); kT = np.zeros_like(qT); ksT = np.zeros_like(qT)
                vv = np.zeros((2, 4352, 512), f32); ld = np.zeros((2, 2), f32)
                for i in range(2):
                    n = core * 2 + i
                    b, h = n // 4, n % 4
                    qq = np.concatenate([pc[b][:, h * 256:(h + 1) * 256], px[b][:, h * 256:(h + 1) * 256]], 0)
                    kk = np.concatenate([pc[b][:, 1024 + h * 256:1024 + (h + 1) * 256], px[b][:, 1024 + h * 256:1024 + (h + 1) * 256]], 0)
                    qT[i] = qq.T.reshape(2, 128, 4352); qsT[i] = qq[:, perm].T.reshape(2, 128, 4352)
                    kT[i] = kk.T.reshape(2, 128, 4352); ksT[i] = kk[:, perm].T.reshape(2, 128, 4352)
                    vv[i] = np.concatenate([pc[b][:, 2048 + h * 512:2048 + (h + 1) * 512], px[b][:, 2048 + h * 512:2048 + (h + 1) * 512]], 0)
                    ld[i] = A(ret_log_decay[j])[:, h]
                d = dict(rc); d.update(qT=qT, qsT=qsT, kT=kT, ksT=ksT, v=vv, ld=ld)
                maps.append(d)
            r = _run(nc_ret, maps)
            o_x = np.zeros((4, 4096, 2048), f32); o_c = np.zeros((4, 256, 2048), f32)
            for core in range(8):
                for i in range(2):
                    n = core * 2 + i
                    b, h = n // 4, n % 4
                    o = r[core]["o"][i]
                    o_c[b][:, h * 512:(h + 1) * 512] = o[:256]; o_x[b][:, h * 512:(h + 1) * 512] = o[256:]
            extra = {"o": (o_x, o_c), "gr": (px[:, :, 4096:6144], pc[:, :, 4096:6144]),
                     "rw": A(router_w[j]), "rbB": _bc(A(router_b[j])[None], (128, 8)), "wout": A(o_w_out[j]),
                     "w1": A(moe_w1[j]), "w3": A(moe_w3[j]), "w2": A(moe_w2[j])}
            x, ctx, px, pc = dense("odd", nxt, l, l + 1, x, ctx, extra)
    return x.astype(np.float32)
```

```python
from contextlib import ExitStack

import numpy as np
import concourse.bass as bass
import concourse.mybir as mybir
from concourse.bass_utils import run_bass_kernel_spmd

F32 = mybir.dt.float32
BF16 = mybir.dt.bfloat16
AF = mybir.ActivationFunctionType
ALU = mybir.AluOpType
AX = mybir.AxisListType


class Prog:
    ENGS = ("sync", "act", "dve", "pool", "pe")
    NDMA = 16
    EPOCH = 8192

    def __init__(self, nc, same_engine_sync=None):
        import os as _os
        if same_engine_sync is None:
            same_engine_sync = _os.environ.get("SAME_SYNC", "act,dve,pool")
        self.nc = nc
        self.ops = {e: [] for e in self.ENGS}
        self.cnt = {e: 0 for e in self.ENGS}
        self.last_w = {}
        self.readers = {}
        self.waited = {e: {} for e in self.ENGS}
        self.ndma = 0
        self.dma_hist = {}
        self.same = same_engine_sync
        self.final_events = []
        self.semnames = set()

    def _deps(self, eng, reads, writes):
        deps = set()
        for k in reads:
            lw = self.last_w.get(k)
            if lw is not None:
                deps.add(lw)
        for k in writes:
            lw = self.last_w.get(k)
            if lw is not None:
                deps.add(lw)
            for r in self.readers.get(k, ()):
                deps.add(r)
        out = []
        best = {}
        for (s, v) in deps:
            if s.split("#")[0] == eng and eng not in self.same:
                continue
            if best.get(s, 0) < v:
                best[s] = v
        for s, v in best.items():
            if self.waited[eng].get(s, 0) < v:
                self.waited[eng][s] = v
                out.append((s, v))
        return out

    def _commit(self, ev, reads, writes):
        for k in reads:
            self.readers.setdefault(k, []).append(ev)
        for k in writes:
            self.last_w[k] = ev
            self.readers[k] = []

    def op(self, eng, fn, reads=(), writes=()):
        waits = self._deps(eng, reads, writes)
        self.cnt[eng] += 1
        n = self.cnt[eng]
        ev = ("%s#%d" % (eng, (n - 1) // self.EPOCH), (n - 1) % self.EPOCH + 1)
        self.semnames.add(ev[0])
        self.ops[eng].append(("op", fn, waits, ev))
        self._commit(ev, reads, writes)
        return ev

    def dma(self, out, in_, reads=(), writes=(), q="sync", **kw):
        i = self.ndma
        self.ndma += 1
        s = "dma%d" % (i % self.NDMA)
        v = 16 * (i // self.NDMA + 1)
        waits = self._deps(q, reads, writes)
        if v > 16 and self.waited[q].get(s, 0) < v - 16:
            self.waited[q][s] = v - 16
            waits.append((s, v - 16))
        ev = (s, v)
        self.ops[q].append(("dma", (out, in_, kw), waits, ev))
        self._commit(ev, reads, writes)
        return ev

    def finish(self, eng="sync"):
        evs = []
        n = self.ndma
        for j in range(min(n, self.NDMA)):
            last = ((n - 1 - j) // self.NDMA) * self.NDMA + j
            evs.append(("dma%d" % j, 16 * (last // self.NDMA + 1)))
        for e in self.ENGS:
            n = self.cnt[e]
            if n > 0:
                evs.append(("%s#%d" % (e, (n - 1) // self.EPOCH), (n - 1) % self.EPOCH + 1))
        self.ops[eng].append(("wait", None, evs, None))

    def wait_all(self, eng, events):
        self.ops[eng].append(("wait", None, list(events), None))

    def emit(self):
        nc = self.nc
        from contextlib import ExitStack
        with ExitStack() as st:
            sems = {}
            for sn in sorted(self.semnames):
                sems[sn] = st.enter_context(nc.semaphore("s_" + sn.replace("#", "_")))
            for i in range(self.NDMA):
                sems["dma%d" % i] = st.enter_context(nc.semaphore("s_dma%d" % i))
            block = st.enter_context(nc.Block())
            emap = {"sync": block.sync, "act": block.scalar, "dve": block.vector,
                    "pool": block.gpsimd, "pe": block.tensor}

            def make(ename):
                def body(eng):
                    for kind, fn, waits, ev in self.ops[ename]:
                        for (s, v) in waits:
                            eng.wait_ge(sems[s], v)
                        if kind == "op":
                            ins = fn(eng)
                            ins.then_inc(sems[ev[0]], 1)
                        elif kind == "dma":
                            out, in_, kw = fn
                            eng.dma_start(out=out, in_=in_, **kw).then_inc(sems[ev[0]], 16)
                return body
            for e in self.ENGS:
                if self.ops[e]:
                    emap[e](make(e))


def build_ada():
    nc = bass.Bass("TRN2", target_bir_lowering=False)
    cT = nc.dram_tensor("cT", [128, 8, 5], F32, kind="ExternalInput").ap()
    w = nc.dram_tensor("w", [1024, 3072], F32, kind="ExternalInput").ap()
    b = nc.dram_tensor("b", [1, 3072], F32, kind="ExternalInput").ap()
    m = nc.dram_tensor("m", [5, 3072], F32, kind="ExternalOutput").ap()
    with ExitStack() as st:
        sb = lambda n, s, d: st.enter_context(nc.sbuf_tensor(n, s, d))
        ct = sb("ct", [128, 8, 5], F32); sT = sb("sT", [128, 8, 5], F32)
        wt = [sb("wt%d" % i, [128, 8, 512], F32) for i in range(2)]
        bt = sb("bt", [5, 3072], F32); ot = sb("ot", [5, 3072], F32)
        ps = [st.enter_context(nc.psum_tensor("ps%d" % i, [5, 512], F32)) for i in range(2)]
        P = Prog(nc)
        P.dma(ct[:], cT[:, :, :], writes=["ct"])
        P.dma(bt[:], b[0:1, :].to_broadcast([5, 3072]), writes=["bt"])
        P.op("act", lambda e: e.activation(out=sT[:], in_=ct[:], func=AF.Silu), reads=["ct"], writes=["sT"])
        wv = w.rearrange("(k p) n -> p k n", p=128)
        for j in range(6):
            bi = j % 2
            P.dma(wt[bi][:], wv[:, :, j * 512:(j + 1) * 512], writes=[("wt", bi)], q="sync" if bi == 0 else "pool")
            for k in range(8):
                P.op("pe", lambda e, k=k, bi=bi: e.matmul(ps[bi][:], lhsT=sT[:, k, :], rhs=wt[bi][:, k, :], start=(k == 0), stop=(k == 7)),
                     reads=["sT", ("wt", bi)], writes=[("ps", bi)])
            P.op("dve", lambda e, j=j, bi=bi: e.tensor_tensor(out=ot[:, j * 512:(j + 1) * 512], in0=ps[bi][:], in1=bt[:, j * 512:(j + 1) * 512], op=ALU.add),
                 reads=[("ps", bi), "bt"], writes=["ot"])
        e1 = P.dma(m[:, :], ot[:], reads=["ot"])
        P.wait_all("sync", [e1])
        P.emit()
    return nc

def run_ada(c, c_ctx, ada_w, ada_b):
    nc = build_ada()
    cc = np.concatenate([c, c_ctx[None]], 0)
    cT = np.ascontiguousarray(cc.T.reshape(8, 128, 5).transpose(1, 0, 2))
    maps = []
    for core in range(8):
        l, h = core // 2, core % 2
        maps.append({"cT": cT, "w": np.ascontiguousarray(ada_w[l][:, h * 3072:(h + 1) * 3072]),
                     "b": np.ascontiguousarray(ada_b[l][None, h * 3072:(h + 1) * 3072])})
    res = run_bass_kernel_spmd(nc, maps, core_ids=list(range(8)))
    out = np.zeros((4, 5, 6144), np.float32)
    for core in range(8):
        l, h = core // 2, core % 2
        out[l][:, h * 3072:(h + 1) * 3072] = res.results[core]["m"]
    return out


import os
DBGMODE = os.environ.get('DENSE_DBG', '')

ALPHA_C = (2 * 4) ** 0.25
NT = 17
GROUPS = [[0, 1, 2, 3, 4, 5], [6, 7, 8, 9, 10, 11], [12, 13, 14, 15, 16]]
GT = 6
LN_EPS = 1e-5
DBG = False
NORM_EPS = 1e-6


def build_dense(post, pre, ntiles=NT, groups=GROUPS):
    nc = bass.Bass("TRN2", target_bir_lowering=False)
    R = ntiles * 128
    din = lambda n, s: nc.dram_tensor(n, s, F32, kind="ExternalInput").ap()
    x_d = din("x", [R, 1024])
    ident_d = din("ident", [128, 128])
    if post:
        modTp_d = din("modTp", [128, 2, 6, 8])
        gateB_d = din("gateB", [128, 2, 2, 1024])
        lnGB_d = din("lnGB", [128, 2, 2, 1024])
        xo_d = nc.dram_tensor("xo", [R, 1024], F32, kind="ExternalOutput").ap()
        if DBG:
            dbg_d = nc.dram_tensor("dbg", [R, 1024], F32, kind="ExternalOutput").ap()
            dbg3_d = nc.dram_tensor("dbg3", [128, 1024], F32, kind="ExternalOutput").ap()
            dbg2_d = nc.dram_tensor("dbg2", [R, 1024], F32, kind="ExternalOutput").ap()
        if post == "even":
            ax_d = din("ax", [R, 512]); of_d = din("of", [R, 512]); ob_d = din("ob", [R, 512]); gr_d = din("gr", [R, 512])
            ngB_d = din("ngB", [128, 512])
            CM = 8; NE = 1; FD = 2816
        else:
            o_d = din("o", [R, 2048]); gr_d = din("gr", [R, 2048])
            rw_d = din("rw", [1024, 8]); rbB_d = din("rbB", [128, 8])
            CM = 16; NE = 8; FD = 3584
        wout_d = din("wout", [CM * 128, 1024])
        w1_d = din("w1", [NE, 1024, FD]); w3_d = din("w3", [NE, 1024, FD]); w2_d = din("w2", [NE, FD, 1024])
    if pre:
        modTq_d = din("modTq", [128, 2, 6, 8])
        NO = 4096 if pre == "even" else 6144
        win_d = din("win", [1024, NO])
        proj_d = nc.dram_tensor("proj", [R, NO], F32, kind="ExternalOutput").ap()

    with ExitStack() as st:
        sb = lambda n, s, d: st.enter_context(nc.sbuf_tensor(n, s, d))
        pst = lambda n, s: st.enter_context(nc.psum_tensor(n, s, F32))
        P = Prog(nc)
        X = sb("X", [128, GT, 1024], F32)
        hT = sb("hT", [128, 8, GT * 128], BF16)
        ident = sb("ident_s", [128, 128], F32)
        wbf = [sb("wbf%d" % i, [128, 4096], BF16) for i in range(5)]
        tr = [pst("tr%d" % i, [128, 512]) for i in range(2)]
        h1p = [pst("h1p%d" % i, [128, 512]) for i in range(2)]
        h3p = [pst("h3p%d" % i, [128, 512]) for i in range(2)]
        yp = [pst("yp%d" % i, [128, 512]) for i in range(2)]
        stats = sb("stats", [128, 24], F32)
        mv = sb("mv", [128, 4, 2], F32)
        rstd = sb("rstd", [128, 4], F32)
        tmp = sb("tmp", [128, 1024], F32)
        P.dma(ident[:], ident_d[:, :], writes=["ident"])
        if post:
            modTp = sb("modTp_s", [128, 2, 6, 8], F32)
            gateB = sb("gateB_s", [128, 2, 2, 1024], F32)
            lnGB = sb("lnGB_s", [128, 2, 2, 1024], F32)
            Yacc = sb("Yacc", [128, GT, 1024], F32)
            aT = sb("aT", [128, 4, GT * 128], BF16)
            s1 = [sb("s1_%d" % i, [128, 512], F32) for i in range(2)]
            woutb = sb("woutb", [128, CM, 1024], BF16)
            mixT = sb("mixT", [128, CM, 128], BF16)
            P.dma(modTp[:], modTp_d[:, :, :, :], writes=["modTp"])
            P.dma(gateB[:], gateB_d[:, :, :, :], writes=["gateB"])
            P.dma(lnGB[:], lnGB_d[:, :, :, :], writes=["lnGB"])
            P.op("dve", lambda e: e.tensor_scalar_add(out=modTp[:, :, 4, :], in0=modTp[:, :, 4, :], scalar1=1.0), reads=["modTp"], writes=["modTp"])
            if post == "even":
                ngB = sb("ngB_s", [128, 512], F32)
                P.dma(ngB[:], ngB_d[:, :], writes=["ngB"])
                mi = [sb("mi%d" % i, [128, 512], F32) for i in range(4)]
                mixtm = sb("mixtm", [128, 1024], F32)
            else:
                rw = sb("rw_s", [128, 8, 8], F32)
                rbB = sb("rbB_s", [128, 8], F32)
                P.dma(rw[:], rw_d.rearrange("(k p) n -> p k n", p=128), writes=["rw"])
                P.dma(rbB[:], rbB_d[:, :], writes=["rbB"])
                mo = sb("mo", [128, 2048], F32); mg = sb("mg", [128, 2048], F32)
                hT32 = sb("hT32", [128, 8, 128], F32)
                lg = sb("lg", [128, 8], F32); m8 = sb("m8", [128, 8], F32); cwt = sb("cwt", [128, GT, 8], F32)
                nm1 = sb("nm1", [128, 1], F32); den = sb("den", [128, 1], F32)
        if pre:
            modTq = sb("modTq_s", [128, 2, 6, 8], F32)
            P.dma(modTq[:], modTq_d[:, :, :, :], writes=["modTq"])
            P.op("dve", lambda e: e.tensor_scalar_add(out=modTq[:, :, 1, :], in0=modTq[:, :, 1, :], scalar1=1.0), reads=["modTq"], writes=["modTq"])
            po = [sb("po%d" % i, [128, 512], F32) for i in range(1)]

        wctr = [0]
        pbc = [0]
        cast_rr = [0]

        def load_w(src_ap, n_free):
            i = wctr[0]; wctr[0] += 1
            b = i % 5
            dview = wbf[b][:, 0:n_free]
            if len(src_ap.shape) == 3:
                dview = dview.rearrange("p (a b) -> p a b", b=src_ap.shape[2])
            P.dma(dview, src_ap, writes=[("wbf", b)], q="pool")
            return wbf[b], ("wbf", b)

        def rsqrt_(dst, src, eps, rkeys, mul=1.0):
            P.op("dve", lambda e: e.tensor_scalar(out=dst, in0=src, scalar1=mul, scalar2=eps, op0=ALU.mult, op1=ALU.add), reads=rkeys, writes=["rstd"])
            P.op("act", lambda e: e.activation(out=dst, in_=dst, func=AF.Sqrt), reads=["rstd"], writes=["rstd"])
            P.op("dve", lambda e: e.reciprocal(out=dst, in_=dst), reads=["rstd"], writes=["rstd"])

        def layer_norm(Xt, xkey, li):
            for c in range(2):
                P.op("dve", lambda e, c=c: e.bn_stats(out=stats[:, c * 6:(c + 1) * 6], in_=Xt[:, c * 512:(c + 1) * 512]), reads=[xkey], writes=["stats"])
            P.op("dve", lambda e: e.bn_aggr(out=mv[:, 0, :], in_=stats[:, 0:12]), reads=["stats"], writes=["mv"])
            rsqrt_(rstd[:, 0:1], mv[:, 0, 1:2], LN_EPS, ["mv"])
            P.op("dve", lambda e: e.tensor_scalar(out=Xt, in0=Xt, scalar1=mv[:, 0, 0:1], scalar2=rstd[:, 0:1], op0=ALU.subtract, op1=ALU.mult), reads=[xkey, "mv", "rstd"], writes=[xkey])
            P.op("dve", lambda e: e.tensor_tensor(out=Xt, in0=Xt, in1=lnGB[:, li, 0, :], op=ALU.mult), reads=[xkey, "lnGB"], writes=[xkey])
            P.op("dve", lambda e: e.tensor_tensor(out=Xt, in0=Xt, in1=lnGB[:, li, 1, :], op=ALU.add), reads=[xkey, "lnGB"], writes=[xkey])

        def make_hT(i, tset, modT, mkey_, isc, ish, want32=False):
            for k in range(8):
                tb = k % 2
                P.op("pe", lambda e, k=k, tb=tb: e.transpose(out=tr[tb][:, 0:128], in_=X[:, i, k * 128:(k + 1) * 128], identity=ident[:]),
                     reads=[("X", i), "ident"], writes=[("tr", tb)])
                P.op("act", lambda e, k=k, tb=tb: e.activation(out=hT[:, k, i * 128:(i + 1) * 128], in_=tr[tb][:, 0:128], func=AF.Identity,
                                                             scale=modT[:, tset, isc, k:k + 1], bias=modT[:, tset, ish, k:k + 1]),
                     reads=[("tr", tb), mkey_], writes=[("hT", i)])
                if want32:
                    P.op("act", lambda e, k=k, tb=tb: e.activation(out=hT32[:, k, :], in_=tr[tb][:, 0:128], func=AF.Identity,
                                                                 scale=modT[:, tset, isc, k:k + 1], bias=modT[:, tset, ish, k:k + 1]),
                         reads=[("tr", tb), mkey_], writes=["hT32"])

        for g, tiles in enumerate(groups):
            nt = len(tiles); T = nt * 128
            for i, t in enumerate(tiles):
                P.dma(X[:, i, :], x_d[t * 128:(t + 1) * 128, :], writes=[("X", i)])
            if post:
                wv = wout_d.rearrange("(c p) n -> p c n", p=128)
                for c0 in range(0, CM, 4):
                    P.dma(woutb[:, c0:c0 + 4, :], wv[:, c0:c0 + 4, :], writes=["woutb"], q="pool")
                for i, t in enumerate(tiles):
                    tset = 1 if t == ntiles - 1 else 0
                    rows = slice(t * 128, (t + 1) * 128)
                    if post == "even":
                        for j, d in enumerate((ax_d, of_d, ob_d, gr_d)):
                            P.dma(mi[j][:], d[rows, :], writes=[("mi", j)], q="sync")
                        P.op("dve", lambda e: e.tensor_tensor(out=mi[1][:], in0=mi[1][:], in1=mi[2][:], op=ALU.add), reads=[("mi", 1), ("mi", 2)], writes=[("mi", 1)])
                        for h in range(4):
                            P.op("act", lambda e, h=h: e.activation(out=tmp[:, h * 128:(h + 1) * 128], in_=mi[1][:, h * 128:(h + 1) * 128], func=AF.Square, accum_out=rstd[:, h:h + 1]),
                                 reads=[("mi", 1)], writes=["tmp", "rstd"])
                        rsqrt_(rstd[:, 0:4], rstd[:, 0:4], NORM_EPS, ["rstd"], mul=1.0 / 128)
                        P.op("act", lambda e: e.activation(out=mi[3][:], in_=mi[3][:], func=AF.Silu), reads=[("mi", 3)], writes=[("mi", 3)])
                        P.op("dve", lambda e: e.tensor_copy(out=mixtm[:, 0:512], in_=mi[0][:]), reads=[("mi", 0)], writes=["mixtm"])
                        for h in range(4):
                            P.op("dve", lambda e, h=h: e.scalar_tensor_tensor(out=mixtm[:, 512 + h * 128:512 + (h + 1) * 128], in0=mi[1][:, h * 128:(h + 1) * 128], scalar=rstd[:, h:h + 1],
                                                                            in1=ngB[:, h * 128:(h + 1) * 128], op0=ALU.mult, op1=ALU.mult), reads=[("mi", 1), "rstd", "ngB"], writes=["mixtm"])
                        P.op("dve", lambda e: e.tensor_tensor(out=mixtm[:, 512:1024], in0=mixtm[:, 512:1024], in1=mi[3][:], op=ALU.mult), reads=["mixtm", ("mi", 3)], writes=["mixtm"])
                        msrc, mkey = mixtm, "mixtm"
                    else:
                        P.dma(mo[:], o_d[rows, :], writes=["mo"], q="sync")
                        P.dma(mg[:], gr_d[rows, :], writes=["mg"], q="sync")
                        for h in range(4):
                            P.op("dve", lambda e, h=h: e.bn_stats(out=stats[:, h * 6:(h + 1) * 6], in_=mo[:, h * 512:(h + 1) * 512]), reads=["mo"], writes=["stats"])
                            P.op("dve", lambda e, h=h: e.bn_aggr(out=mv[:, h, :], in_=stats[:, h * 6:(h + 1) * 6]), reads=["stats"], writes=["mv"])
                        rsqrt_(rstd[:, 0:4], mv[:, :, 1], LN_EPS, ["mv"])
                        P.op("act", lambda e: e.activation(out=mg[:], in_=mg[:], func=AF.Silu), reads=["mg"], writes=["mg"])
                        for h in range(4):
                            P.op("dve", lambda e, h=h: e.tensor_scalar(out=mo[:, h * 512:(h + 1) * 512], in0=mo[:, h * 512:(h + 1) * 512], scalar1=mv[:, h, 0:1], scalar2=rstd[:, h:h + 1],
                                                                     op0=ALU.subtract, op1=ALU.mult), reads=["mo", "mv", "rstd"], writes=["mo"])
                        P.op("dve", lambda e: e.tensor_tensor(out=mo[:], in0=mo[:], in1=mg[:], op=ALU.mult), reads=["mo", "mg"], writes=["mo"])
                        msrc, mkey = mo, "mo"
                    for c in range(CM):
                        tb = c % 2
                        P.op("pe", lambda e, c=c, tb=tb, msrc=msrc: e.transpose(out=tr[tb][:, 0:128], in_=msrc[:, c * 128:(c + 1) * 128], identity=ident[:]),
                             reads=[mkey, "ident"], writes=[("tr", tb)])
                        P.op("act", lambda e, c=c, tb=tb: e.activation(out=mixT[:, c, :], in_=tr[tb][:, 0:128], func=AF.Copy), reads=[("tr", tb)], writes=["mixT"])
                    for hf in range(2):
                        for c in range(CM):
                            P.op("pe", lambda e, c=c, hf=hf: e.matmul(yp[hf][:], lhsT=mixT[:, c, :], rhs=woutb[:, c, hf * 512:(hf + 1) * 512], start=(c == 0), stop=(c == CM - 1)),
                                 reads=["mixT", "woutb"], writes=[("yp", hf)])
                        P.op("dve", lambda e, hf=hf, tset=tset: e.tensor_tensor(out=tmp[:, hf * 512:(hf + 1) * 512], in0=yp[hf][:], in1=gateB[:, tset, 0, hf * 512:(hf + 1) * 512], op=ALU.mult),
                             reads=[("yp", hf), "gateB"], writes=["tmp"])
                    P.op("dve", lambda e, i=i: e.scalar_tensor_tensor(out=X[:, i, :], in0=X[:, i, :], scalar=ALPHA_C, in1=tmp[:], op0=ALU.mult, op1=ALU.add),
                         reads=[("X", i), "tmp"], writes=[("X", i)])
                    layer_norm(X[:, i, :], ("X", i), 0)
                    if DBG:
                        P.dma(dbg_d[t * 128:(t + 1) * 128, :], X[:, i, :], reads=[("X", i)], q="sync")
                for i, t in enumerate(tiles):
                    tset = 1 if t == ntiles - 1 else 0
                    make_hT(i, tset, modTp, "modTp", 4, 3, want32=(post == "odd" and 'no32' not in DBGMODE))
                    P.op("dve", lambda e, i=i: e.tensor_scalar_mul(out=X[:, i, :], in0=X[:, i, :], scalar1=ALPHA_C), reads=[("X", i)], writes=[("X", i)])
                    if post == "odd" and 'noroute' not in DBGMODE:
                        for k in range(8):
                            P.op("pe", lambda e, k=k: e.matmul(yp[0][:, 0:8], lhsT=hT32[:, k, :], rhs=rw[:, k, :], start=(k == 0), stop=(k == 7)), reads=["hT32", "rw"], writes=[("yp", 0)])
                        P.op("dve", lambda e: e.tensor_tensor(out=lg[:], in0=yp[0][:, 0:8], in1=rbB[:], op=ALU.add), reads=[("yp", 0), "rbB"], writes=["lg"])
                        P.op("dve", lambda e: e.max(out=m8[:], in_=lg[:]), reads=["lg"], writes=["m8"])
                        P.op("dve", lambda e: e.tensor_scalar_mul(out=nm1[:], in0=m8[:, 0:1], scalar1=-1.0), reads=["m8"], writes=["nm1"])
                        P.op("act", lambda e, i=i: e.activation(out=cwt[:, i, :], in_=lg[:], func=AF.Exp, bias=nm1[:, 0:1], scale=1.0), reads=["lg", "nm1"], writes=[("cw", i)])
                        P.op("dve", lambda e: e.tensor_scalar(out=lg[:], in0=lg[:], scalar1=m8[:, 1:2], scalar2=None, op0=ALU.is_ge), reads=["lg", "m8"], writes=["lg"])
                        P.op("dve", lambda e, i=i: e.tensor_tensor(out=cwt[:, i, :], in0=cwt[:, i, :], in1=lg[:], op=ALU.mult), reads=[("cw", i), "lg"], writes=[("cw", i)])
                        P.op("dve", lambda e, i=i: e.reduce_sum(out=den[:], in_=cwt[:, i, :], axis=AX.X), reads=[("cw", i)], writes=["den"])
                        P.op("dve", lambda e: e.reciprocal(out=den[:], in_=den[:]), reads=["den"], writes=["den"])
                        P.op("dve", lambda e, i=i: e.tensor_scalar_mul(out=cwt[:, i, :], in0=cwt[:, i, :], scalar1=den[:, 0:1]), reads=[("cw", i), "den"], writes=[("cw", i)])
                first = True
                for ex in range(0 if 'noffn' in DBGMODE else NE):
                    for f0 in range(0, FD, 512):
                        fb = min(512, FD - f0); nch = fb // 128
                        w1b, k1 = load_w(w1_d[ex].rearrange("(k p) n -> p k n", p=128)[:, :, f0:f0 + fb], 8 * fb)
                        w3b, k3 = load_w(w3_d[ex].rearrange("(k p) n -> p k n", p=128)[:, :, f0:f0 + fb], 8 * fb)
                        w2b, k2 = load_w(w2_d[ex][f0:f0 + fb, :].rearrange("(c p) n -> p c n", p=128), nch * 1024)
                        w1v = w1b[:, 0:8 * fb].rearrange("p (k n) -> p k n", n=fb)
                        w3v = w3b[:, 0:8 * fb].rearrange("p (k n) -> p k n", n=fb)
                        w2v = w2b[:, 0:nch * 1024].rearrange("p (c n) -> p c n", n=1024)
                        hkeys = [("hT", i) for i in range(nt)]
                        subs = [(s0, min(512, T - s0)) for s0 in range(0, T, 512)]
                        for j in range(nch):
                            for (s0, sn) in subs:
                                pb = pbc[0] % 2; pbc[0] += 1
                                for k in range(8):
                                    P.op("pe", lambda e, j=j, k=k, pb=pb, w1v=w1v, s0=s0, sn=sn: e.matmul(h1p[pb][:, 0:sn], lhsT=w1v[:, k, j * 128:(j + 1) * 128], rhs=hT[:, k, s0:s0 + sn], start=(k == 0), stop=(k == 7)),
                                         reads=[k1] + hkeys, writes=[("h1p", pb)])
                                for k in range(8):
                                    P.op("pe", lambda e, j=j, k=k, pb=pb, w3v=w3v, s0=s0, sn=sn: e.matmul(h3p[pb][:, 0:sn], lhsT=w3v[:, k, j * 128:(j + 1) * 128], rhs=hT[:, k, s0:s0 + sn], start=(k == 0), stop=(k == 7)),
                                         reads=[k3] + hkeys, writes=[("h3p", pb)])
                                P.op("act", lambda e, pb=pb, sn=sn: e.activation(out=s1[pb][:, 0:sn], in_=h1p[pb][:, 0:sn], func=AF.Silu), reads=[("h1p", pb)], writes=[("s1", pb)])
                                P.op("dve", lambda e, pb=pb, j=j, s0=s0, sn=sn: e.tensor_tensor(out=aT[:, j, s0:s0 + sn], in0=s1[pb][:, 0:sn], in1=h3p[pb][:, 0:sn], op=ALU.mult), reads=[("s1", pb), ("h3p", pb)], writes=[("aT", j)])
                        akeys = [("aT", j) for j in range(nch)]
                        if DBG and g == 0 and ex == 0 and f0 == 0:
                            P.op("dve", lambda e: e.tensor_copy(out=tmp[:, 0:512], in_=aT[:, 0, :]), reads=[("aT", 0)], writes=["tmp"])
                            P.op("dve", lambda e: e.tensor_copy(out=tmp[:, 512:1024], in_=hT[:, 0, :]), reads=[("hT", 0), ("hT", 1), ("hT", 2), ("hT", 3)], writes=["tmp"])
                            P.dma(dbg3_d[:, :], tmp[:], reads=["tmp"], q="sync")
                        for i in range(nt):
                            for hf in range(2):
                                for j in range(nch):
                                    P.op("pe", lambda e, i=i, hf=hf, j=j, w2v=w2v, nch=nch: e.matmul(yp[hf][:], lhsT=aT[:, j, i * 128:(i + 1) * 128], rhs=w2v[:, j, hf * 512:(hf + 1) * 512], start=(j == 0), stop=(j == nch - 1)),
                                         reads=[k2] + akeys, writes=[("yp", hf)])
                                ya = Yacc[:, i, hf * 512:(hf + 1) * 512]
                                if post == "odd":
                                    if first:
                                        P.op("dve", lambda e, hf=hf, ya=ya, i=i, ex=ex: e.tensor_scalar_mul(out=ya, in0=yp[hf][:], scalar1=cwt[:, i, ex:ex + 1]), reads=[("yp", hf), ("cw", i)], writes=[("Y", i)])
                                    else:
                                        P.op("dve", lambda e, hf=hf, ya=ya, i=i, ex=ex: e.scalar_tensor_tensor(out=ya, in0=yp[hf][:], scalar=cwt[:, i, ex:ex + 1], in1=ya, op0=ALU.mult, op1=ALU.add),
                                             reads=[("yp", hf), ("cw", i), ("Y", i)], writes=[("Y", i)])
                                else:
                                    if first:
                                        P.op("dve", lambda e, hf=hf, ya=ya: e.tensor_copy(out=ya, in_=yp[hf][:]), reads=[("yp", hf)], writes=[("Y", i)])
                                    else:
                                        P.op("dve", lambda e, hf=hf, ya=ya: e.tensor_tensor(out=ya, in0=yp[hf][:], in1=ya, op=ALU.add), reads=[("yp", hf), ("Y", i)], writes=[("Y", i)])
                        first = False
                for i, t in enumerate(tiles):
                    tset = 1 if t == ntiles - 1 else 0
                    if 'noffn' in DBGMODE:
                        P.op("dve", lambda e, i=i: e.memset(Yacc[:, i, :], 0.0), writes=[("Y", i)])
                    if DBG:
                        P.dma(dbg2_d[t * 128:(t + 1) * 128, :], Yacc[:, i, :], reads=[("Y", i)], q="sync")
                    P.op("dve", lambda e, i=i, tset=tset: e.tensor_tensor(out=Yacc[:, i, :], in0=Yacc[:, i, :], in1=gateB[:, tset, 1, :], op=ALU.mult), reads=[("Y", i), "gateB"], writes=[("Y", i)])
                    P.op("dve", lambda e, i=i: e.tensor_tensor(out=X[:, i, :], in0=X[:, i, :], in1=Yacc[:, i, :], op=ALU.add), reads=[("X", i), ("Y", i)], writes=[("X", i)])
                    layer_norm(X[:, i, :], ("X", i), 1)
                    P.dma(xo_d[t * 128:(t + 1) * 128, :], X[:, i, :], reads=[("X", i)], q="sync")
            if pre:
                for i, t in enumerate(tiles):
                    tset = 1 if t == ntiles - 1 else 0
                    make_hT(i, tset, modTq, "modTq", 1, 0)
                wv = win_d.rearrange("(k p) n -> p k n", p=128)
                pc = 0
                for n0 in range(0, NO, 512):
                    wb_, wk = load_w(wv[:, :, n0:n0 + 512], 4096)
                    wvv = wb_[:, :].rearrange("p (k n) -> p k n", n=512)
                    for i, t in enumerate(tiles):
                        hf = pc % 2; ob = 0; pc += 1
                        for k in range(8):
                            P.op("pe", lambda e, i=i, k=k, hf=hf, wvv=wvv: e.matmul(yp[hf][:], lhsT=hT[:, k, i * 128:(i + 1) * 128], rhs=wvv[:, k, :], start=(k == 0), stop=(k == 7)),
                                 reads=[wk, ("hT", i)], writes=[("yp", hf)])
                        P.op("act", lambda e, hf=hf, ob=ob: e.activation(out=po[ob][:], in_=yp[hf][:], func=AF.Copy), reads=[("yp", hf)], writes=[("po", ob)])
                        P.dma(proj_d[t * 128:(t + 1) * 128, n0:n0 + 512], po[ob][:], reads=[("po", ob)], q="sync")
        P.finish()
        P.emit()
    return nc


S_RET = 4352
NU_RET = 2


def ret_consts():
    f32 = np.float32
    ki = np.arange(128)[:, None]; qi = np.arange(512)[None, :]
    Mqk = (qi - ki).astype(f32)
    Rp = np.zeros((4, 128, 512), f32); Rn = np.zeros((4, 128, 512), f32); Eq = np.zeros((4, 128, 512), f32)
    for v in range(4):
        d = qi - ki - 128 * v
        Rp[v] = np.maximum(d, 0); Rn[v] = np.maximum(-d, 0); Eq[v] = (d == 0)
    strc = np.ascontiguousarray(np.stack([Rp, Rn, Eq], 0).transpose(2, 0, 1, 3))
    t = np.arange(4096); rows = t // 64; cols = t % 64
    jj = np.arange(128) % 64
    inv = (10000.0 ** (-(jj.astype(np.float64)) / 64))
    cosT = np.ones((2, 128, S_RET), f32); sinT = np.zeros((2, 128, S_RET), f32)
    for c, pos in enumerate((rows, cols)):
        ang = (pos[None, :].astype(np.float32) * inv.astype(np.float32)[:, None]).astype(np.float32)
        cosT[c, :, 256:] = np.cos(ang)
        sgn = np.where(np.arange(128) < 64, -1.0, 1.0)[:, None]
        sinT[c, :, 256:] = np.sin(ang) * sgn
    n128 = (128.0 * np.arange(48)).astype(f32)
    return {"Mqk": Mqk, "strc": strc, "cosT": cosT, "sinT": sinT,
            "n128": np.ascontiguousarray(np.broadcast_to(n128[None], (128, 48)))}


def build_ret():
    nc = bass.Bass("TRN2", target_bir_lowering=False)
    din = lambda n, s: nc.dram_tensor(n, s, F32, kind="ExternalInput").ap()
    S = S_RET
    qT_d = din("qT", [NU_RET, 2, 128, S]); qsT_d = din("qsT", [NU_RET, 2, 128, S])
    kT_d = din("kT", [NU_RET, 2, 128, S]); ksT_d = din("ksT", [NU_RET, 2, 128, S])
    v_d = din("v", [NU_RET, S, 512]); ld_d = din("ld", [NU_RET, 2])
    Mqk_d = din("Mqk", [128, 512]); strc_d = din("strc", [128, 3, 4, 512])
    cosT_d = din("cosT", [2, 128, S]); sinT_d = din("sinT", [2, 128, S]); n128_d = din("n128", [128, 48])
    o_d = nc.dram_tensor("o", [NU_RET, S, 512], F32, kind="ExternalOutput").ap()
    NTK = S // 128
    with ExitStack() as st:
        sb = lambda n, s, d: st.enter_context(nc.sbuf_tensor(n, s, d))
        pst = lambda n, s: st.enter_context(nc.psum_tensor(n, s, F32))
        P = Prog(nc)
        Mqk = sb("Mqk_s", [128, 512], F32); strc = sb("strc_s", [128, 3, 4, 512], F32); n128 = sb("n128_s", [128, 48], F32)
        P.dma(Mqk[:], Mqk_d[:, :], writes=["Mqk"]); P.dma(strc[:], strc_d[:, :, :, :], writes=["strc"]); P.dma(n128[:], n128_d[:, :], writes=["n128"])
        qr = sb("qr", [128, 2, S], BF16); kr = sb("kr", [128, 2, S], BF16); vb = sb("vb", [128, NTK, 512], BF16)
        CH = 1088
        ra = sb("ra", [128, CH], F32); rb_ = sb("rb", [128, CH], F32); rc = sb("rc", [128, CH], F32); rd = sb("rd", [128, CH], F32)
        vst = [sb("vst%d" % i, [128, 512], F32) for i in range(2)]
        ld = sb("ld_s", [128, 2], F32); nld = sb("nld", [128, 2], F32)
        bF = sb("bF", [128, 48], F32); bB = sb("bB", [128, 48], F32)
        Dstr = sb("Dstr", [128, 4, 512], F32); e1 = sb("e1", [128, 512], F32); e2 = sb("e2", [128, 512], F32)
        mk = [sb("mk%d" % i, [128, 512], F32) for i in range(2)]; mk2 = [sb("mk2_%d" % i, [128, 512], F32) for i in range(2)]
        At = [sb("At%d" % i, [128, 512], BF16) for i in range(2)]
        ot = [sb("ot%d" % i, [128, 512], F32) for i in range(2)]
        sp = [pst("sp%d" % i, [128, 512]) for i in range(2)]
        op_ = [pst("op%d" % i, [128, 512]) for i in range(4)]
        octr = [0]
        for u in range(NU_RET):
            P.dma(ld[:], ld_d[u:u + 1, :].to_broadcast([128, 2]), writes=["ld"])
            P.op("dve", lambda e: e.tensor_scalar(out=nld[:], in0=ld[:], scalar1=-1.0, scalar2=None, op0=ALU.mult), reads=["ld"], writes=["nld"])
            P.op("dve", lambda e: e.tensor_scalar(out=bF[:], in0=n128[:], scalar1=ld[:, 0:1], scalar2=None, op0=ALU.mult), reads=["ld", "n128"], writes=["bF"])
            P.op("dve", lambda e: e.tensor_scalar(out=bB[:], in0=n128[:], scalar1=ld[:, 1:2], scalar2=None, op0=ALU.mult), reads=["ld", "n128"], writes=["bB"])
            for v in range(4):
                P.op("act", lambda e, v=v: e.activation(out=e1[:], in_=strc[:, 0, v, :], func=AF.Exp, scale=ld[:, 0:1]), reads=["strc", "ld"], writes=["e1"])
                P.op("act", lambda e, v=v: e.activation(out=e2[:], in_=strc[:, 1, v, :], func=AF.Exp, scale=ld[:, 1:2]), reads=["strc", "ld"], writes=["e2"])
                P.op("dve", lambda e, v=v: e.tensor_tensor(out=Dstr[:, v, :], in0=e1[:], in1=e2[:], op=ALU.mult), reads=["e1", "e2"], writes=["Dstr"])
                P.op("dve", lambda e, v=v: e.tensor_tensor(out=Dstr[:, v, :], in0=Dstr[:, v, :], in1=strc[:, 2, v, :], op=ALU.add), reads=["Dstr", "strc"], writes=["Dstr"])
            for (src, ssrc, dst, dkey) in ((qT_d, qsT_d, qr, "qr"), (kT_d, ksT_d, kr, "kr")):
                for c in range(2):
                    for t0 in range(0, S, CH):
                        P.dma(ra[:], src[u, c, :, t0:t0 + CH], writes=["ra"], q="sync")
                        P.dma(rb_[:], ssrc[u, c, :, t0:t0 + CH], writes=["rb"], q="pool")
                        P.dma(rc[:], cosT_d[c, :, t0:t0 + CH], writes=["rc"], q="sync")
                        P.dma(rd[:], sinT_d[c, :, t0:t0 + CH], writes=["rd"], q="pool")
                        P.op("dve", lambda e: e.tensor_tensor(out=ra[:], in0=ra[:], in1=rc[:], op=ALU.mult), reads=["ra", "rc"], writes=["ra"])
                        P.op("pool", lambda e: e.tensor_tensor(out=rb_[:], in0=rb_[:], in1=rd[:], op=ALU.mult), reads=["rb", "rd"], writes=["rb"])
                        P.op("dve", lambda e, dst=dst, c=c, t0=t0: e.tensor_tensor(out=dst[:, c, t0:t0 + CH], in0=ra[:], in1=rb_[:], op=ALU.add), reads=["ra", "rb"], writes=[dkey])
            for t in range(NTK):
                s_ = t % 2
                P.dma(vst[s_][:], v_d[u, t * 128:(t + 1) * 128, :], writes=[("vst", s_)], q="sync" if s_ == 0 else "pool")
                P.op("act", lambda e, t=t, s_=s_: e.activation(out=vb[:, t, :], in_=vst[s_][:], func=AF.Copy), reads=[("vst", s_)], writes=["vb"])
            blocks = [(0, 256, True)] + [(256 + 512 * i, 512, False) for i in range(8)]
            bc = 0
            blist = []
            for (q0, nq, isctx) in blocks:
                nsub = nq // 128
                ktiles = [0, 1] if isctx else list(range(NTK))
                for ki_, kt in enumerate(ktiles):
                    blist.append((q0, nq, isctx, nsub, ki_, kt, len(ktiles), bc % 2)); bc += 1

            def emitS(blk):
                q0, nq, isctx, nsub, ki_, kt, nk, pb = blk
                for c in range(2):
                    P.op("pe", lambda e, c=c: e.matmul(sp[pb][:, 0:nq], lhsT=kr[:, c, kt * 128:(kt + 1) * 128], rhs=qr[:, c, q0:q0 + nq], start=(c == 0), stop=(c == 1)),
                         reads=["qr", "kr"], writes=[("sp", pb)])

            def emitMid(blk):
                q0, nq, isctx, nsub, ki_, kt, nk, pb = blk
                if isctx:
                    msk = Dstr[:, kt, 0:nq]; mkeys = ["Dstr"]
                else:
                    if kt < 2:
                        qx0 = q0 - 256
                        nf = (qx0 - (kt * 128 - 256)) // 128
                        nb = ((4096 + kt * 128) - qx0) // 128
                        P.op("act", lambda e: e.activation(out=mk[pb][:], in_=Mqk[:], func=AF.Exp, scale=ld[:, 0:1], bias=bF[:, nf:nf + 1]), reads=["Mqk", "ld", "bF"], writes=[("mk", pb)])
                        P.op("act", lambda e: e.activation(out=mk2[pb][:], in_=Mqk[:], func=AF.Exp, scale=nld[:, 1:2], bias=bB[:, nb:nb + 1]), reads=["Mqk", "nld", "bB"], writes=[("mk2", pb)])
                        P.op("pool", lambda e: e.tensor_tensor(out=mk[pb][:], in0=mk[pb][:], in1=mk2[pb][:], op=ALU.add), reads=[("mk", pb), ("mk2", pb)], writes=[("mk", pb)])
                        msk = mk[pb][:, :]; mkeys = [("mk", pb)]
                    else:
                        k0 = kt * 128
                        if q0 >= k0 + 128:
                            n = (q0 - k0) // 128
                            P.op("act", lambda e: e.activation(out=mk[pb][:], in_=Mqk[:], func=AF.Exp, scale=ld[:, 0:1], bias=bF[:, n:n + 1]), reads=["Mqk", "ld", "bF"], writes=[("mk", pb)])
                            msk = mk[pb][:, :]; mkeys = [("mk", pb)]
                        elif q0 + 512 <= k0:
                            n = (k0 - q0) // 128
                            P.op("act", lambda e: e.activation(out=mk[pb][:], in_=Mqk[:], func=AF.Exp, scale=nld[:, 1:2], bias=bB[:, n:n + 1]), reads=["Mqk", "nld", "bB"], writes=[("mk", pb)])
                            msk = mk[pb][:, :]; mkeys = [("mk", pb)]
                        else:
                            v = (k0 - q0) // 128
                            msk = Dstr[:, v, :]; mkeys = ["Dstr"]
                P.op("dve", lambda e: e.scalar_tensor_tensor(out=At[pb][:, 0:nq], in0=sp[pb][:, 0:nq], scalar=1.0 / 16, in1=msk, op0=ALU.mult, op1=ALU.mult),
                     reads=[("sp", pb)] + mkeys, writes=[("At", pb)])

            def emitPV(blk):
                q0, nq, isctx, nsub, ki_, kt, nk, pb = blk
                for s in range(nsub):
                    P.op("pe", lambda e, s=s: e.matmul(op_[s][:], lhsT=At[pb][:, s * 128:(s + 1) * 128], rhs=vb[:, kt, :], start=(ki_ == 0), stop=(ki_ == nk - 1)),
                         reads=[("At", pb), "vb"], writes=[("op", s)])
                if ki_ == nk - 1:
                    for s in range(nsub):
                        ob = octr[0] % 2; octr[0] += 1
                        P.op("act", lambda e, s=s, ob=ob: e.activation(out=ot[ob][:], in_=op_[s][:], func=AF.Copy), reads=[("op", s)], writes=[("ot", ob)])
                        P.dma(o_d[u, q0 + s * 128:q0 + (s + 1) * 128, :], ot[ob][:], reads=[("ot", ob)], q="sync")

            emitS(blist[0])
            for bi, blk in enumerate(blist):
                emitMid(blk)
                if bi + 1 < len(blist):
                    emitS(blist[bi + 1])
                emitPV(blk)
        P.finish(); P.emit()
    return nc


def ret_ref(q, k, v, ld):
    S = q.shape[0]
    A = (q.astype(np.float64) @ k.astype(np.float64).T) / 16
    pos_f = np.concatenate([np.arange(256) - 256, np.arange(4096)]).astype(np.float64)
    pos_b = np.concatenate([np.arange(256) + 4096, np.arange(4096)]).astype(np.float64)
    df = pos_f[:, None] - pos_f[None, :]
    db = pos_b[None, :] - pos_b[:, None]
    D = np.where(df >= 0, np.exp(ld[0] * np.maximum(df, 0)), 0) + np.where(db >= 0, np.exp(ld[1] * np.maximum(db, 0)), 0)
    D[:256, 256:] = 0
    return (A * D) @ v.astype(np.float64)


NU_NA = 4
S_NA = 4352
NEG = -30000.0


def na_variants():
    var_of = {}; reps = []
    for I in range(8):
        jlo, jhi = max(0, 4 * I - 2), min(31, 4 * I + 5)
        for j in range(jlo, jhi + 1):
            if I == 0:
                key = ("a", j)
            elif I == 7:
                key = ("z", j)
            else:
                key = ("m", j - 4 * I)
            if key not in var_of:
                var_of[key] = len(reps); reps.append((I, j))
    return var_of, reps


def na_tables(rpb_h):
    var_of, reps = na_variants()
    kp = np.arange(128); q = np.arange(512)
    a = kp // 64; kc = kp % 64; m = q // 64; qc = q % 64
    Bg = np.zeros((128, len(reps), 512), np.float32); M = np.zeros((128, len(reps), 512), np.float32)
    c_start = np.clip(qc - 8, 0, 48)
    for vi, (I, j) in enumerate(reps):
        kr = (2 * j + a)[:, None]; qr = (8 * I + m)[None, :]
        s = np.clip(qr - 4, 0, 56)
        rowok = (kr >= s) & (kr < s + 8)
        colok = (kc[:, None] >= c_start[None, :]) & (kc[:, None] < c_start[None, :] + 16)
        ok = rowok & colok
        dr = np.clip(kr - qr + 7, 0, 14); dc = np.clip(kc[:, None] - qc[None, :], -15, 15) + 15
        Bg[:, vi, :] = rpb_h[dr, dc]
        M[:, vi, :] = np.where(ok, 0.0, NEG)
    return Bg, M


def build_na():
    nc = bass.Bass("TRN2", target_bir_lowering=False)
    din = lambda n, s: nc.dram_tensor(n, s, F32, kind="ExternalInput").ap()
    S = S_NA
    qT_d = din("qT", [NU_NA, 64, S]); kT_d = din("kT", [NU_NA, 64, S]); v_d = din("v", [NU_NA, S, 64])
    Bg_d = din("Bg", [128, 20, 512]); M_d = din("M", [128, 20, 512])
    o_d = nc.dram_tensor("o", [NU_NA, S, 64], F32, kind="ExternalOutput").ap()
    var_of, reps = na_variants()
    with ExitStack() as st:
        sb = lambda n, s, d: st.enter_context(nc.sbuf_tensor(n, s, d))
        pst = lambda n, s: st.enter_context(nc.psum_tensor(n, s, F32))
        P = Prog(nc)
        Bt = sb("Bt", [128, 20, 512], F32)
        stg = [sb("stg%d" % i, [128, 2560], F32) for i in range(2)]
        for c in range(4):
            P.dma(Bt[:, c * 5:(c + 1) * 5, :], Bg_d[:, c * 5:(c + 1) * 5, :], writes=[("Bt", c)], q="sync")
            P.dma(stg[c % 2][:, :].rearrange("p (a b) -> p a b", b=512), M_d[:, c * 5:(c + 1) * 5, :], writes=[("stg", c % 2)], q="pool")
            P.op("dve", lambda e, c=c: e.tensor_tensor(out=Bt[:, c * 5:(c + 1) * 5, :], in0=Bt[:, c * 5:(c + 1) * 5, :], in1=stg[c % 2][:, :].rearrange("p (a b) -> p a b", b=512), op=ALU.add),
                 reads=[("Bt", c), ("stg", c % 2)], writes=[("Bt", c)])
        Bkeys = [("Bt", c) for c in range(4)]
        qb = sb("qb", [64, S], BF16); kb = sb("kb", [64, S], BF16); va = sb("va", [128, 34, 65], BF16)
        vst = sb("vstg", [128, 34, 64], F32)
        sbt = [sb("sbt%d" % i, [128, 512], F32) for i in range(2)]
        Et = [sb("Et%d" % i, [128, 512], BF16) for i in range(2)]
        rden = sb("rden", [128, 1], F32); ot = [sb("ot%d" % i, [128, 64], F32) for i in range(2)]
        sp = [pst("sp%d" % i, [128, 512]) for i in range(2)]
        op_ = [pst("op%d" % i, [128, 512]) for i in range(4)]
        P.op("pool", lambda e: e.memset(va[:, :, 64:65], 1.0), writes=["va"])
        octr = [0]; bc = 0
        for u in range(NU_NA):
            for (src, dst, dk) in ((qT_d, qb, "qb"), (kT_d, kb, "kb")):
                for h2 in range(2):
                    s_ = h2
                    P.dma(stg[s_][0:64, 0:2176], src[u, :, h2 * 2176:(h2 + 1) * 2176], writes=[("stg", s_)], q="sync" if h2 == 0 else "pool")
                    P.op("act", lambda e, dst=dst, h2=h2, s_=s_: e.activation(out=dst[:, h2 * 2176:(h2 + 1) * 2176], in_=stg[s_][0:64, 0:2176], func=AF.Copy), reads=[("stg", s_)], writes=[dk])
            P.dma(vst[:], v_d[u].rearrange("(t p) d -> p t d", p=128), writes=["vst"], q="sync")
            P.op("dve", lambda e: e.tensor_copy(out=va[:, :, 0:64], in_=vst[:]), reads=["vst"], writes=["va"])
            blocks = [("C", 0, 256)] + [(I, 256 + 512 * I, 512) for I in range(8)]
            blist = []
            for (I, q0, nq) in blocks:
                nsub = nq // 128
                if I == "C":
                    kts = [(0, None), (1, None)]
                else:
                    kts = [(0, None), (1, None)]
                    for j in range(max(0, 4 * I - 2), min(31, 4 * I + 5) + 1):
                        key = ("a", j) if I == 0 else (("z", j) if I == 7 else ("m", j - 4 * I))
                        kts.append((2 + j, var_of[key]))
                for ki_, (kt, vi) in enumerate(kts):
                    blist.append((q0, nq, nsub, ki_, kt, vi, len(kts), bc % 2)); bc += 1

            def emitS(blk):
                q0, nq, nsub, ki_, kt, vi, nk, pb = blk
                P.op("pe", lambda e: e.matmul(sp[pb][:, 0:nq], lhsT=kb[:, kt * 128:(kt + 1) * 128], rhs=qb[:, q0:q0 + nq], start=True, stop=True),
                     reads=["qb", "kb"], writes=[("sp", pb)])

            def emitMid(blk):
                q0, nq, nsub, ki_, kt, vi, nk, pb = blk
                if vi is None:
                    P.op("act", lambda e: e.activation(out=Et[pb][:, 0:nq], in_=sp[pb][:, 0:nq], func=AF.Exp, scale=0.125), reads=[("sp", pb)], writes=[("Et", pb)])
                else:
                    P.op("dve", lambda e: e.scalar_tensor_tensor(out=sbt[pb][:], in0=sp[pb][:], scalar=0.125, in1=Bt[:, vi, :], op0=ALU.mult, op1=ALU.add),
                         reads=[("sp", pb)] + Bkeys, writes=[("sbt", pb)])
                    P.op("act", lambda e: e.activation(out=Et[pb][:], in_=sbt[pb][:], func=AF.Exp), reads=[("sbt", pb)], writes=[("Et", pb)])

            def emitPV(blk):
                q0, nq, nsub, ki_, kt, vi, nk, pb = blk
                for s in range(nsub):
                    P.op("pe", lambda e, s=s: e.matmul(op_[s][:, 0:65], lhsT=Et[pb][:, s * 128:(s + 1) * 128], rhs=va[:, kt, :], start=(ki_ == 0), stop=(ki_ == nk - 1)),
                         reads=[("Et", pb), "va"], writes=[("op", s)])
                if ki_ == nk - 1:
                    for s in range(nsub):
                        ob = octr[0] % 2; octr[0] += 1
                        P.op("dve", lambda e, s=s: e.reciprocal(out=rden[:], in_=op_[s][:, 64:65]), reads=[("op", s)], writes=["rden"])
                        P.op("dve", lambda e, s=s, ob=ob: e.tensor_scalar(out=ot[ob][:], in0=op_[s][:, 0:64], scalar1=rden[:, 0:1], scalar2=None, op0=ALU.mult), reads=[("op", s), "rden"], writes=[("ot", ob)])
                        P.dma(o_d[u, q0 + s * 128:q0 + (s + 1) * 128, :], ot[ob][:], reads=[("ot", ob)], q="sync")

            emitS(blist[0])
            for bi, blk in enumerate(blist):
                emitMid(blk)
                if bi + 1 < len(blist):
                    emitS(blist[bi + 1])
                emitPV(blk)
        P.finish(); P.emit()
    return nc


def na_ref(q, k, v, qc, kc, vc, rpb_h):
    q = q.astype(np.float64) * 0.125; k = k.astype(np.float64); v = v.astype(np.float64)
    kc = kc.astype(np.float64); vc = vc.astype(np.float64)
    out = np.zeros((4096, 64))
    col = np.arange(64)
    for r in range(64):
        s = min(max(r - 4, 0), 56)
        keys = k[s * 64:(s + 8) * 64]; vals = v[s * 64:(s + 8) * 64]
        qq = q[r * 64:(r + 1) * 64]
        sc = qq @ keys.T
        kr = s + np.arange(8).repeat(64); kcol = np.tile(col, 8)
        cst = np.clip(col - 8, 0, 48)
        ok = (kcol[None, :] >= cst[:, None]) & (kcol[None, :] < cst[:, None] + 16)
        dr = kr - r + 7; dc = np.clip(kcol[None, :] - col[:, None], -15, 15) + 15
        sc = np.where(ok, sc + rpb_h[dr[None, :].repeat(64, 0), dc], -np.inf)
        scc = qq @ kc.T
        al = np.concatenate([sc, scc], 1); al = al - al.max(1, keepdims=True); p = np.exp(al); p /= p.sum(1, keepdims=True)
        out[r * 64:(r + 1) * 64] = p[:, :512] @ vals + p[:, 512:] @ vc
    sc = (qc.astype(np.float64) * 0.125) @ kc.T; sc -= sc.max(1, keepdims=True); p = np.exp(sc); p /= p.sum(1, keepdims=True)
    return out, p @ vc


import os
HGV = 2
HGQ = int(os.environ.get('HGQ', '0'))
NU_HG = 4
S_HG = 4352


def hg_consts():
    import ml_dtypes
    sel = np.zeros((128, 128, 128), np.float32)
    selT = np.zeros((128, 128, 128), np.float32)
    for vi in range(128):
        sel[vi, vi, :] = 1.0
        selT[:, vi, vi] = 1.0
    return {"sel": sel.reshape(128, 128 * 128), "selT": selT.reshape(128, 128 * 128),
            "io": np.ascontiguousarray(np.stack([np.eye(128, dtype=np.float32), np.ones((128, 128), np.float32)], 1))}


def build_hg(use_lb):
    nc = bass.Bass("TRN2", target_bir_lowering=False)
    din = lambda n, s: nc.dram_tensor(n, s, F32, kind="ExternalInput").ap()
    S = S_HG
    zq_d = din("zqT", [NU_HG, 128, S]); zf_d = din("zfT", [NU_HG, 128, S]); vT_d = din("vT", [NU_HG, 128, S]); lbl_d = din("lbl", [NU_HG, 128, 2])
    sel_d = din("sel", [128, 128 * 128]); selT_d = din("selT", [128, 128 * 128]); io_d = din("io", [128, 2, 128])
    o_d = nc.dram_tensor("oT", [NU_HG, 128, S], F32, kind="ExternalOutput").ap()
    with ExitStack() as st:
        sb = lambda n, s, d: st.enter_context(nc.sbuf_tensor(n, s, d))
        pst = lambda n, s: st.enter_context(nc.psum_tensor(n, s, F32))
        P = Prog(nc)
        sel = sb("sel_s", [128, 128, 128], BF16); selT = sb("selT_s", [128, 128, 128], BF16)
        stg = sb("stg", [128, 4352], F32)
        for (src, dst, dk) in ((sel_d, sel, "sel"), (selT_d, selT, "selT")):
            for c in range(4):
                P.dma(stg[:, 0:4096], src[:, c * 4096:(c + 1) * 4096], writes=["stg"], q="sync")
                P.op("dve", lambda e, dst=dst, c=c: e.tensor_copy(out=dst[:, c * 32:(c + 1) * 32, :], in_=stg[:, 0:4096].rearrange("p (a b) -> p a b", b=128)), reads=["stg"], writes=[dk])
        ft = sb("ft", [128, S], F32); qt = sb("qt", [128, S], F32); vTb = sb("vTb", [128, S], BF16); vTl = sb("vTl", [128, S], BF16); vt32 = sb("vt32", [128, S + 1], F32)
        io = sb("io_s", [128, 2, 128], F32); Dg = sb("Dg", [128, 128], F32)
        P.dma(io[:], io_d[:, :, :], writes=["io"])
        lbl = sb("lbl_s", [128, 2], F32); lb = sb("lb", [128, 1], F32); oml = sb("oml", [128, 1], F32)
        state = sb("state", [128, 128], F32)
        NB = 6; NBP = 4
        Sv = [sb("Sv_%d" % i, [128, 512], F32) for i in range(NB)]
        qs = [sb("qs_%d" % i, [128, 512], BF16) for i in range(NB)]
        ot = [sb("ot_%d" % i, [128, 512], F32) for i in range(2)]
        tmpc = sb("tmpc", [128, 512], F32)
        vbp = [pst("vbp%d" % i, [128, 512]) for i in range(NBP)]
        opp = [pst("opp%d" % i, [128, 512]) for i in range(2)]
        qsp = pst("qsp", [128, 512])
        cc = 0
        for u in range(NU_HG):
            if use_lb:
                P.dma(lbl[:], lbl_d[u, :, :], writes=["lbl"])
                P.op("dve", lambda e: e.tensor_tensor(out=lb[:], in0=lbl[:, 1:2], in1=lbl[:, 0:1], op=ALU.subtract), reads=["lbl"], writes=["lb"])
                P.op("act", lambda e: e.activation(out=lb[:], in_=lb[:], func=AF.Sigmoid), reads=["lb"], writes=["lb"])
                P.op("dve", lambda e: e.tensor_scalar(out=oml[:], in0=lb[:], scalar1=-1.0, scalar2=1.0, op0=ALU.mult, op1=ALU.add), reads=["lb"], writes=["oml"])
            P.dma(stg[:], zf_d[u, :, :], writes=["stg"], q="sync")
            P.op("act", lambda e: e.activation(out=ft[:], in_=stg[:], func=AF.Sigmoid), reads=["stg"], writes=["ft"])
            if use_lb:
                P.op("dve", lambda e: e.tensor_scalar(out=ft[:], in0=ft[:], scalar1=oml[:, 0:1], scalar2=lb[:, 0:1], op0=ALU.mult, op1=ALU.add), reads=["ft", "oml", "lb"], writes=["ft"])
            P.dma(stg[:], zq_d[u, :, :], writes=["stg"], q="sync")
            P.op("act", lambda e: e.activation(out=qt[:], in_=stg[:], func=AF.Silu), reads=["stg"], writes=["qt"])
            P.dma(vt32[:, 0:S], vT_d[u, :, :], writes=["vt32"], q="sync")
            P.op("dve", lambda e: e.memset(vt32[:, S:S + 1], 0.0), reads=["vt32"], writes=["vt32"])
            P.op("dve", lambda e: e.tensor_tensor(out=stg[:, 0:S], in0=vt32[:, 0:S], in1=vt32[:, 1:S + 1], op=ALU.subtract), reads=["vt32", "stg"], writes=["stg"])
            P.op("dve", lambda e: e.tensor_copy(out=vTb[:, 0:S], in_=stg[:, 0:S]), reads=["stg"], writes=["vTb"])
            P.op("dve", lambda e: e.tensor_tensor(out=vTl[:, 0:S], in0=stg[:, 0:S], in1=vTb[:, 0:S], op=ALU.subtract), reads=["stg", "vTb"], writes=["vTl"])
            P.op("dve", lambda e: e.tensor_scalar(out=Dg[:], in0=io[:, 0, :], scalar1=vt32[:, 0:1], scalar2=-1.0, op0=ALU.mult, op1=ALU.mult), reads=["io", "vt32"], writes=["Dg"])
            P.op("pe", lambda e: e.matmul(qsp[:, 0:128], lhsT=io[:, 1, :], rhs=Dg[:], start=True, stop=True), reads=["io", "Dg"], writes=["qsp"])
            P.op("act", lambda e: e.activation(out=state[:], in_=qsp[:, 0:128], func=AF.Copy), reads=["qsp"], writes=[("state", vi) for vi in range(128)])
            items = []
            for t0 in range(0, S, 512):
                n = min(512, S - t0)
                ob = cc % 2; cc += 1
                for vi in range(128):
                    items.append((t0, n, ob, vi))
            G = len(items)

            def stage(k, g):
                t0, n, ob, vi = items[g]
                b = g % NB; pb = g % NBP
                if k == 0:
                    P.op("pe", lambda e: e.matmul(vbp[pb][:, 0:n], lhsT=sel[:, vi, :], rhs=vTb[:, t0:t0 + n], start=True, stop=False), reads=["sel", "vTb"], writes=[("vbp", pb)])
                    P.op("pe", lambda e: e.matmul(vbp[pb][:, 0:n], lhsT=sel[:, vi, :], rhs=vTl[:, t0:t0 + n], start=False, stop=True), reads=["sel", "vTl"], writes=[("vbp", pb)])
                elif k == 1:
                    P.op("dve", lambda e: e.tensor_tensor_scan(out=Sv[b][:, 0:n], data0=ft[:, t0:t0 + n], data1=vbp[pb][:, 0:n], initial=state[:, vi:vi + 1], op0=ALU.mult, op1=ALU.add),
                         reads=["ft", ("vbp", pb), ("state", vi)], writes=[("Sv", b)])
                elif k == 2:
                    P.op("act", lambda e: e.activation(out=state[:, vi:vi + 1], in_=Sv[b][:, n - 1:n], func=AF.Copy), reads=[("Sv", b)], writes=[("state", vi)])
                    qeng = "dve" if (HGQ and vi % HGQ == 0) else "pool"
                    P.op(qeng, lambda e: e.tensor_tensor(out=qs[b][:, 0:n], in0=qt[:, t0:t0 + n], in1=Sv[b][:, 0:n], op=ALU.mult), reads=["qt", ("Sv", b)], writes=[("qs", b)])
                elif k == 3:
                    P.op("pe", lambda e: e.matmul(opp[ob][:, 0:n], lhsT=selT[:, vi, :], rhs=qs[b][:, 0:n], start=(vi == 0), stop=(vi == 127)), reads=["selT", ("qs", b)], writes=[("opp", ob)])
                    if vi == 127:
                        P.op("pe", lambda e: e.matmul(qsp[:, 0:n], lhsT=io[:, 1, :], rhs=qt[:, t0:t0 + n], start=True, stop=True), reads=["io", "qt"], writes=["qsp"])
                        P.op("dve", lambda e: e.tensor_tensor(out=tmpc[:, 0:n], in0=vt32[:, t0 + 1:t0 + n + 1], in1=qsp[:, 0:n], op=ALU.mult), reads=["vt32", "qsp"], writes=["tmpc"])
                        P.op("dve", lambda e: e.tensor_tensor(out=ot[ob][:, 0:n], in0=opp[ob][:, 0:n], in1=tmpc[:, 0:n], op=ALU.add), reads=[("opp", ob), "tmpc"], writes=[("ot", ob)])
                        P.dma(o_d[u, :, t0:t0 + n], ot[ob][:, 0:n], reads=[("ot", ob)], q="sync")

            for step in range(G + 3):
                for k in range(4):
                    g = step - k
                    if 0 <= g < G:
                        stage(k, g)
        P.finish(); P.emit()
    return nc


def hg_ref(zq, zf, v, lb):
    zq = zq.astype(np.float64); zf = zf.astype(np.float64); v = v.astype(np.float64)
    q = zq / (1 + np.exp(-zq)); f = lb + (1 - lb) / (1 + np.exp(-zf)); k = 1 - f
    St = np.zeros((128, 128)); o = np.zeros((zq.shape[0], 128))
    for t in range(zq.shape[0]):
        St = f[t][:, None] * St + k[t][:, None] * v[t][None, :]
        o[t] = q[t] @ St
    return o


_CACHE = {}


def _prog(key, fn):
    if key not in _CACHE:
        _CACHE[key] = fn()
    return _CACHE[key]


def _bc(a, shape):
    return np.ascontiguousarray(np.broadcast_to(a, shape)).astype(np.float32)


def _rows_core(xf, cf, c):
    b, h = c // 2, c % 2
    return np.ascontiguousarray(np.concatenate([xf[b, h * 2048:(h + 1) * 2048], cf[b, h * 128:(h + 1) * 128]], 0))


def _unrows(outs, W):
    xf = np.zeros((4, 4096, W), np.float32); cf = np.zeros((4, 256, W), np.float32)
    for c in range(8):
        b, h = c // 2, c % 2
        xf[b, h * 2048:(h + 1) * 2048] = outs[c][:2048]
        cf[b, h * 128:(h + 1) * 128] = outs[c][2048:]
    return xf, cf


def _modT(m, l, b):
    mods = np.stack([m[l, b].reshape(6, 1024), m[l, 4].reshape(6, 1024)], 0)
    modT = np.ascontiguousarray(mods.reshape(2, 6, 8, 128).transpose(3, 0, 1, 2))
    gateB = _bc(mods[:, [2, 5], :][None], (128, 2, 2, 1024))
    return modT, gateB


def _run(nc, maps):
    res = run_bass_kernel_spmd(nc, maps, core_ids=list(range(8)))
    return res.results


def kernel(x, c, ctx, c_ctx, ada_w, ada_b, ln_g, ln_b, e_w_in, e_w_out, na_rpb, hg_lb_logits, hg_norm_g,
           ffn_w1, ffn_w3, ffn_w2, o_w_in, o_w_out, ret_log_decay, router_w, router_b, moe_w1, moe_w3, moe_w2):
    f32 = np.float32
    A = lambda a: np.ascontiguousarray(np.asarray(a, dtype=f32))
    x = A(x); ctx = A(ctx)
    ident = np.eye(128, dtype=f32)
    nc_ada = _prog("ada", build_ada)
    cc = np.concatenate([A(c), A(c_ctx)[None]], 0)
    cT = np.ascontiguousarray(cc.T.reshape(8, 128, 5).transpose(1, 0, 2))
    maps = []
    for core in range(8):
        l, h = core // 2, core % 2
        maps.append({"cT": cT, "w": A(ada_w[l][:, h * 3072:(h + 1) * 3072]), "b": A(ada_b[l][None, h * 3072:(h + 1) * 3072])})
    r = _run(nc_ada, maps)
    m = np.zeros((4, 5, 6144), f32)
    for core in range(8):
        l, h = core // 2, core % 2
        m[l][:, h * 3072:(h + 1) * 3072] = r[core]["m"]

    def dense(post, pre, l_post, l_pre, xf, cf, extra):
        nc = _prog(("dense", post, pre), lambda: build_dense(post, pre))
        maps = []
        for core in range(8):
            b = core // 2
            d = {"x": _rows_core(xf, cf, core), "ident": ident}
            if post:
                modT, gateB = _modT(m, l_post, b)
                d["modTp"] = modT; d["gateB"] = gateB
                d["lnGB"] = _bc(np.stack([A(ln_g[l_post]), A(ln_b[l_post])], 1)[None], (128, 2, 2, 1024))
                for k_, v_ in extra.items():
                    if isinstance(v_, tuple):
                        d[k_] = _rows_core(v_[0], v_[1], core)
                    else:
                        d[k_] = v_
            if pre:
                modT, _ = _modT(m, l_pre, b)
                d["modTq"] = modT
                d["win"] = A(e_w_in[l_pre // 2]) if pre == "even" else A(o_w_in[l_pre // 2])
            maps.append(d)
        r = _run(nc, maps)
        xo = co = px = pc = None
        if post:
            xo, co = _unrows([r[c_]["xo"] for c_ in range(8)], 1024)
        if pre:
            NO = 4096 if pre == "even" else 6144
            px, pc = _unrows([r[c_]["proj"] for c_ in range(8)], NO)
        return xo, co, px, pc

    _, _, px, pc = dense(None, "even", None, 0, x, ctx, {})
    for l in range(4):
        j = l // 2
        nxt = None if l == 3 else ("odd" if l % 2 == 0 else "even")
        if l % 2 == 0:
            nc_na = _prog("na", build_na)
            maps = []
            for h in range(8):
                Bg, M = na_tables(A(na_rpb[j][h]))
                sl = lambda a, o: a[:, :, o + h * 64:o + (h + 1) * 64]
                qcat = np.concatenate([sl(pc, 0), sl(px, 0)], 1)
                kcat = np.concatenate([sl(pc, 512), sl(px, 512)], 1)
                vcat = np.concatenate([sl(pc, 1024), sl(px, 1024)], 1)
                maps.append({"qT": np.ascontiguousarray(qcat.transpose(0, 2, 1)), "kT": np.ascontiguousarray(kcat.transpose(0, 2, 1)),
                             "v": np.ascontiguousarray(vcat), "Bg": Bg, "M": M})
            r = _run(nc_na, maps)
            a_x = np.zeros((4, 4096, 512), f32); a_c = np.zeros((4, 256, 512), f32)
            for h in range(8):
                o = r[h]["o"]
                a_x[:, :, h * 64:(h + 1) * 64] = o[:, 256:]; a_c[:, :, h * 64:(h + 1) * 64] = o[:, :256]
            nc_hg = _prog(("hg", j), lambda: build_hg(j == 1))
            hc = hg_consts()
            maps = []
            for core in range(8):
                zq = np.zeros((4, 128, 4352), f32); zf = np.zeros((4, 128, 4352), f32); vT = np.zeros((4, 128, 4352), f32); lbl = np.zeros((4, 128, 2), f32)
                for i in range(4):
                    n = core * 4 + i
                    b, h, dr = n // 8, (n // 2) % 4, n % 2
                    def seq(o):
                        cpart = pc[b][:, o + h * 128:o + (h + 1) * 128]; xpart = px[b][:, o + h * 128:o + (h + 1) * 128]
                        if dr == 1:
                            cpart = cpart[::-1]; xpart = xpart[::-1]
                        return np.concatenate([cpart, xpart], 0).T
                    zq[i] = seq(1536); zf[i] = seq(2048 + 512 * dr); vT[i] = seq(3072)
                    lbl[i] = A(hg_lb_logits[dr][:, h * 128:(h + 1) * 128]).T
                d = dict(hc); d.update(zqT=zq, zfT=zf, vT=vT, lbl=lbl)
                maps.append(d)
            r = _run(nc_hg, maps)
            of_x = np.zeros((4, 4096, 512), f32); ob_x = np.zeros((4, 4096, 512), f32)
            of_c = np.zeros((4, 256, 512), f32); ob_c = np.zeros((4, 256, 512), f32)
            for core in range(8):
                for i in range(4):
                    n = core * 4 + i
                    b, h, dr = n // 8, (n // 2) % 4, n % 2
                    o = r[core]["oT"][i].T
                    oc_, ox_ = o[:256], o[256:]
                    if dr == 1:
                        ob_c[b][:, h * 128:(h + 1) * 128] = oc_[::-1]; ob_x[b][:, h * 128:(h + 1) * 128] = ox_[::-1]
                    else:
                        of_c[b][:, h * 128:(h + 1) * 128] = oc_; of_x[b][:, h * 128:(h + 1) * 128] = ox_
            extra = {"ax": (a_x, a_c), "of": (of_x, of_c), "ob": (ob_x, ob_c), "gr": (px[:, :, 3584:4096], pc[:, :, 3584:4096]),
                     "ngB": _bc(np.tile(A(hg_norm_g[j]), 4)[None], (128, 512)), "wout": A(e_w_out[j]),
                     "w1": A(ffn_w1[j])[None], "w3": A(ffn_w3[j])[None], "w2": A(ffn_w2[j])[None]}
            x, ctx, px, pc = dense("even", nxt, l, l + 1, x, ctx, extra)
        else:
            nc_ret = _prog("ret", build_ret)
            rc = ret_consts()
            perm = np.concatenate([(np.arange(128) + 64) % 128, 128 + (np.arange(128) + 64) % 128])
            maps = []
            for core in range(8):
                qT = np.zeros((2, 2, 128, 4352), f32); qsT = np.zeros_like(qT); kT = np.zeros_like(qT); ksT = np.zeros_like(qT)
                vv = np.zeros((2, 4352, 512), f32); ld = np.zeros((2, 2), f32)
                for i in range(2):
                    n = core * 2 + i
                    b, h = n // 4, n % 4
                    qq = np.concatenate([pc[b][:, h * 256:(h + 1) * 256], px[b][:, h * 256:(h + 1) * 256]], 0)
                    kk = np.concatenate([pc[b][:, 1024 + h * 256:1024 + (h + 1) * 256], px[b][:, 1024 + h * 256:1024 + (h + 1) * 256]], 0)
                    qT[i] = qq.T.reshape(2, 128, 4352); qsT[i] = qq[:, perm].T.reshape(2, 128, 4352)
                    kT[i] = kk.T.reshape(2, 128, 4352); ksT[i] = kk[:, perm].T.reshape(2, 128, 4352)
                    vv[i] = np.concatenate([pc[b][:, 2048 + h * 512:2048 + (h + 1) * 512], px[b][:, 2048 + h * 512:2048 + (h + 1) * 512]], 0)
                    ld[i] = A(ret_log_decay[j])[:, h]
                d = dict(rc); d.update(qT=qT, qsT=qsT, kT=kT, ksT=ksT, v=vv, ld=ld)
                maps.append(d)
            r = _run(nc_ret, maps)
            o_x = np.zeros((4, 4096, 2048), f32); o_c = np.zeros((4, 256, 2048), f32)
            for core in range(8):
                for i in range(2):
                    n = core * 2 + i
                    b, h = n // 4, n % 4
                    o = r[core]["o"][i]
                    o_c[b][:, h * 512:(h + 1) * 512] = o[:256]; o_x[b][:, h * 512:(h + 1) * 512] = o[256:]
            extra = {"o": (o_x, o_c), "gr": (px[:, :, 4096:6144], pc[:, :, 4096:6144]),
                     "rw": A(router_w[j]), "rbB": _bc(A(router_b[j])[None], (128, 8)), "wout": A(o_w_out[j]),
                     "w1": A(moe_w1[j]), "w3": A(moe_w3[j]), "w2": A(moe_w2[j])}
            x, ctx, px, pc = dense("odd", nxt, l, l + 1, x, ctx, extra)
    return x.astype(np.float32)
```

```python
from contextlib import ExitStack

import numpy as np
import concourse.bass as bass
import concourse.mybir as mybir
from concourse.bass_utils import run_bass_kernel_spmd

F32 = mybir.dt.float32
BF16 = mybir.dt.bfloat16
AF = mybir.ActivationFunctionType
ALU = mybir.AluOpType
AX = mybir.AxisListType


class Prog:
    ENGS = ("sync", "act", "dve", "pool", "pe")
    NDMA = 16
    EPOCH = 8192

    def __init__(self, nc, same_engine_sync=None):
        import os as _os
        if same_engine_sync is None:
            same_engine_sync = _os.environ.get("SAME_SYNC", "act,dve,pool")
        self.nc = nc
        self.ops = {e: [] for e in self.ENGS}
        self.cnt = {e: 0 for e in self.ENGS}
        self.last_w = {}
        self.readers = {}
        self.waited = {e: {} for e in self.ENGS}
        self.ndma = 0
        self.dma_hist = {}
        self.same = same_engine_sync
        self.final_events = []
        self.semnames = set()

    def _deps(self, eng, reads, writes):
        deps = set()
        for k in reads:
            lw = self.last_w.get(k)
            if lw is not None:
                deps.add(lw)
        for k in writes:
            lw = self.last_w.get(k)
            if lw is not None:
                deps.add(lw)
            for r in self.readers.get(k, ()):
                deps.add(r)
        out = []
        best = {}
        for (s, v) in deps:
            if s.split("#")[0] == eng and eng not in self.same:
                continue
            if best.get(s, 0) < v:
                best[s] = v
        for s, v in best.items():
            if self.waited[eng].get(s, 0) < v:
                self.waited[eng][s] = v
                out.append((s, v))
        return out

    def _commit(self, ev, reads, writes):
        for k in reads:
            self.readers.setdefault(k, []).append(ev)
        for k in writes:
            self.last_w[k] = ev
            self.readers[k] = []

    def op(self, eng, fn, reads=(), writes=()):
        waits = self._deps(eng, reads, writes)
        self.cnt[eng] += 1
        n = self.cnt[eng]
        ev = ("%s#%d" % (eng, (n - 1) // self.EPOCH), (n - 1) % self.EPOCH + 1)
        self.semnames.add(ev[0])
        self.ops[eng].append(("op", fn, waits, ev))
        self._commit(ev, reads, writes)
        return ev

    def dma(self, out, in_, reads=(), writes=(), q="sync", **kw):
        i = self.ndma
        self.ndma += 1
        s = "dma%d" % (i % self.NDMA)
        v = 16 * (i // self.NDMA + 1)
        waits = self._deps(q, reads, writes)
        if v > 16 and self.waited[q].get(s, 0) < v - 16:
            self.waited[q][s] = v - 16
            waits.append((s, v - 16))
        ev = (s, v)
        self.ops[q].append(("dma", (out, in_, kw), waits, ev))
        self._commit(ev, reads, writes)
        return ev

    def finish(self, eng="sync"):
        evs = []
        n = self.ndma
        for j in range(min(n, self.NDMA)):
            last = ((n - 1 - j) // self.NDMA) * self.NDMA + j
            evs.append(("dma%d" % j, 16 * (last // self.NDMA + 1)))
        for e in self.ENGS:
            n = self.cnt[e]
            if n > 0:
                evs.append(("%s#%d" % (e, (n - 1) // self.EPOCH), (n - 1) % self.EPOCH + 1))
        self.ops[eng].append(("wait", None, evs, None))

    def wait_all(self, eng, events):
        self.ops[eng].append(("wait", None, list(events), None))

    def emit(self):
        nc = self.nc
        from contextlib import ExitStack
        with ExitStack() as st:
            sems = {}
            for sn in sorted(self.semnames):
                sems[sn] = st.enter_context(nc.semaphore("s_" + sn.replace("#", "_")))
            for i in range(self.NDMA):
                sems["dma%d" % i] = st.enter_context(nc.semaphore("s_dma%d" % i))
            block = st.enter_context(nc.Block())
            emap = {"sync": block.sync, "act": block.scalar, "dve": block.vector,
                    "pool": block.gpsimd, "pe": block.tensor}

            def make(ename):
                def body(eng):
                    for kind, fn, waits, ev in self.ops[ename]:
                        for (s, v) in waits:
                            eng.wait_ge(sems[s], v)
                        if kind == "op":
                            ins = fn(eng)
                            ins.then_inc(sems[ev[0]], 1)
                        elif kind == "dma":
                            out, in_, kw = fn
                            eng.dma_start(out=out, in_=in_, **kw).then_inc(sems[ev[0]], 16)
                return body
            for e in self.ENGS:
                if self.ops[e]:
                    emap[e](make(e))


def build_ada():
    nc = bass.Bass("TRN2", target_bir_lowering=False)
    cT = nc.dram_tensor("cT", [128, 8, 5], F32, kind="ExternalInput").ap()
    w = nc.dram_tensor("w", [1024, 3072], F32, kind="ExternalInput").ap()
    b = nc.dram_tensor("b", [1, 3072], F32, kind="ExternalInput").ap()
    m = nc.dram_tensor("m", [5, 3072], F32, kind="ExternalOutput").ap()
    with ExitStack() as st:
        sb = lambda n, s, d: st.enter_context(nc.sbuf_tensor(n, s, d))
        ct = sb("ct", [128, 8, 5], F32); sT = sb("sT", [128, 8, 5], F32)
        wt = [sb("wt%d" % i, [128, 8, 512], F32) for i in range(2)]
        bt = sb("bt", [5, 3072], F32); ot = sb("ot", [5, 3072], F32)
        ps = [st.enter_context(nc.psum_tensor("ps%d" % i, [5, 512], F32)) for i in range(2)]
        P = Prog(nc)
        P.dma(ct[:], cT[:, :, :], writes=["ct"])
        P.dma(bt[:], b[0:1, :].to_broadcast([5, 3072]), writes=["bt"])
        P.op("act", lambda e: e.activation(out=sT[:], in_=ct[:], func=AF.Silu), reads=["ct"], writes=["sT"])
        wv = w.rearrange("(k p) n -> p k n", p=128)
        for j in range(6):
            bi = j % 2
            P.dma(wt[bi][:], wv[:, :, j * 512:(j + 1) * 512], writes=[("wt", bi)], q="sync" if bi == 0 else "pool")
            for k in range(8):
                P.op("pe", lambda e, k=k, bi=bi: e.matmul(ps[bi][:], lhsT=sT[:, k, :], rhs=wt[bi][:, k, :], start=(k == 0), stop=(k == 7)),
                     reads=["sT", ("wt", bi)], writes=[("ps", bi)])
            P.op("dve", lambda e, j=j, bi=bi: e.tensor_tensor(out=ot[:, j * 512:(j + 1) * 512], in0=ps[bi][:], in1=bt[:, j * 512:(j + 1) * 512], op=ALU.add),
                 reads=[("ps", bi), "bt"], writes=["ot"])
        e1 = P.dma(m[:, :], ot[:], reads=["ot"])
        P.wait_all("sync", [e1])
        P.emit()
    return nc

def run_ada(c, c_ctx, ada_w, ada_b):
    nc = build_ada()
    cc = np.concatenate([c, c_ctx[None]], 0)
    cT = np.ascontiguousarray(cc.T.reshape(8, 128, 5).transpose(1, 0, 2))
    maps = []
    for core in range(8):
        l, h = core // 2, core % 2
        maps.append({"cT": cT, "w": np.ascontiguousarray(ada_w[l][:, h * 3072:(h + 1) * 3072]),
                     "b": np.ascontiguousarray(ada_b[l][None, h * 3072:(h + 1) * 3072])})
    res = run_bass_kernel_spmd(nc, maps, core_ids=list(range(8)))
    out = np.zeros((4, 5, 6144), np.float32)
    for core in range(8):
        l, h = core // 2, core % 2
        out[l][:, h * 3072:(h + 1) * 3072] = res.results[core]["m"]
    return out


import os
DBGMODE = os.environ.get('DENSE_DBG', '')

ALPHA_C = (2 * 4) ** 0.25
NT = 17
GROUPS = [[0, 1, 2, 3, 4, 5], [6, 7, 8, 9, 10, 11], [12, 13, 14, 15, 16]]
GT = 6
LN_EPS = 1e-5
DBG = False
NORM_EPS = 1e-6


def build_dense(post, pre, ntiles=NT, groups=GROUPS):
    nc = bass.Bass("TRN2", target_bir_lowering=False)
    R = ntiles * 128
    din = lambda n, s: nc.dram_tensor(n, s, F32, kind="ExternalInput").ap()
    x_d = din("x", [R, 1024])
    ident_d = din("ident", [128, 128])
    if post:
        modTp_d = din("modTp", [128, 2, 6, 8])
        gateB_d = din("gateB", [128, 2, 2, 1024])
        lnGB_d = din("lnGB", [128, 2, 2, 1024])
        xo_d = nc.dram_tensor("xo", [R, 1024], F32, kind="ExternalOutput").ap()
        if DBG:
            dbg_d = nc.dram_tensor("dbg", [R, 1024], F32, kind="ExternalOutput").ap()
            dbg3_d = nc.dram_tensor("dbg3", [128, 1024], F32, kind="ExternalOutput").ap()
            dbg2_d = nc.dram_tensor("dbg2", [R, 1024], F32, kind="ExternalOutput").ap()
        if post == "even":
            ax_d = din("ax", [R, 512]); of_d = din("of", [R, 512]); ob_d = din("ob", [R, 512]); gr_d = din("gr", [R, 512])
            ngB_d = din("ngB", [128, 512])
            CM = 8; NE = 1; FD = 2816
        else:
            o_d = din("o", [R, 2048]); gr_d = din("gr", [R, 2048])
            rw_d = din("rw", [1024, 8]); rbB_d = din("rbB", [128, 8])
            CM = 16; NE = 8; FD = 3584
        wout_d = din("wout", [CM * 128, 1024])
        w1_d = din("w1", [NE, 1024, FD]); w3_d = din("w3", [NE, 1024, FD]); w2_d = din("w2", [NE, FD, 1024])
    if pre:
        modTq_d = din("modTq", [128, 2, 6, 8])
        NO = 4096 if pre == "even" else 6144
        win_d = din("win", [1024, NO])
        proj_d = nc.dram_tensor("proj", [R, NO], F32, kind="ExternalOutput").ap()

    with ExitStack() as st:
        sb = lambda n, s, d: st.enter_context(nc.sbuf_tensor(n, s, d))
        pst = lambda n, s: st.enter_context(nc.psum_tensor(n, s, F32))
        P = Prog(nc)
        X = sb("X", [128, GT, 1024], F32)
        hT = sb("hT", [128, 8, GT * 128], BF16)
        ident = sb("ident_s", [128, 128], F32)
        wbf = [sb("wbf%d" % i, [128, 4096], BF16) for i in range(5)]
        tr = [pst("tr%d" % i, [128, 512]) for i in range(2)]
        h1p = [pst("h1p%d" % i, [128, 512]) for i in range(2)]
        h3p = [pst("h3p%d" % i, [128, 512]) for i in range(2)]
        yp = [pst("yp%d" % i, [128, 512]) for i in range(2)]
        stats = sb("stats", [128, 24], F32)
        mv = sb("mv", [128, 4, 2], F32)
        rstd = sb("rstd", [128, 4], F32)
        tmp = sb("tmp", [128, 1024], F32)
        P.dma(ident[:], ident_d[:, :], writes=["ident"])
        if post:
            modTp = sb("modTp_s", [128, 2, 6, 8], F32)
            gateB = sb("gateB_s", [128, 2, 2, 1024], F32)
            lnGB = sb("lnGB_s", [128, 2, 2, 1024], F32)
            Yacc = sb("Yacc", [128, GT, 1024], F32)
            aT = sb("aT", [128, 4, GT * 128], BF16)
            s1 = [sb("s1_%d" % i, [128, 512], F32) for i in range(1 if post == "odd" else 2)]
            NS1 = 1 if post == "odd" else 2
            woutb = sb("woutb", [128, CM, 1024], BF16)
            mixT = sb("mixT", [128, CM, 128], BF16)
            P.dma(modTp[:], modTp_d[:, :, :, :], writes=["modTp"])
            P.dma(gateB[:], gateB_d[:, :, :, :], writes=["gateB"])
            P.dma(lnGB[:], lnGB_d[:, :, :, :], writes=["lnGB"])
            P.op("dve", lambda e: e.tensor_scalar_add(out=modTp[:, :, 4, :], in0=modTp[:, :, 4, :], scalar1=1.0), reads=["modTp"], writes=["modTp"])
            if post == "even":
                ngB = sb("ngB_s", [128, 512], F32)
                P.dma(ngB[:], ngB_d[:, :], writes=["ngB"])
                mi = [sb("mi%d" % i, [128, 512], F32) for i in range(4)]
                mixtm = sb("mixtm", [128, 1024], F32)
            else:
                rw = sb("rw_s", [128, 8, 8], F32)
                rbB = sb("rbB_s", [128, 8], F32)
                P.dma(rw[:], rw_d.rearrange("(k p) n -> p k n", p=128), writes=["rw"])
                P.dma(rbB[:], rbB_d[:, :], writes=["rbB"])
                mo = sb("mo", [128, 2048], F32); mg = sb("mg", [128, 2048], F32)
                hT32 = sb("hT32", [128, 8, 128], F32)
                lg = sb("lg", [128, 8], F32); m8 = sb("m8", [128, 8], F32); cwt = sb("cwt", [128, GT, 8], F32)
                nm1 = sb("nm1", [128, 1], F32); den = sb("den", [128, 1], F32)
        if pre:
            modTq = sb("modTq_s", [128, 2, 6, 8], F32)
            P.dma(modTq[:], modTq_d[:, :, :, :], writes=["modTq"])
            P.op("dve", lambda e: e.tensor_scalar_add(out=modTq[:, :, 1, :], in0=modTq[:, :, 1, :], scalar1=1.0), reads=["modTq"], writes=["modTq"])
            NPO = 2 if post == "odd" else 3
            po = [sb("po%d" % i, [128, 512], F32) for i in range(NPO)]

        wctr = [0]
        pbc = [0]
        cast_rr = [0]

        def load_w(src_ap, n_free):
            i = wctr[0]; wctr[0] += 1
            b = i % 5
            dview = wbf[b][:, 0:n_free]
            if len(src_ap.shape) == 3:
                dview = dview.rearrange("p (a b) -> p a b", b=src_ap.shape[2])
            P.dma(dview, src_ap, writes=[("wbf", b)], q="pool")
            return wbf[b], ("wbf", b)

        def rsqrt_(dst, src, eps, rkeys, mul=1.0):
            P.op("dve", lambda e: e.tensor_scalar(out=dst, in0=src, scalar1=mul, scalar2=eps, op0=ALU.mult, op1=ALU.add), reads=rkeys, writes=["rstd"])
            P.op("act", lambda e: e.activation(out=dst, in_=dst, func=AF.Sqrt), reads=["rstd"], writes=["rstd"])
            P.op("dve", lambda e: e.reciprocal(out=dst, in_=dst), reads=["rstd"], writes=["rstd"])

        def layer_norm(Xt, xkey, li):
            for c in range(2):
                P.op("dve", lambda e, c=c: e.bn_stats(out=stats[:, c * 6:(c + 1) * 6], in_=Xt[:, c * 512:(c + 1) * 512]), reads=[xkey], writes=["stats"])
            P.op("dve", lambda e: e.bn_aggr(out=mv[:, 0, :], in_=stats[:, 0:12]), reads=["stats"], writes=["mv"])
            rsqrt_(rstd[:, 0:1], mv[:, 0, 1:2], LN_EPS, ["mv"])
            P.op("dve", lambda e: e.tensor_scalar(out=Xt, in0=Xt, scalar1=mv[:, 0, 0:1], scalar2=rstd[:, 0:1], op0=ALU.subtract, op1=ALU.mult), reads=[xkey, "mv", "rstd"], writes=[xkey])
            P.op("dve", lambda e: e.tensor_tensor(out=Xt, in0=Xt, in1=lnGB[:, li, 0, :], op=ALU.mult), reads=[xkey, "lnGB"], writes=[xkey])
            P.op("dve", lambda e: e.tensor_tensor(out=Xt, in0=Xt, in1=lnGB[:, li, 1, :], op=ALU.add), reads=[xkey, "lnGB"], writes=[xkey])

        def make_hT(i, tset, modT, mkey_, isc, ish, want32=False):
            for k in range(8):
                tb = k % 2
                P.op("pe", lambda e, k=k, tb=tb: e.transpose(out=tr[tb][:, 0:128], in_=X[:, i, k * 128:(k + 1) * 128], identity=ident[:]),
                     reads=[("X", i), "ident"], writes=[("tr", tb)])
                P.op("act", lambda e, k=k, tb=tb: e.activation(out=hT[:, k, i * 128:(i + 1) * 128], in_=tr[tb][:, 0:128], func=AF.Identity,
                                                             scale=modT[:, tset, isc, k:k + 1], bias=modT[:, tset, ish, k:k + 1]),
                     reads=[("tr", tb), mkey_], writes=[("hT", i)])
                if want32:
                    P.op("act", lambda e, k=k, tb=tb: e.activation(out=hT32[:, k, :], in_=tr[tb][:, 0:128], func=AF.Identity,
                                                                 scale=modT[:, tset, isc, k:k + 1], bias=modT[:, tset, ish, k:k + 1]),
                         reads=[("tr", tb), mkey_], writes=["hT32"])

        for g, tiles in enumerate(groups):
            nt = len(tiles); T = nt * 128
            for i, t in enumerate(tiles):
                P.dma(X[:, i, :], x_d[t * 128:(t + 1) * 128, :], writes=[("X", i)])
            if post:
                wv = wout_d.rearrange("(c p) n -> p c n", p=128)
                for c0 in range(0, CM, 4):
                    P.dma(woutb[:, c0:c0 + 4, :], wv[:, c0:c0 + 4, :], writes=["woutb"], q="pool")
                for i, t in enumerate(tiles):
                    tset = 1 if t == ntiles - 1 else 0
                    rows = slice(t * 128, (t + 1) * 128)
                    if post == "even":
                        for j, d in enumerate((ax_d, of_d, ob_d, gr_d)):
                            P.dma(mi[j][:], d[rows, :], writes=[("mi", j)], q="sync")
                        P.op("dve", lambda e: e.tensor_tensor(out=mi[1][:], in0=mi[1][:], in1=mi[2][:], op=ALU.add), reads=[("mi", 1), ("mi", 2)], writes=[("mi", 1)])
                        for h in range(4):
                            P.op("act", lambda e, h=h: e.activation(out=tmp[:, h * 128:(h + 1) * 128], in_=mi[1][:, h * 128:(h + 1) * 128], func=AF.Square, accum_out=rstd[:, h:h + 1]),
                                 reads=[("mi", 1)], writes=["tmp", "rstd"])
                        rsqrt_(rstd[:, 0:4], rstd[:, 0:4], NORM_EPS, ["rstd"], mul=1.0 / 128)
                        P.op("act", lambda e: e.activation(out=mi[3][:], in_=mi[3][:], func=AF.Silu), reads=[("mi", 3)], writes=[("mi", 3)])
                        P.op("dve", lambda e: e.tensor_copy(out=mixtm[:, 0:512], in_=mi[0][:]), reads=[("mi", 0)], writes=["mixtm"])
                        for h in range(4):
                            P.op("dve", lambda e, h=h: e.scalar_tensor_tensor(out=mixtm[:, 512 + h * 128:512 + (h + 1) * 128], in0=mi[1][:, h * 128:(h + 1) * 128], scalar=rstd[:, h:h + 1],
                                                                            in1=ngB[:, h * 128:(h + 1) * 128], op0=ALU.mult, op1=ALU.mult), reads=[("mi", 1), "rstd", "ngB"], writes=["mixtm"])
                        P.op("dve", lambda e: e.tensor_tensor(out=mixtm[:, 512:1024], in0=mixtm[:, 512:1024], in1=mi[3][:], op=ALU.mult), reads=["mixtm", ("mi", 3)], writes=["mixtm"])
                        msrc, mkey = mixtm, "mixtm"
                    else:
                        P.dma(mo[:], o_d[rows, :], writes=["mo"], q="sync")
                        P.dma(mg[:], gr_d[rows, :], writes=["mg"], q="sync")
                        for h in range(4):
                            P.op("dve", lambda e, h=h: e.bn_stats(out=stats[:, h * 6:(h + 1) * 6], in_=mo[:, h * 512:(h + 1) * 512]), reads=["mo"], writes=["stats"])
                            P.op("dve", lambda e, h=h: e.bn_aggr(out=mv[:, h, :], in_=stats[:, h * 6:(h + 1) * 6]), reads=["stats"], writes=["mv"])
                        rsqrt_(rstd[:, 0:4], mv[:, :, 1], LN_EPS, ["mv"])
                        P.op("act", lambda e: e.activation(out=mg[:], in_=mg[:], func=AF.Silu), reads=["mg"], writes=["mg"])
                        for h in range(4):
                            P.op("dve", lambda e, h=h: e.tensor_scalar(out=mo[:, h * 512:(h + 1) * 512], in0=mo[:, h * 512:(h + 1) * 512], scalar1=mv[:, h, 0:1], scalar2=rstd[:, h:h + 1],
                                                                     op0=ALU.subtract, op1=ALU.mult), reads=["mo", "mv", "rstd"], writes=["mo"])
                        P.op("dve", lambda e: e.tensor_tensor(out=mo[:], in0=mo[:], in1=mg[:], op=ALU.mult), reads=["mo", "mg"], writes=["mo"])
                        msrc, mkey = mo, "mo"
                    for c in range(CM):
                        tb = c % 2
                        P.op("pe", lambda e, c=c, tb=tb, msrc=msrc: e.transpose(out=tr[tb][:, 0:128], in_=msrc[:, c * 128:(c + 1) * 128], identity=ident[:]),
                             reads=[mkey, "ident"], writes=[("tr", tb)])
                        P.op("act", lambda e, c=c, tb=tb: e.activation(out=mixT[:, c, :], in_=tr[tb][:, 0:128], func=AF.Copy), reads=[("tr", tb)], writes=["mixT"])
                    for hf in range(2):
                        for c in range(CM):
                            P.op("pe", lambda e, c=c, hf=hf: e.matmul(yp[hf][:], lhsT=mixT[:, c, :], rhs=woutb[:, c, hf * 512:(hf + 1) * 512], start=(c == 0), stop=(c == CM - 1)),
                                 reads=["mixT", "woutb"], writes=[("yp", hf)])
                        P.op("dve", lambda e, hf=hf, tset=tset: e.tensor_tensor(out=tmp[:, hf * 512:(hf + 1) * 512], in0=yp[hf][:], in1=gateB[:, tset, 0, hf * 512:(hf + 1) * 512], op=ALU.mult),
                             reads=[("yp", hf), "gateB"], writes=["tmp"])
                    P.op("dve", lambda e, i=i: e.scalar_tensor_tensor(out=X[:, i, :], in0=X[:, i, :], scalar=ALPHA_C, in1=tmp[:], op0=ALU.mult, op1=ALU.add),
                         reads=[("X", i), "tmp"], writes=[("X", i)])
                    layer_norm(X[:, i, :], ("X", i), 0)
                    if DBG:
                        P.dma(dbg_d[t * 128:(t + 1) * 128, :], X[:, i, :], reads=[("X", i)], q="sync")
                for i, t in enumerate(tiles):
                    tset = 1 if t == ntiles - 1 else 0
                    make_hT(i, tset, modTp, "modTp", 4, 3, want32=(post == "odd" and 'no32' not in DBGMODE))
                    P.op("dve", lambda e, i=i: e.tensor_scalar_mul(out=X[:, i, :], in0=X[:, i, :], scalar1=ALPHA_C), reads=[("X", i)], writes=[("X", i)])
                    if post == "odd" and 'noroute' not in DBGMODE:
                        for k in range(8):
                            P.op("pe", lambda e, k=k: e.matmul(yp[0][:, 0:8], lhsT=hT32[:, k, :], rhs=rw[:, k, :], start=(k == 0), stop=(k == 7)), reads=["hT32", "rw"], writes=[("yp", 0)])
                        P.op("dve", lambda e: e.tensor_tensor(out=lg[:], in0=yp[0][:, 0:8], in1=rbB[:], op=ALU.add), reads=[("yp", 0), "rbB"], writes=["lg"])
                        P.op("dve", lambda e: e.max(out=m8[:], in_=lg[:]), reads=["lg"], writes=["m8"])
                        P.op("dve", lambda e: e.tensor_scalar_mul(out=nm1[:], in0=m8[:, 0:1], scalar1=-1.0), reads=["m8"], writes=["nm1"])
                        P.op("act", lambda e, i=i: e.activation(out=cwt[:, i, :], in_=lg[:], func=AF.Exp, bias=nm1[:, 0:1], scale=1.0), reads=["lg", "nm1"], writes=[("cw", i)])
                        P.op("dve", lambda e: e.tensor_scalar(out=lg[:], in0=lg[:], scalar1=m8[:, 1:2], scalar2=None, op0=ALU.is_ge), reads=["lg", "m8"], writes=["lg"])
                        P.op("dve", lambda e, i=i: e.tensor_tensor(out=cwt[:, i, :], in0=cwt[:, i, :], in1=lg[:], op=ALU.mult), reads=[("cw", i), "lg"], writes=[("cw", i)])
                        P.op("dve", lambda e, i=i: e.reduce_sum(out=den[:], in_=cwt[:, i, :], axis=AX.X), reads=[("cw", i)], writes=["den"])
                        P.op("dve", lambda e: e.reciprocal(out=den[:], in_=den[:]), reads=["den"], writes=["den"])
                        P.op("dve", lambda e, i=i: e.tensor_scalar_mul(out=cwt[:, i, :], in0=cwt[:, i, :], scalar1=den[:, 0:1]), reads=[("cw", i), "den"], writes=[("cw", i)])
                first = True
                for ex in range(0 if 'noffn' in DBGMODE else NE):
                    for f0 in range(0, FD, 512):
                        fb = min(512, FD - f0); nch = fb // 128
                        w1b, k1 = load_w(w1_d[ex].rearrange("(k p) n -> p k n", p=128)[:, :, f0:f0 + fb], 8 * fb)
                        w3b, k3 = load_w(w3_d[ex].rearrange("(k p) n -> p k n", p=128)[:, :, f0:f0 + fb], 8 * fb)
                        w2b, k2 = load_w(w2_d[ex][f0:f0 + fb, :].rearrange("(c p) n -> p c n", p=128), nch * 1024)
                        w1v = w1b[:, 0:8 * fb].rearrange("p (k n) -> p k n", n=fb)
                        w3v = w3b[:, 0:8 * fb].rearrange("p (k n) -> p k n", n=fb)
                        w2v = w2b[:, 0:nch * 1024].rearrange("p (c n) -> p c n", n=1024)
                        hkeys = [("hT", i) for i in range(nt)]
                        subs = [(s0, min(512, T - s0)) for s0 in range(0, T, 512)]
                        for j in range(nch):
                            for (s0, sn) in subs:
                                pb = pbc[0] % 2; pbc[0] += 1
                                for k in range(8):
                                    P.op("pe", lambda e, j=j, k=k, pb=pb, w1v=w1v, s0=s0, sn=sn: e.matmul(h1p[pb][:, 0:sn], lhsT=w1v[:, k, j * 128:(j + 1) * 128], rhs=hT[:, k, s0:s0 + sn], start=(k == 0), stop=(k == 7)),
                                         reads=[k1] + hkeys, writes=[("h1p", pb)])
                                for k in range(8):
                                    P.op("pe", lambda e, j=j, k=k, pb=pb, w3v=w3v, s0=s0, sn=sn: e.matmul(h3p[pb][:, 0:sn], lhsT=w3v[:, k, j * 128:(j + 1) * 128], rhs=hT[:, k, s0:s0 + sn], start=(k == 0), stop=(k == 7)),
                                         reads=[k3] + hkeys, writes=[("h3p", pb)])
                                P.op("act", lambda e, pb=pb, sn=sn: e.activation(out=s1[pb % NS1][:, 0:sn], in_=h1p[pb][:, 0:sn], func=AF.Silu), reads=[("h1p", pb)], writes=[("s1", pb % NS1)])
                                P.op("dve", lambda e, pb=pb, j=j, s0=s0, sn=sn: e.tensor_tensor(out=aT[:, j, s0:s0 + sn], in0=s1[pb % NS1][:, 0:sn], in1=h3p[pb][:, 0:sn], op=ALU.mult), reads=[("s1", pb % NS1), ("h3p", pb)], writes=[("aT", j)])
                        akeys = [("aT", j) for j in range(nch)]
                        if DBG and g == 0 and ex == 0 and f0 == 0:
                            P.op("dve", lambda e: e.tensor_copy(out=tmp[:, 0:512], in_=aT[:, 0, :]), reads=[("aT", 0)], writes=["tmp"])
                            P.op("dve", lambda e: e.tensor_copy(out=tmp[:, 512:1024], in_=hT[:, 0, :]), reads=[("hT", 0), ("hT", 1), ("hT", 2), ("hT", 3)], writes=["tmp"])
                            P.dma(dbg3_d[:, :], tmp[:], reads=["tmp"], q="sync")
                        for i in range(nt):
                            for hf in range(2):
                                for j in range(nch):
                                    P.op("pe", lambda e, i=i, hf=hf, j=j, w2v=w2v, nch=nch: e.matmul(yp[hf][:], lhsT=aT[:, j, i * 128:(i + 1) * 128], rhs=w2v[:, j, hf * 512:(hf + 1) * 512], start=(j == 0), stop=(j == nch - 1)),
                                         reads=[k2] + akeys, writes=[("yp", hf)])
                                ya = Yacc[:, i, hf * 512:(hf + 1) * 512]
                                if post == "odd":
                                    if first:
                                        P.op("dve", lambda e, hf=hf, ya=ya, i=i, ex=ex: e.tensor_scalar_mul(out=ya, in0=yp[hf][:], scalar1=cwt[:, i, ex:ex + 1]), reads=[("yp", hf), ("cw", i)], writes=[("Y", i)])
                                    else:
                                        P.op("dve", lambda e, hf=hf, ya=ya, i=i, ex=ex: e.scalar_tensor_tensor(out=ya, in0=yp[hf][:], scalar=cwt[:, i, ex:ex + 1], in1=ya, op0=ALU.mult, op1=ALU.add),
                                             reads=[("yp", hf), ("cw", i), ("Y", i)], writes=[("Y", i)])
                                else:
                                    if first:
                                        P.op("dve", lambda e, hf=hf, ya=ya: e.tensor_copy(out=ya, in_=yp[hf][:]), reads=[("yp", hf)], writes=[("Y", i)])
                                    else:
                                        P.op("dve", lambda e, hf=hf, ya=ya: e.tensor_tensor(out=ya, in0=yp[hf][:], in1=ya, op=ALU.add), reads=[("yp", hf), ("Y", i)], writes=[("Y", i)])
                        first = False
                for i, t in enumerate(tiles):
                    tset = 1 if t == ntiles - 1 else 0
                    if 'noffn' in DBGMODE:
                        P.op("dve", lambda e, i=i: e.memset(Yacc[:, i, :], 0.0), writes=[("Y", i)])
                    if DBG:
                        P.dma(dbg2_d[t * 128:(t + 1) * 128, :], Yacc[:, i, :], reads=[("Y", i)], q="sync")
                    P.op("dve", lambda e, i=i, tset=tset: e.tensor_tensor(out=Yacc[:, i, :], in0=Yacc[:, i, :], in1=gateB[:, tset, 1, :], op=ALU.mult), reads=[("Y", i), "gateB"], writes=[("Y", i)])
                    P.op("dve", lambda e, i=i: e.tensor_tensor(out=X[:, i, :], in0=X[:, i, :], in1=Yacc[:, i, :], op=ALU.add), reads=[("X", i), ("Y", i)], writes=[("X", i)])
                    layer_norm(X[:, i, :], ("X", i), 1)
                    P.dma(xo_d[t * 128:(t + 1) * 128, :], X[:, i, :], reads=[("X", i)], q="sync")
            if pre:
                for i, t in enumerate(tiles):
                    tset = 1 if t == ntiles - 1 else 0
                    make_hT(i, tset, modTq, "modTq", 1, 0)
                wv = win_d.rearrange("(k p) n -> p k n", p=128)
                pc = 0
                for n0 in range(0, NO, 512):
                    wb_, wk = load_w(wv[:, :, n0:n0 + 512], 4096)
                    wvv = wb_[:, :].rearrange("p (k n) -> p k n", n=512)
                    for i, t in enumerate(tiles):
                        hf = pc % 2; ob = pc % NPO; pc += 1
                        for k in range(8):
                            P.op("pe", lambda e, i=i, k=k, hf=hf, wvv=wvv: e.matmul(yp[hf][:], lhsT=hT[:, k, i * 128:(i + 1) * 128], rhs=wvv[:, k, :], start=(k == 0), stop=(k == 7)),
                                 reads=[wk, ("hT", i)], writes=[("yp", hf)])
                        P.op("act", lambda e, hf=hf, ob=ob: e.activation(out=po[ob][:], in_=yp[hf][:], func=AF.Copy), reads=[("yp", hf)], writes=[("po", ob)])
                        P.dma(proj_d[t * 128:(t + 1) * 128, n0:n0 + 512], po[ob][:], reads=[("po", ob)], q="sync")
        P.finish()
        P.emit()
    return nc


S_RET = 4352
NU_RET = 2


def ret_consts():
    f32 = np.float32
    ki = np.arange(128)[:, None]; qi = np.arange(512)[None, :]
    Mqk = (qi - ki).astype(f32)
    Rp = np.zeros((4, 128, 512), f32); Rn = np.zeros((4, 128, 512), f32); Eq = np.zeros((4, 128, 512), f32)
    for v in range(4):
        d = qi - ki - 128 * v
        Rp[v] = np.maximum(d, 0); Rn[v] = np.maximum(-d, 0); Eq[v] = (d == 0)
    strc = np.ascontiguousarray(np.stack([Rp, Rn, Eq], 0).transpose(2, 0, 1, 3))
    t = np.arange(4096); rows = t // 64; cols = t % 64
    jj = np.arange(128) % 64
    inv = (10000.0 ** (-(jj.astype(np.float64)) / 64))
    cosT = np.ones((2, 128, S_RET), f32); sinT = np.zeros((2, 128, S_RET), f32)
    for c, pos in enumerate((rows, cols)):
        ang = (pos[None, :].astype(np.float32) * inv.astype(np.float32)[:, None]).astype(np.float32)
        cosT[c, :, 256:] = np.cos(ang)
        sgn = np.where(np.arange(128) < 64, -1.0, 1.0)[:, None]
        sinT[c, :, 256:] = np.sin(ang) * sgn
    n128 = (128.0 * np.arange(48)).astype(f32)
    return {"Mqk": Mqk, "strc": strc, "cosT": cosT, "sinT": sinT,
            "n128": np.ascontiguousarray(np.broadcast_to(n128[None], (128, 48)))}


def build_ret():
    nc = bass.Bass("TRN2", target_bir_lowering=False)
    din = lambda n, s: nc.dram_tensor(n, s, F32, kind="ExternalInput").ap()
    S = S_RET
    qT_d = din("qT", [NU_RET, 2, 128, S]); qsT_d = din("qsT", [NU_RET, 2, 128, S])
    kT_d = din("kT", [NU_RET, 2, 128, S]); ksT_d = din("ksT", [NU_RET, 2, 128, S])
    v_d = din("v", [NU_RET, S, 512]); ld_d = din("ld", [NU_RET, 2])
    Mqk_d = din("Mqk", [128, 512]); strc_d = din("strc", [128, 3, 4, 512])
    cosT_d = din("cosT", [2, 128, S]); sinT_d = din("sinT", [2, 128, S]); n128_d = din("n128", [128, 48])
    o_d = nc.dram_tensor("o", [NU_RET, S, 512], F32, kind="ExternalOutput").ap()
    NTK = S // 128
    with ExitStack() as st:
        sb = lambda n, s, d: st.enter_context(nc.sbuf_tensor(n, s, d))
        pst = lambda n, s: st.enter_context(nc.psum_tensor(n, s, F32))
        P = Prog(nc)
        Mqk = sb("Mqk_s", [128, 512], F32); strc = sb("strc_s", [128, 3, 4, 512], F32); n128 = sb("n128_s", [128, 48], F32)
        P.dma(Mqk[:], Mqk_d[:, :], writes=["Mqk"]); P.dma(strc[:], strc_d[:, :, :, :], writes=["strc"]); P.dma(n128[:], n128_d[:, :], writes=["n128"])
        qr = sb("qr", [128, 2, S], BF16); kr = sb("kr", [128, 2, S], BF16); vb = sb("vb", [128, NTK, 512], BF16)
        CH = 1088
        ra = sb("ra", [128, CH], F32); rb_ = sb("rb", [128, CH], F32); rc = sb("rc", [128, CH], F32); rd = sb("rd", [128, CH], F32)
        vst = [sb("vst%d" % i, [128, 512], F32) for i in range(2)]
        ld = sb("ld_s", [128, 2], F32); nld = sb("nld", [128, 2], F32)
        bF = sb("bF", [128, 48], F32); bB = sb("bB", [128, 48], F32)
        Dstr = sb("Dstr", [128, 4, 512], F32); e1 = sb("e1", [128, 512], F32); e2 = sb("e2", [128, 512], F32)
        mk = [sb("mk%d" % i, [128, 512], F32) for i in range(2)]; mk2 = [sb("mk2_%d" % i, [128, 512], F32) for i in range(2)]
        At = [sb("At%d" % i, [128, 512], BF16) for i in range(2)]
        ot = [sb("ot%d" % i, [128, 512], F32) for i in range(2)]
        sp = [pst("sp%d" % i, [128, 512]) for i in range(2)]
        op_ = [pst("op%d" % i, [128, 512]) for i in range(4)]
        octr = [0]
        for u in range(NU_RET):
            P.dma(ld[:], ld_d[u:u + 1, :].to_broadcast([128, 2]), writes=["ld"])
            P.op("dve", lambda e: e.tensor_scalar(out=nld[:], in0=ld[:], scalar1=-1.0, scalar2=None, op0=ALU.mult), reads=["ld"], writes=["nld"])
            P.op("dve", lambda e: e.tensor_scalar(out=bF[:], in0=n128[:], scalar1=ld[:, 0:1], scalar2=None, op0=ALU.mult), reads=["ld", "n128"], writes=["bF"])
            P.op("dve", lambda e: e.tensor_scalar(out=bB[:], in0=n128[:], scalar1=ld[:, 1:2], scalar2=None, op0=ALU.mult), reads=["ld", "n128"], writes=["bB"])
            for v in range(4):
                P.op("act", lambda e, v=v: e.activation(out=e1[:], in_=strc[:, 0, v, :], func=AF.Exp, scale=ld[:, 0:1]), reads=["strc", "ld"], writes=["e1"])
                P.op("act", lambda e, v=v: e.activation(out=e2[:], in_=strc[:, 1, v, :], func=AF.Exp, scale=ld[:, 1:2]), reads=["strc", "ld"], writes=["e2"])
                P.op("dve", lambda e, v=v: e.tensor_tensor(out=Dstr[:, v, :], in0=e1[:], in1=e2[:], op=ALU.mult), reads=["e1", "e2"], writes=["Dstr"])
                P.op("dve", lambda e, v=v: e.tensor_tensor(out=Dstr[:, v, :], in0=Dstr[:, v, :], in1=strc[:, 2, v, :], op=ALU.add), reads=["Dstr", "strc"], writes=["Dstr"])
            for (src, ssrc, dst, dkey) in ((qT_d, qsT_d, qr, "qr"), (kT_d, ksT_d, kr, "kr")):
                for c in range(2):
                    for t0 in range(0, S, CH):
                        P.dma(ra[:], src[u, c, :, t0:t0 + CH], writes=["ra"], q="sync")
                        P.dma(rb_[:], ssrc[u, c, :, t0:t0 + CH], writes=["rb"], q="pool")
                        P.dma(rc[:], cosT_d[c, :, t0:t0 + CH], writes=["rc"], q="sync")
                        P.dma(rd[:], sinT_d[c, :, t0:t0 + CH], writes=["rd"], q="pool")
                        P.op("dve", lambda e: e.tensor_tensor(out=ra[:], in0=ra[:], in1=rc[:], op=ALU.mult), reads=["ra", "rc"], writes=["ra"])
                        P.op("pool", lambda e: e.tensor_tensor(out=rb_[:], in0=rb_[:], in1=rd[:], op=ALU.mult), reads=["rb", "rd"], writes=["rb"])
                        P.op("dve", lambda e, dst=dst, c=c, t0=t0: e.tensor_tensor(out=dst[:, c, t0:t0 + CH], in0=ra[:], in1=rb_[:], op=ALU.add), reads=["ra", "rb"], writes=[dkey])
            for t in range(NTK):
                s_ = t % 2
                P.dma(vst[s_][:], v_d[u, t * 128:(t + 1) * 128, :], writes=[("vst", s_)], q="sync" if s_ == 0 else "pool")
                P.op("act", lambda e, t=t, s_=s_: e.activation(out=vb[:, t, :], in_=vst[s_][:], func=AF.Copy), reads=[("vst", s_)], writes=["vb"])
            blocks = [(0, 256, True)] + [(256 + 512 * i, 512, False) for i in range(8)]
            bc = 0
            blist = []
            for (q0, nq, isctx) in blocks:
                nsub = nq // 128
                ktiles = [0, 1] if isctx else list(range(NTK))
                for ki_, kt in enumerate(ktiles):
                    blist.append((q0, nq, isctx, nsub, ki_, kt, len(ktiles), bc % 2)); bc += 1

            def emitS(blk):
                q0, nq, isctx, nsub, ki_, kt, nk, pb = blk
                for c in range(2):
                    P.op("pe", lambda e, c=c: e.matmul(sp[pb][:, 0:nq], lhsT=kr[:, c, kt * 128:(kt + 1) * 128], rhs=qr[:, c, q0:q0 + nq], start=(c == 0), stop=(c == 1)),
                         reads=["qr", "kr"], writes=[("sp", pb)])

            def emitMid(blk):
                q0, nq, isctx, nsub, ki_, kt, nk, pb = blk
                if isctx:
                    msk = Dstr[:, kt, 0:nq]; mkeys = ["Dstr"]
                else:
                    if kt < 2:
                        qx0 = q0 - 256
                        nf = (qx0 - (kt * 128 - 256)) // 128
                        nb = ((4096 + kt * 128) - qx0) // 128
                        P.op("act", lambda e: e.activation(out=mk[pb][:], in_=Mqk[:], func=AF.Exp, scale=ld[:, 0:1], bias=bF[:, nf:nf + 1]), reads=["Mqk", "ld", "bF"], writes=[("mk", pb)])
                        P.op("act", lambda e: e.activation(out=mk2[pb][:], in_=Mqk[:], func=AF.Exp, scale=nld[:, 1:2], bias=bB[:, nb:nb + 1]), reads=["Mqk", "nld", "bB"], writes=[("mk2", pb)])
                        P.op("pool", lambda e: e.tensor_tensor(out=mk[pb][:], in0=mk[pb][:], in1=mk2[pb][:], op=ALU.add), reads=[("mk", pb), ("mk2", pb)], writes=[("mk", pb)])
                        msk = mk[pb][:, :]; mkeys = [("mk", pb)]
                    else:
                        k0 = kt * 128
                        if q0 >= k0 + 128:
                            n = (q0 - k0) // 128
                            P.op("act", lambda e: e.activation(out=mk[pb][:], in_=Mqk[:], func=AF.Exp, scale=ld[:, 0:1], bias=bF[:, n:n + 1]), reads=["Mqk", "ld", "bF"], writes=[("mk", pb)])
                            msk = mk[pb][:, :]; mkeys = [("mk", pb)]
                        elif q0 + 512 <= k0:
                            n = (k0 - q0) // 128
                            P.op("act", lambda e: e.activation(out=mk[pb][:], in_=Mqk[:], func=AF.Exp, scale=nld[:, 1:2], bias=bB[:, n:n + 1]), reads=["Mqk", "nld", "bB"], writes=[("mk", pb)])
                            msk = mk[pb][:, :]; mkeys = [("mk", pb)]
                        else:
                            v = (k0 - q0) // 128
                            msk = Dstr[:, v, :]; mkeys = ["Dstr"]
                P.op("dve", lambda e: e.scalar_tensor_tensor(out=At[pb][:, 0:nq], in0=sp[pb][:, 0:nq], scalar=1.0 / 16, in1=msk, op0=ALU.mult, op1=ALU.mult),
                     reads=[("sp", pb)] + mkeys, writes=[("At", pb)])

            def emitPV(blk):
                q0, nq, isctx, nsub, ki_, kt, nk, pb = blk
                for s in range(nsub):
                    P.op("pe", lambda e, s=s: e.matmul(op_[s][:], lhsT=At[pb][:, s * 128:(s + 1) * 128], rhs=vb[:, kt, :], start=(ki_ == 0), stop=(ki_ == nk - 1)),
                         reads=[("At", pb), "vb"], writes=[("op", s)])
                if ki_ == nk - 1:
                    for s in range(nsub):
                        ob = octr[0] % 2; octr[0] += 1
                        P.op("act", lambda e, s=s, ob=ob: e.activation(out=ot[ob][:], in_=op_[s][:], func=AF.Copy), reads=[("op", s)], writes=[("ot", ob)])
                        P.dma(o_d[u, q0 + s * 128:q0 + (s + 1) * 128, :], ot[ob][:], reads=[("ot", ob)], q="sync")

            emitS(blist[0])
            for bi, blk in enumerate(blist):
                emitMid(blk)
                if bi + 1 < len(blist):
                    emitS(blist[bi + 1])
                emitPV(blk)
        P.finish(); P.emit()
    return nc


def ret_ref(q, k, v, ld):
    S = q.shape[0]
    A = (q.astype(np.float64) @ k.astype(np.float64).T) / 16
    pos_f = np.concatenate([np.arange(256) - 256, np.arange(4096)]).astype(np.float64)
    pos_b = np.concatenate([np.arange(256) + 4096, np.arange(4096)]).astype(np.float64)
    df = pos_f[:, None] - pos_f[None, :]
    db = pos_b[None, :] - pos_b[:, None]
    D = np.where(df >= 0, np.exp(ld[0] * np.maximum(df, 0)), 0) + np.where(db >= 0, np.exp(ld[1] * np.maximum(db, 0)), 0)
    D[:256, 256:] = 0
    return (A * D) @ v.astype(np.float64)


NU_NA = 4
S_NA = 4352
NEG = -30000.0


def na_variants():
    var_of = {}; reps = []
    for I in range(8):
        jlo, jhi = max(0, 4 * I - 2), min(31, 4 * I + 5)
        for j in range(jlo, jhi + 1):
            if I == 0:
                key = ("a", j)
            elif I == 7:
                key = ("z", j)
            else:
                key = ("m", j - 4 * I)
            if key not in var_of:
                var_of[key] = len(reps); reps.append((I, j))
    return var_of, reps


def na_tables(rpb_h):
    var_of, reps = na_variants()
    kp = np.arange(128); q = np.arange(512)
    a = kp // 64; kc = kp % 64; m = q // 64; qc = q % 64
    Bg = np.zeros((128, len(reps), 512), np.float32); M = np.zeros((128, len(reps), 512), np.float32)
    c_start = np.clip(qc - 8, 0, 48)
    for vi, (I, j) in enumerate(reps):
        kr = (2 * j + a)[:, None]; qr = (8 * I + m)[None, :]
        s = np.clip(qr - 4, 0, 56)
        rowok = (kr >= s) & (kr < s + 8)
        colok = (kc[:, None] >= c_start[None, :]) & (kc[:, None] < c_start[None, :] + 16)
        ok = rowok & colok
        dr = np.clip(kr - qr + 7, 0, 14); dc = np.clip(kc[:, None] - qc[None, :], -15, 15) + 15
        Bg[:, vi, :] = rpb_h[dr, dc]
        M[:, vi, :] = np.where(ok, 0.0, NEG)
    return Bg, M


def build_na():
    nc = bass.Bass("TRN2", target_bir_lowering=False)
    din = lambda n, s: nc.dram_tensor(n, s, F32, kind="ExternalInput").ap()
    S = S_NA
    qT_d = din("qT", [NU_NA, 64, S]); kT_d = din("kT", [NU_NA, 64, S]); v_d = din("v", [NU_NA, S, 64])
    Bg_d = din("Bg", [128, 20, 512]); M_d = din("M", [128, 20, 512])
    o_d = nc.dram_tensor("o", [NU_NA, S, 64], F32, kind="ExternalOutput").ap()
    var_of, reps = na_variants()
    with ExitStack() as st:
        sb = lambda n, s, d: st.enter_context(nc.sbuf_tensor(n, s, d))
        pst = lambda n, s: st.enter_context(nc.psum_tensor(n, s, F32))
        P = Prog(nc)
        Bt = sb("Bt", [128, 20, 512], F32)
        stg = [sb("stg%d" % i, [128, 2560], F32) for i in range(2)]
        for c in range(4):
            P.dma(Bt[:, c * 5:(c + 1) * 5, :], Bg_d[:, c * 5:(c + 1) * 5, :], writes=[("Bt", c)], q="sync")
            P.dma(stg[c % 2][:, :].rearrange("p (a b) -> p a b", b=512), M_d[:, c * 5:(c + 1) * 5, :], writes=[("stg", c % 2)], q="pool")
            P.op("dve", lambda e, c=c: e.tensor_tensor(out=Bt[:, c * 5:(c + 1) * 5, :], in0=Bt[:, c * 5:(c + 1) * 5, :], in1=stg[c % 2][:, :].rearrange("p (a b) -> p a b", b=512), op=ALU.add),
                 reads=[("Bt", c), ("stg", c % 2)], writes=[("Bt", c)])
        Bkeys = [("Bt", c) for c in range(4)]
        qb = sb("qb", [64, S], BF16); kb = sb("kb", [64, S], BF16); va = sb("va", [128, 34, 65], BF16)
        vst = sb("vstg", [128, 34, 64], F32)
        sbt = [sb("sbt%d" % i, [128, 512], F32) for i in range(2)]
        Et = [sb("Et%d" % i, [128, 512], BF16) for i in range(2)]
        rden = sb("rden", [128, 1], F32); ot = [sb("ot%d" % i, [128, 64], F32) for i in range(2)]
        sp = [pst("sp%d" % i, [128, 512]) for i in range(2)]
        op_ = [pst("op%d" % i, [128, 512]) for i in range(4)]
        P.op("pool", lambda e: e.memset(va[:, :, 64:65], 1.0), writes=["va"])
        octr = [0]; bc = 0
        for u in range(NU_NA):
            for (src, dst, dk) in ((qT_d, qb, "qb"), (kT_d, kb, "kb")):
                for h2 in range(2):
                    s_ = h2
                    P.dma(stg[s_][0:64, 0:2176], src[u, :, h2 * 2176:(h2 + 1) * 2176], writes=[("stg", s_)], q="sync" if h2 == 0 else "pool")
                    P.op("act", lambda e, dst=dst, h2=h2, s_=s_: e.activation(out=dst[:, h2 * 2176:(h2 + 1) * 2176], in_=stg[s_][0:64, 0:2176], func=AF.Copy), reads=[("stg", s_)], writes=[dk])
            P.dma(vst[:], v_d[u].rearrange("(t p) d -> p t d", p=128), writes=["vst"], q="sync")
            P.op("dve", lambda e: e.tensor_copy(out=va[:, :, 0:64], in_=vst[:]), reads=["vst"], writes=["va"])
            blocks = [("C", 0, 256)] + [(I, 256 + 512 * I, 512) for I in range(8)]
            blist = []
            for (I, q0, nq) in blocks:
                nsub = nq // 128
                if I == "C":
                    kts = [(0, None), (1, None)]
                else:
                    kts = [(0, None), (1, None)]
                    for j in range(max(0, 4 * I - 2), min(31, 4 * I + 5) + 1):
                        key = ("a", j) if I == 0 else (("z", j) if I == 7 else ("m", j - 4 * I))
                        kts.append((2 + j, var_of[key]))
                for ki_, (kt, vi) in enumerate(kts):
                    blist.append((q0, nq, nsub, ki_, kt, vi, len(kts), bc % 2)); bc += 1

            def emitS(blk):
                q0, nq, nsub, ki_, kt, vi, nk, pb = blk
                P.op("pe", lambda e: e.matmul(sp[pb][:, 0:nq], lhsT=kb[:, kt * 128:(kt + 1) * 128], rhs=qb[:, q0:q0 + nq], start=True, stop=True),
                     reads=["qb", "kb"], writes=[("sp", pb)])

            def emitMid(blk):
                q0, nq, nsub, ki_, kt, vi, nk, pb = blk
                if vi is None:
                    P.op("act", lambda e: e.activation(out=Et[pb][:, 0:nq], in_=sp[pb][:, 0:nq], func=AF.Exp, scale=0.125), reads=[("sp", pb)], writes=[("Et", pb)])
                else:
                    P.op("dve", lambda e: e.scalar_tensor_tensor(out=sbt[pb][:], in0=sp[pb][:], scalar=0.125, in1=Bt[:, vi, :], op0=ALU.mult, op1=ALU.add),
                         reads=[("sp", pb)] + Bkeys, writes=[("sbt", pb)])
                    P.op("act", lambda e: e.activation(out=Et[pb][:], in_=sbt[pb][:], func=AF.Exp), reads=[("sbt", pb)], writes=[("Et", pb)])

            def emitPV(blk):
                q0, nq, nsub, ki_, kt, vi, nk, pb = blk
                for s in range(nsub):
                    P.op("pe", lambda e, s=s: e.matmul(op_[s][:, 0:65], lhsT=Et[pb][:, s * 128:(s + 1) * 128], rhs=va[:, kt, :], start=(ki_ == 0), stop=(ki_ == nk - 1)),
                         reads=[("Et", pb), "va"], writes=[("op", s)])
                if ki_ == nk - 1:
                    for s in range(nsub):
                        ob = octr[0] % 2; octr[0] += 1
                        P.op("dve", lambda e, s=s: e.reciprocal(out=rden[:], in_=op_[s][:, 64:65]), reads=[("op", s)], writes=["rden"])
                        P.op("dve", lambda e, s=s, ob=ob: e.tensor_scalar(out=ot[ob][:], in0=op_[s][:, 0:64], scalar1=rden[:, 0:1], scalar2=None, op0=ALU.mult), reads=[("op", s), "rden"], writes=[("ot", ob)])
                        P.dma(o_d[u, q0 + s * 128:q0 + (s + 1) * 128, :], ot[ob][:], reads=[("ot", ob)], q="sync")

            emitS(blist[0])
            for bi, blk in enumerate(blist):
                emitMid(blk)
                if bi + 1 < len(blist):
                    emitS(blist[bi + 1])
                emitPV(blk)
        P.finish(); P.emit()
    return nc


def na_ref(q, k, v, qc, kc, vc, rpb_h):
    q = q.astype(np.float64) * 0.125; k = k.astype(np.float64); v = v.astype(np.float64)
    kc = kc.astype(np.float64); vc = vc.astype(np.float64)
    out = np.zeros((4096, 64))
    col = np.arange(64)
    for r in range(64):
        s = min(max(r - 4, 0), 56)
        keys = k[s * 64:(s + 8) * 64]; vals = v[s * 64:(s + 8) * 64]
        qq = q[r * 64:(r + 1) * 64]
        sc = qq @ keys.T
        kr = s + np.arange(8).repeat(64); kcol = np.tile(col, 8)
        cst = np.clip(col - 8, 0, 48)
        ok = (kcol[None, :] >= cst[:, None]) & (kcol[None, :] < cst[:, None] + 16)
        dr = kr - r + 7; dc = np.clip(kcol[None, :] - col[:, None], -15, 15) + 15
        sc = np.where(ok, sc + rpb_h[dr[None, :].repeat(64, 0), dc], -np.inf)
        scc = qq @ kc.T
        al = np.concatenate([sc, scc], 1); al = al - al.max(1, keepdims=True); p = np.exp(al); p /= p.sum(1, keepdims=True)
        out[r * 64:(r + 1) * 64] = p[:, :512] @ vals + p[:, 512:] @ vc
    sc = (qc.astype(np.float64) * 0.125) @ kc.T; sc -= sc.max(1, keepdims=True); p = np.exp(sc); p /= p.sum(1, keepdims=True)
    return out, p @ vc


import os
HGV = 2
HGQ = int(os.environ.get('HGQ', '0'))
NU_HG = 4
S_HG = 4352


def hg_consts():
    import ml_dtypes
    sel = np.zeros((128, 128, 128), np.float32)
    selT = np.zeros((128, 128, 128), np.float32)
    for vi in range(128):
        sel[vi, vi, :] = 1.0
        selT[:, vi, vi] = 1.0
    return {"sel": sel.reshape(128, 128 * 128), "selT": selT.reshape(128, 128 * 128),
            "io": np.ascontiguousarray(np.stack([np.eye(128, dtype=np.float32), np.ones((128, 128), np.float32)], 1))}


def build_hg(use_lb):
    nc = bass.Bass("TRN2", target_bir_lowering=False)
    din = lambda n, s: nc.dram_tensor(n, s, F32, kind="ExternalInput").ap()
    S = S_HG
    zq_d = din("zqT", [NU_HG, 128, S]); zf_d = din("zfT", [NU_HG, 128, S]); vT_d = din("vT", [NU_HG, 128, S]); lbl_d = din("lbl", [NU_HG, 128, 2])
    sel_d = din("sel", [128, 128 * 128]); selT_d = din("selT", [128, 128 * 128]); io_d = din("io", [128, 2, 128])
    o_d = nc.dram_tensor("oT", [NU_HG, 128, S], F32, kind="ExternalOutput").ap()
    with ExitStack() as st:
        sb = lambda n, s, d: st.enter_context(nc.sbuf_tensor(n, s, d))
        pst = lambda n, s: st.enter_context(nc.psum_tensor(n, s, F32))
        P = Prog(nc)
        sel = sb("sel_s", [128, 128, 128], BF16); selT = sb("selT_s", [128, 128, 128], BF16)
        stg = sb("stg", [128, 4352], F32)
        for (src, dst, dk) in ((sel_d, sel, "sel"), (selT_d, selT, "selT")):
            for c in range(4):
                P.dma(stg[:, 0:4096], src[:, c * 4096:(c + 1) * 4096], writes=["stg"], q="sync")
                P.op("dve", lambda e, dst=dst, c=c: e.tensor_copy(out=dst[:, c * 32:(c + 1) * 32, :], in_=stg[:, 0:4096].rearrange("p (a b) -> p a b", b=128)), reads=["stg"], writes=[dk])
        ft = sb("ft", [128, S], F32); qt = sb("qt", [128, S], F32); vTb = sb("vTb", [128, S], BF16); vTl = sb("vTl", [128, S], BF16); vt32 = sb("vt32", [128, S + 1], F32)
        io = sb("io_s", [128, 2, 128], F32); Dg = sb("Dg", [128, 128], F32)
        P.dma(io[:], io_d[:, :, :], writes=["io"])
        lbl = sb("lbl_s", [128, 2], F32); lb = sb("lb", [128, 1], F32); oml = sb("oml", [128, 1], F32)
        state = sb("state", [128, 128], F32)
        NB = 6; NBP = 4
        Sv = [sb("Sv_%d" % i, [128, 512], F32) for i in range(NB)]
        qs = [sb("qs_%d" % i, [128, 512], BF16) for i in range(NB)]
        ot = [sb("ot_%d" % i, [128, 512], F32) for i in range(2)]
        tmpc = sb("tmpc", [128, 512], F32)
        vbp = [pst("vbp%d" % i, [128, 512]) for i in range(NBP)]
        opp = [pst("opp%d" % i, [128, 512]) for i in range(2)]
        qsp = pst("qsp", [128, 512])
        cc = 0
        for u in range(NU_HG):
            if use_lb:
                P.dma(lbl[:], lbl_d[u, :, :], writes=["lbl"])
                P.op("dve", lambda e: e.tensor_tensor(out=lb[:], in0=lbl[:, 1:2], in1=lbl[:, 0:1], op=ALU.subtract), reads=["lbl"], writes=["lb"])
                P.op("act", lambda e: e.activation(out=lb[:], in_=lb[:], func=AF.Sigmoid), reads=["lb"], writes=["lb"])
                P.op("dve", lambda e: e.tensor_scalar(out=oml[:], in0=lb[:], scalar1=-1.0, scalar2=1.0, op0=ALU.mult, op1=ALU.add), reads=["lb"], writes=["oml"])
            P.dma(stg[:], zf_d[u, :, :], writes=["stg"], q="sync")
            P.op("act", lambda e: e.activation(out=ft[:], in_=stg[:], func=AF.Sigmoid), reads=["stg"], writes=["ft"])
            if use_lb:
                P.op("dve", lambda e: e.tensor_scalar(out=ft[:], in0=ft[:], scalar1=oml[:, 0:1], scalar2=lb[:, 0:1], op0=ALU.mult, op1=ALU.add), reads=["ft", "oml", "lb"], writes=["ft"])
            P.dma(stg[:], zq_d[u, :, :], writes=["stg"], q="sync")
            P.op("act", lambda e: e.activation(out=qt[:], in_=stg[:], func=AF.Silu), reads=["stg"], writes=["qt"])
            P.dma(vt32[:, 0:S], vT_d[u, :, :], writes=["vt32"], q="sync")
            P.op("dve", lambda e: e.memset(vt32[:, S:S + 1], 0.0), reads=["vt32"], writes=["vt32"])
            P.op("dve", lambda e: e.tensor_tensor(out=stg[:, 0:S], in0=vt32[:, 0:S], in1=vt32[:, 1:S + 1], op=ALU.subtract), reads=["vt32", "stg"], writes=["stg"])
            P.op("dve", lambda e: e.tensor_copy(out=vTb[:, 0:S], in_=stg[:, 0:S]), reads=["stg"], writes=["vTb"])
            P.op("dve", lambda e: e.tensor_tensor(out=vTl[:, 0:S], in0=stg[:, 0:S], in1=vTb[:, 0:S], op=ALU.subtract), reads=["stg", "vTb"], writes=["vTl"])
            P.op("dve", lambda e: e.tensor_scalar(out=Dg[:], in0=io[:, 0, :], scalar1=vt32[:, 0:1], scalar2=-1.0, op0=ALU.mult, op1=ALU.mult), reads=["io", "vt32"], writes=["Dg"])
            P.op("pe", lambda e: e.matmul(qsp[:, 0:128], lhsT=io[:, 1, :], rhs=Dg[:], start=True, stop=True), reads=["io", "Dg"], writes=["qsp"])
            P.op("act", lambda e: e.activation(out=state[:], in_=qsp[:, 0:128], func=AF.Copy), reads=["qsp"], writes=[("state", vi) for vi in range(128)])
            items = []
            for t0 in range(0, S, 512):
                n = min(512, S - t0)
                ob = cc % 2; cc += 1
                for vi in range(128):
                    items.append((t0, n, ob, vi))
            G = len(items)

            def stage(k, g):
                t0, n, ob, vi = items[g]
                b = g % NB; pb = g % NBP
                if k == 0:
                    P.op("pe", lambda e: e.matmul(vbp[pb][:, 0:n], lhsT=sel[:, vi, :], rhs=vTb[:, t0:t0 + n], start=True, stop=False), reads=["sel", "vTb"], writes=[("vbp", pb)])
                    P.op("pe", lambda e: e.matmul(vbp[pb][:, 0:n], lhsT=sel[:, vi, :], rhs=vTl[:, t0:t0 + n], start=False, stop=True), reads=["sel", "vTl"], writes=[("vbp", pb)])
                elif k == 1:
                    P.op("dve", lambda e: e.tensor_tensor_scan(out=Sv[b][:, 0:n], data0=ft[:, t0:t0 + n], data1=vbp[pb][:, 0:n], initial=state[:, vi:vi + 1], op0=ALU.mult, op1=ALU.add),
                         reads=["ft", ("vbp", pb), ("state", vi)], writes=[("Sv", b)])
                elif k == 2:
                    P.op("act", lambda e: e.activation(out=state[:, vi:vi + 1], in_=Sv[b][:, n - 1:n], func=AF.Copy), reads=[("Sv", b)], writes=[("state", vi)])
                    qeng = "dve" if (HGQ and vi % HGQ == 0) else "pool"
                    P.op(qeng, lambda e: e.tensor_tensor(out=qs[b][:, 0:n], in0=qt[:, t0:t0 + n], in1=Sv[b][:, 0:n], op=ALU.mult), reads=["qt", ("Sv", b)], writes=[("qs", b)])
                elif k == 3:
                    P.op("pe", lambda e: e.matmul(opp[ob][:, 0:n], lhsT=selT[:, vi, :], rhs=qs[b][:, 0:n], start=(vi == 0), stop=(vi == 127)), reads=["selT", ("qs", b)], writes=[("opp", ob)])
                    if vi == 127:
                        P.op("pe", lambda e: e.matmul(qsp[:, 0:n], lhsT=io[:, 1, :], rhs=qt[:, t0:t0 + n], start=True, stop=True), reads=["io", "qt"], writes=["qsp"])
                        P.op("dve", lambda e: e.tensor_tensor(out=tmpc[:, 0:n], in0=vt32[:, t0 + 1:t0 + n + 1], in1=qsp[:, 0:n], op=ALU.mult), reads=["vt32", "qsp"], writes=["tmpc"])
                        P.op("dve", lambda e: e.tensor_tensor(out=ot[ob][:, 0:n], in0=opp[ob][:, 0:n], in1=tmpc[:, 0:n], op=ALU.add), reads=[("opp", ob), "tmpc"], writes=[("ot", ob)])
                        P.dma(o_d[u, :, t0:t0 + n], ot[ob][:, 0:n], reads=[("ot", ob)], q="sync")

            for step in range(G + 3):
                for k in range(4):
                    g = step - k
                    if 0 <= g < G:
                        stage(k, g)
        P.finish(); P.emit()
    return nc


def hg_ref(zq, zf, v, lb):
    zq = zq.astype(np.float64); zf = zf.astype(np.float64); v = v.astype(np.float64)
    q = zq / (1 + np.exp(-zq)); f = lb + (1 - lb) / (1 + np.exp(-zf)); k = 1 - f
    St = np.zeros((128, 128)); o = np.zeros((zq.shape[0], 128))
    for t in range(zq.shape[0]):
        St = f[t][:, None] * St + k[t][:, None] * v[t][None, :]
        o[t] = q[t] @ St
    return o


_CACHE = {}


def _prog(key, fn):
    if key not in _CACHE:
        _CACHE[key] = fn()
    return _CACHE[key]


def _bc(a, shape):
    return np.ascontiguousarray(np.broadcast_to(a, shape)).astype(np.float32)


def _rows_core(xf, cf, c):
    b, h = c // 2, c % 2
    return np.ascontiguousarray(np.concatenate([xf[b, h * 2048:(h + 1) * 2048], cf[b, h * 128:(h + 1) * 128]], 0))


def _unrows(outs, W):
    xf = np.zeros((4, 4096, W), np.float32); cf = np.zeros((4, 256, W), np.float32)
    for c in range(8):
        b, h = c // 2, c % 2
        xf[b, h * 2048:(h + 1) * 2048] = outs[c][:2048]
        cf[b, h * 128:(h + 1) * 128] = outs[c][2048:]
    return xf, cf


def _modT(m, l, b):
    mods = np.stack([m[l, b].reshape(6, 1024), m[l, 4].reshape(6, 1024)], 0)
    modT = np.ascontiguousarray(mods.reshape(2, 6, 8, 128).transpose(3, 0, 1, 2))
    gateB = _bc(mods[:, [2, 5], :][None], (128, 2, 2, 1024))
    return modT, gateB


def _run(nc, maps):
    res = run_bass_kernel_spmd(nc, maps, core_ids=list(range(8)))
    return res.results


def kernel(x, c, ctx, c_ctx, ada_w, ada_b, ln_g, ln_b, e_w_in, e_w_out, na_rpb, hg_lb_logits, hg_norm_g,
           ffn_w1, ffn_w3, ffn_w2, o_w_in, o_w_out, ret_log_decay, router_w, router_b, moe_w1, moe_w3, moe_w2):
    f32 = np.float32
    A = lambda a: np.ascontiguousarray(np.asarray(a, dtype=f32))
    x = A(x); ctx = A(ctx)
    ident = np.eye(128, dtype=f32)
    nc_ada = _prog("ada", build_ada)
    cc = np.concatenate([A(c), A(c_ctx)[None]], 0)
    cT = np.ascontiguousarray(cc.T.reshape(8, 128, 5).transpose(1, 0, 2))
    maps = []
    for core in range(8):
        l, h = core // 2, core % 2
        maps.append({"cT": cT, "w": A(ada_w[l][:, h * 3072:(h + 1) * 3072]), "b": A(ada_b[l][None, h * 3072:(h + 1) * 3072])})
    r = _run(nc_ada, maps)
    m = np.zeros((4, 5, 6144), f32)
    for core in range(8):
        l, h = core // 2, core % 2
        m[l][:, h * 3072:(h + 1) * 3072] = r[core]["m"]

    def dense(post, pre, l_post, l_pre, xf, cf, extra):
        nc = _prog(("dense", post, pre), lambda: build_dense(post, pre))
        maps = []
        for core in range(8):
            b = core // 2
            d = {"x": _rows_core(xf, cf, core), "ident": ident}
            if post:
                modT, gateB = _modT(m, l_post, b)
                d["modTp"] = modT; d["gateB"] = gateB
                d["lnGB"] = _bc(np.stack([A(ln_g[l_post]), A(ln_b[l_post])], 1)[None], (128, 2, 2, 1024))
                for k_, v_ in extra.items():
                    if isinstance(v_, tuple):
                        d[k_] = _rows_core(v_[0], v_[1], core)
                    else:
                        d[k_] = v_
            if pre:
                modT, _ = _modT(m, l_pre, b)
                d["modTq"] = modT
                d["win"] = A(e_w_in[l_pre // 2]) if pre == "even" else A(o_w_in[l_pre // 2])
            maps.append(d)
        r = _run(nc, maps)
        xo = co = px = pc = None
        if post:
            xo, co = _unrows([r[c_]["xo"] for c_ in range(8)], 1024)
        if pre:
            NO = 4096 if pre == "even" else 6144
            px, pc = _unrows([r[c_]["proj"] for c_ in range(8)], NO)
        return xo, co, px, pc

    _, _, px, pc = dense(None, "even", None, 0, x, ctx, {})
    for l in range(4):
        j = l // 2
        nxt = None if l == 3 else ("odd" if l % 2 == 0 else "even")
        if l % 2 == 0:
            nc_na = _prog("na", build_na)
            maps = []
            for h in range(8):
                Bg, M = na_tables(A(na_rpb[j][h]))
                sl = lambda a, o: a[:, :, o + h * 64:o + (h + 1) * 64]
                qcat = np.concatenate([sl(pc, 0), sl(px, 0)], 1)
                kcat = np.concatenate([sl(pc, 512), sl(px, 512)], 1)
                vcat = np.concatenate([sl(pc, 1024), sl(px, 1024)], 1)
                maps.append({"qT": np.ascontiguousarray(qcat.transpose(0, 2, 1)), "kT": np.ascontiguousarray(kcat.transpose(0, 2, 1)),
                             "v": np.ascontiguousarray(vcat), "Bg": Bg, "M": M})
            r = _run(nc_na, maps)
            a_x = np.zeros((4, 4096, 512), f32); a_c = np.zeros((4, 256, 512), f32)
            for h in range(8):
                o = r[h]["o"]
                a_x[:, :, h * 64:(h + 1) * 64] = o[:, 256:]; a_c[:, :, h * 64:(h + 1) * 64] = o[:, :256]
            nc_hg = _prog(("hg", j), lambda: build_hg(j == 1))
            hc = hg_consts()
            maps = []
            for core in range(8):
                zq = np.zeros((4, 128, 4352), f32); zf = np.zeros((4, 128, 4352), f32); vT = np.zeros((4, 128, 4352), f32); lbl = np.zeros((4, 128, 2), f32)
                for i in range(4):
                    n = core * 4 + i
                    b, h, dr = n // 8, (n // 2) % 4, n % 2
                    def seq(o):
                        cpart = pc[b][:, o + h * 128:o + (h + 1) * 128]; xpart = px[b][:, o + h * 128:o + (h + 1) * 128]
                        if dr == 1:
                            cpart = cpart[::-1]; xpart = xpart[::-1]
                        return np.concatenate([cpart, xpart], 0).T
                    zq[i] = seq(1536); zf[i] = seq(2048 + 512 * dr); vT[i] = seq(3072)
                    lbl[i] = A(hg_lb_logits[dr][:, h * 128:(h + 1) * 128]).T
                d = dict(hc); d.update(zqT=zq, zfT=zf, vT=vT, lbl=lbl)
                maps.append(d)
            r = _run(nc_hg, maps)
            of_x = np.zeros((4, 4096, 512), f32); ob_x = np.zeros((4, 4096, 512), f32)
            of_c = np.zeros((4, 256, 512), f32); ob_c = np.zeros((4, 256, 512), f32)
            for core in range(8):
                for i in range(4):
                    n = core * 4 + i
                    b, h, dr = n // 8, (n // 2) % 4, n % 2
                    o = r[core]["oT"][i].T
                    oc_, ox_ = o[:256], o[256:]
                    if dr == 1:
                        ob_c[b][:, h * 128:(h + 1) * 128] = oc_[::-1]; ob_x[b][:, h * 128:(h + 1) * 128] = ox_[::-1]
                    else:
                        of_c[b][:, h * 128:(h + 1) * 128] = oc_; of_x[b][:, h * 128:(h + 1) * 128] = ox_
            extra = {"ax": (a_x, a_c), "of": (of_x, of_c), "ob": (ob_x, ob_c), "gr": (px[:, :, 3584:4096], pc[:, :, 3584:4096]),
                     "ngB": _bc(np.tile(A(hg_norm_g[j]), 4)[None], (128, 512)), "wout": A(e_w_out[j]),
                     "w1": A(ffn_w1[j])[None], "w3": A(ffn_w3[j])[None], "w2": A(ffn_w2[j])[None]}
            x, ctx, px, pc = dense("even", nxt, l, l + 1, x, ctx, extra)
        else:
            nc_ret = _prog("ret", build_ret)
            rc = ret_consts()
            perm = np.concatenate([(np.arange(128) + 64) % 128, 128 + (np.arange(128) + 64) % 128])
            maps = []
            for core in range(8):
                qT = np.zeros((2, 2, 128, 4352), f32); qsT = np.zeros_like(qT); kT = np.zeros_like(qT); ksT = np.zeros_like(qT)
                vv = np.zeros((2, 4352, 512), f32); ld = np.zeros((2, 2), f32)
                for i in range(2):
                    n = core * 2 + i
                    b, h = n // 4, n % 4
                    qq = np.concatenate([pc[b][:, h * 256:(h + 1) * 256], px[b][:, h * 256:(h + 1) * 256]], 0)
                    kk = np.concatenate([pc[b][:, 1024 + h * 256:1024 + (h + 1) * 256], px[b][:, 1024 + h * 256:1024 + (h + 1) * 256]], 0)
                    qT[i] = qq.T.reshape(2, 128, 4352); qsT[i] = qq[:, perm].T.reshape(2, 128, 4352)
                    kT[i] = kk.T.reshape(2, 128, 4352); ksT[i] = kk[:, perm].T.reshape(2, 128, 4352)
                    vv[i] = np.concatenate([pc[b][:, 2048 + h * 512:2048 + (h + 1) * 512], px[b][:, 2048 + h * 512:2048 + (h + 1) * 512]], 0)
                    ld[i] = A(ret_log_decay[j])[:, h]
                d = dict(rc); d.update(qT=qT, qsT=qsT, kT=kT, ksT=ksT, v=vv, ld=ld)
                maps.append(d)
            r = _run(nc_ret, maps)
            o_x = np.zeros((4, 4096, 2048), f32); o_c = np.zeros((4, 256, 2048), f32)
            for core in range(8):
                for i in range(2):
                    n = core * 2 + i
                    b, h = n // 4, n % 4
                    o = r[core]["o"][i]
                    o_c[b][:, h * 512:(h + 1) * 512] = o[:256]; o_x[b][:, h * 512:(h + 1) * 512] = o[256:]
            extra = {"o": (o_x, o_c), "gr": (px[:, :, 4096:6144], pc[:, :, 4096:6144]),
                     "rw": A(router_w[j]), "rbB": _bc(A(router_b[j])[None], (128, 8)), "wout": A(o_w_out[j]),
                     "w1": A(moe_w1[j]), "w3": A(moe_w3[j]), "w2": A(moe_w2[j])}
            x, ctx, px, pc = dense("odd", nxt, l, l + 1, x, ctx, extra)
    return x.astype(np.float32)
```
